# Optimizing a Trainium2 kernel written in Bass

```python
import math
import numpy as np
import jax
import jax.numpy as jnp
from jax import lax

D_MODEL = 1024
BATCH = 8
SEQ = 4096
DEPTH = 2

PLE_DIM = 256
N_MIXERS = 2
N_NSA_LAYERS = (DEPTH + 1) // 2
N_RET_LAYERS = DEPTH // 2
RMS_EPS = 1e-6
GN_EPS = 1e-5

NSA_HEADS = 16
NSA_HEAD_DIM = D_MODEL // NSA_HEADS
NSA_KV_GROUPS = 4
NSA_HPG = NSA_HEADS // NSA_KV_GROUPS
CMP_BLOCK = 32
CMP_STRIDE = 16
CMP_HIDDEN = 256
SEL_BLOCK = 64
SEL_TOPN = 16
WINDOW = 512
Q_CHUNK = 32
FORCE_BONUS = 1e4
NEG = -1e30
NSA_Q_W = NSA_HEADS * NSA_HEAD_DIM
NSA_KV_W = NSA_KV_GROUPS * NSA_HEAD_DIM
NSA_GATE_COLS = 3 * NSA_HEADS
NSA_Z_W = NSA_HEADS * NSA_HEAD_DIM
NSA_SIZES = [NSA_Q_W] + [NSA_KV_W] * 6 + [NSA_GATE_COLS, NSA_Z_W]
NSA_IN_COLS = int(sum(NSA_SIZES))
NSA_SPLITS = [int(v) for v in np.cumsum(NSA_SIZES[:-1])]

RET_HEADS = 4
RET_KEY_DIM = D_MODEL // RET_HEADS
RET_VAL_DIM = 2 * RET_KEY_DIM
RET_CHUNK = 128
ROPE_BASE = 10000.0
RET_SIZES = [RET_HEADS * RET_KEY_DIM, RET_HEADS * RET_KEY_DIM,
             RET_HEADS * RET_VAL_DIM, RET_HEADS * RET_VAL_DIM]
RET_IN_COLS = int(sum(RET_SIZES))
RET_SPLITS = [int(v) for v in np.cumsum(RET_SIZES[:-1])]

kernel_name = "nsa_retention_interleaved_hybrid"


def rms_norm(x, g):
    xf = x.astype(jnp.float32)
    y = xf * lax.rsqrt(jnp.mean(xf * xf, axis=-1, keepdims=True) + RMS_EPS)
    return (y * g.astype(jnp.float32)).astype(x.dtype)


def masked_softmax(s, mask):
    s32 = jnp.where(mask, s.astype(jnp.float32), NEG)
    p = jax.nn.softmax(s32, axis=-1)
    return jnp.where(mask, p, 0.0)


def cmp_sel_overlap(n_cmp, n_sel):
    i = np.arange(n_cmp)[:, None]
    j = np.arange(n_sel)[None, :]
    c_start = i * CMP_STRIDE
    c_end = c_start + CMP_BLOCK
    ov = (c_start < (j + 1) * SEL_BLOCK) & (c_end > j * SEL_BLOCK)
    return ov.astype(np.float32)


def compress(k, pos, w1, w2):
    b, s, g, d = k.shape
    n_sub_tot = s // CMP_STRIDE
    n_sub = CMP_BLOCK // CMP_STRIDE
    sub = k.reshape(b, n_sub_tot, CMP_STRIDE, g, d)
    n_cmp = n_sub_tot - n_sub + 1
    blocks = jnp.concatenate([sub[:, j:j + n_cmp] for j in range(n_sub)], axis=2)
    blocks = blocks + pos[None, None, :, None, :]
    flat = blocks.transpose(0, 1, 3, 2, 4).reshape(b, n_cmp, g, CMP_BLOCK * d)
    return jax.nn.silu(flat @ w1) @ w2


def nsa_mixer(h, w_in, q_g, kc_g, ks_g, kw_g, pos_k, pos_v, ck_w1, ck_w2, cv_w1, cv_w2, w_out):
    b, s, _ = h.shape
    G, HPG, dk = NSA_KV_GROUPS, NSA_HPG, NSA_HEAD_DIM
    proj = h @ w_in
    q, kc, vc, ks, vs, kw, vw, gl, z = jnp.split(proj, NSA_SPLITS, axis=-1)
    q = rms_norm(q.reshape(b, s, G, HPG, dk), q_g) * (dk ** -0.5)
    kvs = (b, s, G, dk)
    kc = rms_norm(compress(kc.reshape(kvs), pos_k, ck_w1, ck_w2), kc_g)
    vc = compress(vc.reshape(kvs), pos_v, cv_w1, cv_w2)
    ks = rms_norm(ks.reshape(kvs), ks_g)
    vs = vs.reshape(kvs)
    kw = rms_norm(kw.reshape(kvs), kw_g)
    vw = vw.reshape(kvs)
    gates = jax.nn.sigmoid(gl.reshape(b, s, G, HPG, 3))

    n_cmp = kc.shape[1]
    n_sel = s // SEL_BLOCK
    top_n = min(SEL_TOPN, n_sel)
    cmp_end = jnp.arange(n_cmp) * CMP_STRIDE + CMP_BLOCK - 1
    overlap = jnp.asarray(cmp_sel_overlap(n_cmp, n_sel))
    ks_blk = ks.reshape(b, n_sel, SEL_BLOCK, G, dk).transpose(0, 3, 1, 2, 4)
    vs_blk = vs.reshape(b, n_sel, SEL_BLOCK, G, dk).transpose(0, 3, 1, 2, 4)
    kw_pad = jnp.pad(kw, ((0, 0), (WINDOW, 0), (0, 0), (0, 0)))
    vw_pad = jnp.pad(vw, ((0, 0), (WINDOW, 0), (0, 0), (0, 0)))
    b_ix = jnp.arange(b)[:, None, None, None]
    g_ix = jnp.arange(G)[None, None, :, None]
    sel_off = jnp.arange(SEL_BLOCK)
    win_off = jnp.arange(WINDOW + Q_CHUNK)
    blk = jnp.arange(n_sel)
    n_keys_sel = top_n * SEL_BLOCK

    def chunk(c):
        t0 = c * Q_CHUNK
        t = t0 + jnp.arange(Q_CHUNK)
        qc = lax.dynamic_slice_in_dim(q, t0, Q_CHUNK, axis=1)
        gc = lax.dynamic_slice_in_dim(gates, t0, Q_CHUNK, axis=1)
        sc = jnp.einsum('btghd,bngd->btghn', qc, kc)
        mc = (cmp_end[None, :] <= t[:, None])[None, :, None, None, :]
        pc = masked_softmax(sc, mc)
        o_c = jnp.einsum('btghn,bngd->btghd', pc.astype(vc.dtype), vc)
        imp = jnp.einsum('btgn,nj->btgj', pc.sum(axis=3), overlap)
        cur = t // SEL_BLOCK
        valid = blk[None, :] <= cur[:, None]
        forced = (blk[None, :] == 0) | (blk[None, :] == cur[:, None]) | (blk[None, :] == cur[:, None] - 1)
        score = jnp.where(valid[None, :, None, :],
                          imp + FORCE_BONUS * forced[None, :, None, :].astype(jnp.float32), NEG)
        _, idx = lax.top_k(score, top_n)
        kg = ks_blk[b_ix, g_ix, idx].reshape(b, Q_CHUNK, G, n_keys_sel, dk)
        vg = vs_blk[b_ix, g_ix, idx].reshape(b, Q_CHUNK, G, n_keys_sel, dk)
        ss = jnp.einsum('btghd,btgmd->btghm', qc, kg)
        kpos = (idx[..., None] * SEL_BLOCK + sel_off).reshape(b, Q_CHUNK, G, n_keys_sel)
        ms = (kpos <= t[None, :, None, None])[:, :, :, None, :]
        ps = masked_softmax(ss, ms)
        o_s = jnp.einsum('btghm,btgmd->btghd', ps.astype(vg.dtype), vg)
        kwc = lax.dynamic_slice_in_dim(kw_pad, t0, WINDOW + Q_CHUNK, axis=1)
        vwc = lax.dynamic_slice_in_dim(vw_pad, t0, WINDOW + Q_CHUNK, axis=1)
        spos = t0 - WINDOW + win_off
        mw = (spos[None, :] >= 0) & (spos[None, :] <= t[:, None]) & (t[:, None] - spos[None, :] < WINDOW)
        sw = jnp.einsum('btghd,bsgd->btghs', qc, kwc)
        pw = masked_softmax(sw, mw[None, :, None, None, :])
        o_w = jnp.einsum('btghs,bsgd->btghd', pw.astype(vwc.dtype), vwc)
        return gc[..., 0:1] * o_c + gc[..., 1:2] * o_s + gc[..., 2:3] * o_w

    out = lax.map(chunk, jnp.arange(s // Q_CHUNK))
    out = out.transpose(1, 0, 2, 3, 4, 5).reshape(b, s, NSA_HEADS * dk)
    return (out * jax.nn.silu(z)) @ w_out


def rotary(x, pos):
    half = x.shape[-1] // 2
    inv = ROPE_BASE ** (-jnp.linspace(0.0, 1.0, half, dtype=jnp.float32))
    ang = pos[:, None] * inv[None, :]
    cos = jnp.cos(ang)[None, :, None, :].astype(x.dtype)
    sin = jnp.sin(ang)[None, :, None, :].astype(x.dtype)
    x1, x2 = x[..., :half], x[..., half:]
    return jnp.concatenate([x1 * cos - x2 * sin, x1 * sin + x2 * cos], axis=-1)


def retention_mixer(h, w_in, w_out):
    b, s, _ = h.shape
    H, dk, dv, C = RET_HEADS, RET_KEY_DIM, RET_VAL_DIM, RET_CHUNK
    proj = h @ w_in
    q, k, v, z = jnp.split(proj, RET_SPLITS, axis=-1)
    pos = jnp.arange(s, dtype=jnp.float32)
    q = rotary(q.reshape(b, s, H, dk), pos)
    k = rotary(k.reshape(b, s, H, dk), pos) * (dk ** -0.5)
    v = v.reshape(b, s, H, dv)
    n_c = s // C

    def to_chunks(a):
        return a.reshape(b, n_c, C, H, a.shape[-1]).transpose(1, 0, 3, 2, 4).astype(jnp.float32)

    log_g = jnp.log(1.0 - 2.0 ** (-5.0 - jnp.arange(H, dtype=jnp.float32)))
    ix = jnp.arange(C, dtype=jnp.float32)
    diff = ix[:, None] - ix[None, :]
    intra_decay = jnp.where(diff >= 0, jnp.exp(log_g[:, None, None] * jnp.maximum(diff, 0.0)), 0.0)
    q_decay = jnp.exp(log_g[:, None] * (ix + 1.0))
    k_decay = jnp.exp(log_g[:, None] * (C - 1.0 - ix))
    chunk_decay = jnp.exp(log_g * C)

    def step(state, qkv):
        qc, kc, vc = qkv
        att = jnp.einsum('bhid,bhjd->bhij', qc, kc) * intra_decay
        o = (jnp.einsum('bhij,bhje->bhie', att, vc)
             + jnp.einsum('bhid,bhde->bhie', qc, state) * q_decay[..., None])
        state = (state * chunk_decay[:, None, None]
                 + jnp.einsum('bhjd,bhje->bhde', kc * k_decay[..., None], vc))
        return state, o

    state0 = jnp.zeros((b, H, dk, dv), jnp.float32)
    _, o = lax.scan(step, state0, (to_chunks(q), to_chunks(k), to_chunks(v)))
    o = o.transpose(1, 0, 3, 2, 4).reshape(b, s, H, dv)
    mu = jnp.mean(o, axis=-1, keepdims=True)
    var = jnp.mean(jnp.square(o - mu), axis=-1, keepdims=True)
    o = ((o - mu) * lax.rsqrt(var + GN_EPS)).reshape(b, s, H * dv).astype(h.dtype)
    return (o * jax.nn.silu(z)) @ w_out


def setup_inputs(seed: int = 0) -> dict:
    key = jax.random.key(seed)
    ks = jax.random.split(key, 20)

    def nrm(k, shape, fan_in):
        return jax.random.normal(k, shape, jnp.float32) * (fan_in ** -0.5)

    def gain(k, shape):
        return 1.0 + 0.02 * jax.random.normal(k, shape, jnp.float32)

    NA, NB, dk = N_NSA_LAYERS, N_RET_LAYERS, NSA_HEAD_DIM
    return {
        "x": jax.random.normal(ks[0], (BATCH, SEQ, D_MODEL), jnp.float32),
        "p": jax.random.normal(ks[1], (DEPTH, BATCH, SEQ, PLE_DIM), jnp.float32),
        "norm_g": gain(ks[2], (DEPTH, D_MODEL)),
        "nsa_w_in": nrm(ks[3], (NA, D_MODEL, NSA_IN_COLS), D_MODEL),
        "nsa_q_g": gain(ks[4], (NA, dk)),
        "nsa_kc_g": gain(ks[5], (NA, dk)),
        "nsa_ks_g": gain(ks[6], (NA, dk)),
        "nsa_kw_g": gain(ks[7], (NA, dk)),
        "nsa_cmp_pos_k": 0.1 * jax.random.normal(ks[8], (NA, CMP_BLOCK, dk), jnp.float32),
        "nsa_cmp_pos_v": 0.1 * jax.random.normal(ks[9], (NA, CMP_BLOCK, dk), jnp.float32),
        "nsa_cmp_k_w1": nrm(ks[10], (NA, CMP_BLOCK * dk, CMP_HIDDEN), CMP_BLOCK * dk),
        "nsa_cmp_k_w2": nrm(ks[11], (NA, CMP_HIDDEN, dk), CMP_HIDDEN),
        "nsa_cmp_v_w1": nrm(ks[12], (NA, CMP_BLOCK * dk, CMP_HIDDEN), CMP_BLOCK * dk),
        "nsa_cmp_v_w2": nrm(ks[13], (NA, CMP_HIDDEN, dk), CMP_HIDDEN),
        "nsa_w_out": nrm(ks[14], (NA, NSA_Z_W, D_MODEL), NSA_Z_W),
        "ret_w_in": nrm(ks[15], (NB, D_MODEL, RET_IN_COLS), D_MODEL),
        "ret_w_out": nrm(ks[16], (NB, RET_HEADS * RET_VAL_DIM, D_MODEL), RET_HEADS * RET_VAL_DIM),
        "ple_w": nrm(ks[17], (DEPTH, PLE_DIM, D_MODEL), PLE_DIM),
        "ple_gate_w": nrm(ks[18], (DEPTH, D_MODEL, D_MODEL), D_MODEL),
    }


def reference(x, p, norm_g, nsa_w_in, nsa_q_g, nsa_kc_g, nsa_ks_g, nsa_kw_g,
              nsa_cmp_pos_k, nsa_cmp_pos_v, nsa_cmp_k_w1, nsa_cmp_k_w2,
              nsa_cmp_v_w1, nsa_cmp_v_w2, nsa_w_out, ret_w_in, ret_w_out,
              ple_w, ple_gate_w):
    for i in range(DEPTH):
        h = rms_norm(x, norm_g[i])
        j = i // N_MIXERS
        if i % N_MIXERS == 0:
            y = nsa_mixer(h, nsa_w_in[j], nsa_q_g[j], nsa_kc_g[j], nsa_ks_g[j], nsa_kw_g[j],
                          nsa_cmp_pos_k[j], nsa_cmp_pos_v[j], nsa_cmp_k_w1[j], nsa_cmp_k_w2[j],
                          nsa_cmp_v_w1[j], nsa_cmp_v_w2[j], nsa_w_out[j])
        else:
            y = retention_mixer(h, ret_w_in[j], ret_w_out[j])
        x = x + y
        x = x + jax.nn.sigmoid(x @ ple_gate_w[i]) * (p[i] @ ple_w[i])
    return x
```

```python
import numpy as np
import ml_dtypes
import concourse.bass as bass
import concourse.mybir as mybir
from contextlib import ExitStack

F32 = mybir.dt.float32
BF16 = mybir.dt.bfloat16
AF = mybir.ActivationFunctionType
ALU = mybir.AluOpType
AX = mybir.AxisListType
ENGS = ("sync", "scalar", "vector", "gpsimd", "tensor")


class Buf:
    __slots__ = ("w", "r", "name", "dsem")

    def __init__(self, name=""):
        self.w = None
        self.r = {}
        self.name = name
        self.dsem = None


class Glob:
    def __init__(self, nc, es, n_dma_sems=96):
        self.nc = nc
        self.esem = {e: es.enter_context(nc.semaphore("es_" + e)) for e in ENGS}
        self.cnt = {e: 0 for e in ENGS}
        self.dsems = [es.enter_context(nc.semaphore(f"ds{i}")) for i in range(n_dma_sems)]
        self.dcnt = [0] * n_dma_sems
        self.next_dsem = 0
        self.seen = {e: {} for e in ENGS}

    def sem_of(self, key):
        if isinstance(key, str):
            return self.esem[key]
        return self.dsems[key]

    def alloc_dsem(self):
        i = self.next_dsem
        assert i < len(self.dsems), "out of dma semaphores"
        self.next_dsem += 1
        return i


class Sched:
    def __init__(self, G):
        self.G = G
        self.q = {e: [] for e in ENGS}
        self.pending_dma = []

    def _wait(self, eng, tok):
        if tok is None:
            return
        key, val = tok
        if eng == "tensor" and key == "tensor":
            return
        seen = self.G.seen[eng]
        if seen.get(key, 0) >= val:
            return
        seen[key] = val
        sem = self.G.sem_of(key)
        self.q[eng].append(lambda e, sem=sem, val=val: e.wait_ge(sem, val))

    def _deps(self, eng, reads, writes, extra):
        for b in reads:
            self._wait(eng, b.w)
        for b in writes:
            self._wait(eng, b.w)
            for k, v in b.r.items():
                self._wait(eng, (k, v))
        for t in extra:
            self._wait(eng, t)

    @staticmethod
    def _mark(tok, reads, writes):
        k, v = tok
        for b in reads:
            if b.r.get(k, 0) < v:
                b.r[k] = v
        for b in writes:
            b.w = tok
            b.r = {}

    def op(self, eng, fn, reads=(), writes=(), extra=()):
        G = self.G
        self._deps(eng, reads, writes, extra)
        G.cnt[eng] += 1
        tok = (eng, G.cnt[eng])
        sem = G.esem[eng]
        self.q[eng].append(lambda e, fn=fn, sem=sem: fn(e).then_inc(sem, 1))
        self._mark(tok, reads, writes)
        return tok

    def dma(self, q, pairs, reads=(), writes=(), extra=(), **kw):
        G = self.G
        bufs = list(writes) + list(reads)
        b0 = bufs[0]
        if b0.dsem is None:
            b0.dsem = G.alloc_dsem()
        si = b0.dsem
        self._deps(q, reads, writes, extra)
        sem = G.dsems[si]
        for (o, i) in pairs:
            G.dcnt[si] += 16
            self.q[q].append(lambda e, o=o, i=i, sem=sem, kw=kw: e.dma_start(out=o, in_=i, **kw).then_inc(sem, 16))
        tok = (si, G.dcnt[si])
        self._mark(tok, reads, writes)
        self.pending_dma.append(tok)
        return tok

    def finish(self, nc):
        for t in self.pending_dma:
            self._wait("sync", t)
        with nc.Block() as block:
            for e in ENGS:
                lst = self.q[e]

                def body(eng, lst=lst):
                    for f in lst:
                        f(eng)
                getattr(block, e)(body)

    def act(self, out, in_, func, reads=(), writes=(), eng="scalar", **kw):
        return self.op(eng, lambda e: e.activation(out=out, in_=in_, func=func, **kw), reads, writes)

    def mm(self, out, lhsT, rhs, start, stop, reads=(), writes=(), **kw):
        return self.op("tensor", lambda e: e.matmul(out, lhsT, rhs, start=start, stop=stop, **kw), reads, writes)

    def tr(self, out, in_, ident, reads=(), writes=()):
        return self.op("tensor", lambda e: e.transpose(out, in_, ident), reads, writes)

    def tt(self, eng, out, in0, in1, op, reads=(), writes=()):
        return self.op(eng, lambda e: e.tensor_tensor(out=out, in0=in0, in1=in1, op=op), reads, writes)

    def ts(self, eng, out, in0, s1, s2, op0, op1=None, reads=(), writes=(), **kw):
        if op1 is None:
            return self.op(eng, lambda e: e.tensor_scalar(out=out, in0=in0, scalar1=s1, scalar2=None, op0=op0, **kw), reads, writes)
        return self.op(eng, lambda e: e.tensor_scalar(out=out, in0=in0, scalar1=s1, scalar2=s2, op0=op0, op1=op1, **kw), reads, writes)

    def stt(self, out, in0, scalar, in1, op0, op1, reads=(), writes=(), eng="vector"):
        return self.op(eng, lambda e: e.scalar_tensor_tensor(out=out, in0=in0, scalar=scalar, in1=in1, op0=op0, op1=op1), reads, writes)

    def copy(self, eng, out, in_, reads=(), writes=()):
        if eng == "scalar":
            return self.op(eng, lambda e: e.copy(out=out, in_=in_), reads, writes)
        return self.op(eng, lambda e: e.tensor_copy(out=out, in_=in_), reads, writes)

    def memset(self, eng, ap, val, writes=()):
        return self.op(eng, lambda e: e.memset(ap, val), (), writes)

    def reduce(self, out, in_, op, axis, reads=(), writes=(), eng="vector"):
        return self.op(eng, lambda e: e.tensor_reduce(out=out, in_=in_, axis=axis, op=op), reads, writes)


def bc(ap, n):
    return bass.AP(ap.tensor, ap.offset, [list(x) for x in ap.ap] + [[0, n]])


def mkap(ap, dims):
    return bass.AP(ap.tensor, ap.offset, [list(ap.ap[0])] + [list(d) for d in dims])


BIG = 30000.0
def make_consts():
    c = {}
    c["ident"] = np.eye(128, dtype=ml_dtypes.bfloat16)
    i = np.arange(256)[:, None]; j = np.arange(64)[None, :]
    ov = ((i * 16 < (j + 1) * 64) & (i * 16 + 32 > j * 64)).astype(np.float32)
    ov[255] = 0
    c["overlap"] = ov
    E = (np.arange(4096)[None, :] // 64 == np.arange(64)[:, None]).astype(np.float32)
    c["Eexp"] = E.astype(ml_dtypes.bfloat16)
    t = (np.arange(32)[None, :, None] * 128 + np.arange(128)[:, None, None])
    cur = t // 64
    b = np.arange(64)[None, None, :]
    valid = b <= cur
    forced = (b == 0) | (b == cur) | (b == cur - 1)
    F = np.where(valid, np.where(forced, 1e4, 0.0), -1e30).astype(np.float32)
    c["Ftab"] = np.ascontiguousarray(F)
    n = (np.arange(2)[None, :, None, None] * 128 + np.arange(128)[None, None, :, None])
    tt = (np.arange(32)[:, None, None, None] * 128 + np.arange(128)[None, None, None, :])
    cb = np.where(16 * n + 31 > tt, -BIG, 0.0).astype(np.float32)
    c["cmpb"] = cb.astype(ml_dtypes.bfloat16)
    ii = np.arange(128)[:, None]; uu = np.arange(128)[None, :]
    wb = np.stack([np.where(ii > uu, -BIG, 0.0), np.where(ii <= uu, -BIG, 0.0)]).astype(np.float32)
    c["wbias"] = wb.astype(ml_dtypes.bfloat16)
    H, C = 4, 128
    log_g = np.log(1.0 - 2.0 ** (-5.0 - np.arange(H, dtype=np.float64)))
    ix = np.arange(C, dtype=np.float64)
    diff = ix[:, None] - ix[None, :]
    intra = np.where(diff >= 0, np.exp(log_g[:, None, None] * np.maximum(diff, 0.0)), 0.0)
    c["decT"] = np.ascontiguousarray(intra.transpose(2, 0, 1)).astype(np.float32)
    q_decay = np.exp(log_g[:, None] * (ix + 1.0))
    c["qdT"] = np.ascontiguousarray(np.broadcast_to(q_decay[None], (128, H, C))).astype(np.float32)
    k_decay = np.exp(log_g[:, None] * (C - 1.0 - ix))
    c["kdec"] = np.ascontiguousarray(k_decay.T).astype(np.float32)
    c["chunk_decay"] = np.exp(log_g * C)
    half = 128
    inv = (np.float32(10000.0) ** (-np.linspace(0.0, 1.0, half, dtype=np.float32))).astype(np.float32)
    pos = np.arange(4096, dtype=np.float32)
    ang = (pos[:, None] * inv[None, :]).astype(np.float32).astype(np.float64)
    cs, sn = np.cos(ang), np.sin(ang)
    c["rope"] = np.ascontiguousarray(np.stack([cs, sn, cs / 16.0, sn / 16.0], axis=1)).astype(np.float32)
    return c


S_, D_ = 4096, 1024
NT = 32
EPS = 1e-6
NSA_COLS = 3632


def declare_scratch(nc, debug):
    kind = dict(kind="ExternalOutput") if debug else {}
    dbg = debug if isinstance(debug, (set, list, tuple)) else None
    d = {}
    d["qT"] = nc.dram_tensor("qT_s", [16, 64, S_], BF16, **(kind if (dbg is None or "qT" in dbg) else {})).ap()
    d["kvT"] = nc.dram_tensor("kvT_s", [16, 64, S_], BF16, **(kind if (dbg is None or "kvT" in dbg) else {})).ap()
    d["vsA"] = nc.dram_tensor("vsA_s", [NT, 128, 260], BF16, **(kind if (dbg is None or "vsA" in dbg) else {})).ap()
    d["vwA"] = nc.dram_tensor("vwA_s", [NT, 128, 260], BF16, **(kind if (dbg is None or "vwA" in dbg) else {})).ap()
    d["gates"] = nc.dram_tensor("gates_s", [S_, 48], F32, **(kind if (dbg is None or "gates" in dbg) else {})).ap()
    d["sz"] = nc.dram_tensor("sz_s", [S_, 1024], BF16, **(kind if (dbg is None or "sz" in dbg) else {})).ap()
    return d


def phase1(nc, G, I, SC, ntg=8):
    S = Sched(G)
    with ExitStack() as es:
        def sb(name, shape, dt):
            return es.enter_context(nc.sbuf_tensor("p1_" + name, shape, dt))

        def ps(name, shape, dt):
            return es.enter_context(nc.psum_tensor("p1_" + name, shape, dt))

        w_sb = sb("w_sb", [128, 8, NSA_COLS], BF16)
        gb = sb("gb", [128, 1024], F32)
        gcol = sb("gcol", [64, 4], F32)
        ident = sb("ident", [128, 128], BF16)
        mhalf = sb("mhalf", [128, 8], F32)
        xt = [sb(f"xt{i}", [128, 1024], F32) for i in range(2)]
        junk = sb("junk", [128, 1024], BF16)
        ssx = [sb(f"ssx{i}", [128, 2], F32) for i in range(2)]
        hb = [sb(f"hb{i}", [128, 1024], BF16) for i in range(2)]
        hT = [sb(f"hT{i}", [128, 8, 128], BF16) for i in range(2)]
        sq = [sb(f"sq{i}", [128, 512], F32) for i in range(2)]
        ss = [sb(f"ss{i}", [128, 8], F32) for i in range(2)]
        rs = [sb(f"rs{i}", [128, 8], F32) for i in range(2)]
        qn = [sb(f"qn{i}", [128, 512], BF16) for i in range(2)]
        zt = [sb(f"zt{i}", [128, 512], F32) for i in range(2)]
        zh = [sb(f"zh{i}", [128, 512], F32) for i in range(2)]
        gtmp = sb("gtmp", [128, 48], F32)
        qTt = [sb(f"qTt{i}", [64, 16, 512], BF16) for i in range(2)]
        kvTt = [sb(f"kvTt{i}", [64, 16, 512], BF16) for i in range(2)]
        vsAt = [sb(f"vsAt{i}", [128, 4, 260], BF16) for i in range(2)]
        vwAt = [sb(f"vwAt{i}", [128, 4, 260], BF16) for i in range(2)]
        szt = [sb(f"szt{i}", [128, 4, 1024], BF16) for i in range(2)]
        gt = [sb(f"gt{i}", [128, 4, 48], F32) for i in range(2)]

        pj = [ps(f"pj{i}", [128, 512], F32) for i in range(4)]
        tpa = ps("tpa", [128, 1024], BF16)
        tpq = [ps(f"tpq{i}", [64, 1024], BF16) for i in range(2)]

        B = lambda n: Buf(n)
        b_w = B("w"); b_c = B("consts")
        b_xt = [B("xt") for _ in range(2)]; b_junk = B("junk"); b_ssx = [B("ssx") for _ in range(2)]
        b_hb = [B("hb") for _ in range(2)]; b_hT = [B("hT") for _ in range(2)]
        b_sq = [B("sq") for _ in range(2)]; b_ss = [B("ss") for _ in range(2)]; b_rs = [B("rs") for _ in range(2)]
        b_qn = [B("qn") for _ in range(2)]; b_zt = [B("zt") for _ in range(2)]; b_zh = [B("zh") for _ in range(2)]
        b_gtmp = B("gtmp")
        b_qTt = [B("qTt") for _ in range(2)]; b_kvTt = [B("kvTt") for _ in range(2)]
        b_vsAt = [B("vsAt") for _ in range(2)]; b_vwAt = [B("vwAt") for _ in range(2)]
        b_szt = [B("szt") for _ in range(2)]; b_gt = [B("gt") for _ in range(2)]
        b_pj = [B("pj") for _ in range(4)]; b_tpa = B("tpa"); b_tpq = [B("tpq") for _ in range(2)]

        S.dma("sync", [(ident[:], I["ident"])], writes=[b_c])
        S.dma("sync", [(gb[:], bass.AP(I["norm_g"].tensor, 0, [[0, 128], [1, 1024]]))], writes=[b_c])
        for j, nm in enumerate(["nsa_q_g", "nsa_ks_g", "nsa_kw_g"]):
            S.dma("sync", [(gcol[:, j:j + 1], bass.AP(I[nm].tensor, 0, [[1, 64], [1, 1]]))], writes=[b_c])
        S.ts("vector", gcol[:, 0:1], gcol[:, 0:1], 0.125, None, ALU.mult, reads=[b_c], writes=[b_c])
        S.memset("vector", mhalf[:], -0.5, writes=[b_c])
        for i in range(2):
            S.memset("vector", vsAt[i][:], 1.0, writes=[b_vsAt[i]])
            S.memset("vector", vwAt[i][:], 1.0, writes=[b_vwAt[i]])
        half = NSA_COLS // 2
        for kc in range(8):
            S.dma("gpsimd", [(w_sb[:, kc, c0:c0 + half], I["nsa_w_in"][kc * 128:(kc + 1) * 128, c0:c0 + half])
                             for c0 in (0, half)], writes=[b_w])

        nseg = 0
        tq = 0

        def norm_heads(pj_ap, b_pjn, nh, out_ap3, b_out):
            nonlocal nseg
            k = nseg % 2
            nseg += 1
            n = nh * 64
            S.act(sq[k][:, 0:n], pj_ap, AF.Square, reads=[b_pjn], writes=[b_sq[k]])
            S.reduce(ss[k][:, 0:nh], sq[k][:, 0:n].rearrange("p (h d) -> p h d", d=64), ALU.add, AX.X,
                     reads=[b_sq[k]], writes=[b_ss[k]])
            S.ts("vector", ss[k][:, 0:nh], ss[k][:, 0:nh], 1.0 / 64, EPS, ALU.mult, ALU.add,
                 reads=[b_ss[k]], writes=[b_ss[k]])
            S.tt("gpsimd", rs[k][:, 0:nh], ss[k][:, 0:nh], mhalf[:, 0:nh], ALU.pow,
                 reads=[b_ss[k], b_c], writes=[b_rs[k]])
            S.tt("vector", out_ap3, pj_ap.rearrange("p (h d) -> p h d", d=64), bc(rs[k][:, 0:nh], 64), ALU.mult,
                 reads=[b_pjn, b_rs[k]], writes=[b_out])

        for tg in range(ntg):
            sp = tg % 2
            for tl in range(4):
                ti = tg * 4 + tl
                xp = ti % 2
                S.dma("sync", [(xt[xp][:], I["x"][ti * 128:(ti + 1) * 128, :])], writes=[b_xt[xp]])
                S.act(junk[:], xt[xp][:], AF.Square, reads=[b_xt[xp]], writes=[b_junk, b_ssx[xp]],
                      accum_out=ssx[xp][:, 0:1])
                S.ts("vector", ssx[xp][:, 0:1], ssx[xp][:, 0:1], 1.0 / 1024, EPS, ALU.mult, ALU.add,
                     reads=[b_ssx[xp]], writes=[b_ssx[xp]])
                S.tt("gpsimd", ssx[xp][:, 1:2], ssx[xp][:, 0:1], mhalf[:, 0:1], ALU.pow,
                     reads=[b_ssx[xp], b_c], writes=[b_ssx[xp]])
                S.stt(hb[xp][:], xt[xp][:], ssx[xp][:, 1:2], gb[:], ALU.mult, ALU.mult,
                      reads=[b_xt[xp], b_ssx[xp], b_c], writes=[b_hb[xp]])
                for kc in range(8):
                    S.tr(tpa[:, kc * 128:(kc + 1) * 128], hb[xp][:, kc * 128:(kc + 1) * 128], ident[:],
                         reads=[b_hb[xp], b_c], writes=[b_tpa])
                S.copy("scalar", hT[xp][:].rearrange("p a b -> p (a b)"), tpa[:], reads=[b_tpa], writes=[b_hT[xp]])

                segs = [(0, 512, "q0"), (512, 512, "q1"), (1024, 512, "kcvc"), (1536, 512, "ksvs"),
                        (2048, 512, "kwvw"), (2608, 512, "z0"), (3120, 512, "z1"), (2560, 48, "gl")]
                for si, (c0, n, kind) in enumerate(segs):
                    pb = si % 4
                    for kc in range(8):
                        S.mm(pj[pb][:, 0:n], hT[xp][:, kc, :], w_sb[:, kc, c0:c0 + n], kc == 0, kc == 7,
                             reads=[b_hT[xp], b_w], writes=[b_pj[pb]])
                    if kind in ("q0", "q1"):
                        qk = nseg % 2
                        norm_heads(pj[pb][:, 0:512], b_pj[pb], 8, qn[qk][:].rearrange("p (h d) -> p h d", d=64), b_qn[qk])
                        tk = tq % 2; tq += 1
                        for h in range(8):
                            S.tr(tpq[tk][:, h * 128:(h + 1) * 128], qn[qk][:, h * 64:(h + 1) * 64], ident[:],
                                 reads=[b_qn[qk], b_c], writes=[b_tpq[tk]])
                        h0 = 0 if kind == "q0" else 8
                        S.act(qTt[sp][:, h0:h0 + 8, tl * 128:(tl + 1) * 128],
                              tpq[tk][:].rearrange("p (h t) -> p h t", t=128), AF.Copy,
                              reads=[b_tpq[tk], b_c], writes=[b_qTt[sp]], scale=gcol[:, 0:1])
                    elif kind == "kcvc":
                        qk = nseg % 2; nseg += 1
                        S.copy("scalar", qn[qk][:], pj[pb][:, 0:512], reads=[b_pj[pb]], writes=[b_qn[qk]])
                        tk = tq % 2; tq += 1
                        for h in range(8):
                            S.tr(tpq[tk][:, h * 128:(h + 1) * 128], qn[qk][:, h * 64:(h + 1) * 64], ident[:],
                                 reads=[b_qn[qk], b_c], writes=[b_tpq[tk]])
                        S.copy("vector", kvTt[sp][:, 0:8, tl * 128:(tl + 1) * 128],
                               tpq[tk][:].rearrange("p (h t) -> p h t", t=128),
                               reads=[b_tpq[tk]], writes=[b_kvTt[sp]])
                    elif kind in ("ksvs", "kwvw"):
                        qk = nseg % 2
                        norm_heads(pj[pb][:, 0:256], b_pj[pb], 4,
                                   qn[qk][:, 0:256].rearrange("p (h d) -> p h d", d=64), b_qn[qk])
                        tk = tq % 2; tq += 1
                        for h in range(4):
                            S.tr(tpq[tk][:, h * 128:(h + 1) * 128], qn[qk][:, h * 64:(h + 1) * 64], ident[:],
                                 reads=[b_qn[qk], b_c], writes=[b_tpq[tk]])
                        r0, gc = (8, 1) if kind == "ksvs" else (12, 2)
                        S.act(kvTt[sp][:, r0:r0 + 4, tl * 128:(tl + 1) * 128],
                              tpq[tk][:, 0:512].rearrange("p (h t) -> p h t", t=128), AF.Copy,
                              reads=[b_tpq[tk], b_c], writes=[b_kvTt[sp]], scale=gcol[:, gc:gc + 1])
                        vt, bvt = (vsAt, b_vsAt) if kind == "ksvs" else (vwAt, b_vwAt)
                        S.copy("vector", vt[sp][:, tl, :].rearrange("p (g c) -> p g c", c=65)[:, :, 0:64],
                               pj[pb][:, 256:512].rearrange("p (g c) -> p g c", c=64),
                               reads=[b_pj[pb]], writes=[bvt[sp]])
                    elif kind in ("z0", "z1"):
                        zk = nseg % 2; nseg += 1
                        S.act(zt[zk][:], pj[pb][:, 0:512], AF.Tanh, reads=[b_pj[pb]], writes=[b_zt[zk]], scale=0.5)
                        S.act(zh[zk][:], pj[pb][:, 0:512], AF.Copy, reads=[b_pj[pb]], writes=[b_zh[zk]], scale=0.5)
                        z0 = 0 if kind == "z0" else 512
                        S.stt(szt[sp][:, tl, z0:z0 + 512], zt[zk][:], 1.0, zh[zk][:], ALU.add, ALU.mult,
                              reads=[b_zt[zk], b_zh[zk]], writes=[b_szt[sp]])
                    else:
                        S.act(gtmp[:], pj[pb][:, 0:48], AF.Tanh, reads=[b_pj[pb]], writes=[b_gtmp], scale=0.5)
                        S.ts("vector", gt[sp][:, tl, :], gtmp[:], 0.5, 0.5, ALU.mult, ALU.add,
                             reads=[b_gtmp], writes=[b_gt[sp]])
            t0 = tg * 512
            S.dma("sync", [(SC["qT"].rearrange("h d t -> d h t")[:, :, t0:t0 + 512], qTt[sp][:])], reads=[b_qTt[sp]])
            S.dma("sync", [(SC["kvT"].rearrange("h d t -> d h t")[:, :, t0:t0 + 512], kvTt[sp][:])], reads=[b_kvTt[sp]])
            S.dma("sync", [(SC["vsA"][tg * 4:(tg + 1) * 4].rearrange("t p c -> p t c"), vsAt[sp][:])], reads=[b_vsAt[sp]])
            S.dma("sync", [(SC["vwA"][tg * 4:(tg + 1) * 4].rearrange("t p c -> p t c"), vwAt[sp][:])], reads=[b_vwAt[sp]])
            S.dma("sync", [(SC["sz"][t0:t0 + 512, :].rearrange("(t p) c -> p t c", p=128), szt[sp][:])], reads=[b_szt[sp]])
            S.dma("sync", [(SC["gates"][t0:t0 + 512, :].rearrange("(t p) c -> p t c", p=128), gt[sp][:])], reads=[b_gt[sp]])
        S.finish(nc)


EPS = 1e-6


def declare_scratch2(nc, debug):
    kind = dict(kind="ExternalOutput") if debug else {}
    dbg = debug if isinstance(debug, (set, list, tuple)) else None
    d = {}
    d["kccT"] = nc.dram_tensor("kccT_s", [64, 4, 256], BF16, **(kind if (dbg is None or "kccT" in dbg) else {})).ap()
    d["vcA"] = nc.dram_tensor("vcA_s", [128, 2, 4, 129], BF16, **(kind if (dbg is None or "vcA" in dbg) else {})).ap()
    return d


def phase2(nc, G, I, SC):
    S = Sched(G)
    with ExitStack() as es:
        def sb(name, shape, dt):
            return es.enter_context(nc.sbuf_tensor("p2_" + name, shape, dt))

        def ps(name, shape, dt):
            return es.enter_context(nc.psum_tensor("p2_" + name, shape, dt))

        kvT = sb("kvT", [64, 8, 4096], BF16)
        W1 = [sb(f"W1{i}", [64, 32, 256], BF16) for i in range(2)]
        W2 = [sb(f"W2{i}", [128, 2, 64], BF16) for i in range(2)]
        posf = sb("posf", [64, 2, 32], F32)
        posb = sb("posb", [64, 2, 32], BF16)
        c1h = sb("c1h", [128, 4], F32)
        ident = sb("ident", [128, 128], BF16)
        ovf = sb("ovf", [128, 2, 64], F32)
        gk = sb("gk", [64, 1], F32)
        mhalf = sb("mhalf", [128, 8], F32)
        th = [sb(f"th{i}", [128, 256], F32) for i in range(2)]
        uu = [sb(f"uu{i}", [128, 256], F32) for i in range(2)]
        hid = [[sb(f"hid{a}{b}", [128, 256], BF16) for b in range(2)] for a in range(2)]
        sq = sb("sq", [128, 256], F32)
        ss = sb("ss", [128, 4], F32)
        rs = sb("rs", [128, 4], F32)
        kn = sb("kn", [128, 256], BF16)
        kccT = sb("kccT", [64, 4, 256], BF16)
        vcA = sb("vcA", [128, 2, 4, 129], BF16)

        ph = [ps(f"ph{i}", [128, 512], F32) for i in range(2)]
        pc1 = ps("pc1", [128, 512], F32)
        po = ps("po", [128, 512], F32)
        tp = ps("tp", [64, 1024], BF16)

        b_kvT = Buf(); b_W1 = [Buf(), Buf()]; b_W2 = [Buf(), Buf()]; b_c = Buf(); b_pos = Buf(); b_c1h = Buf()
        b_th = [Buf(), Buf()]; b_uu = [Buf(), Buf()]; b_hid = [[Buf(), Buf()], [Buf(), Buf()]]
        b_sq = Buf(); b_ss = Buf(); b_rs = Buf(); b_kn = Buf(); b_kccT = Buf(); b_vcA = Buf()
        b_ph = [Buf(), Buf()]; b_pc1 = Buf(); b_po = Buf(); b_tp = Buf()

        S.dma("sync", [(kvT[:, 0:4, :], SC["kvT"][0:4].rearrange("h d t -> d h t")),
                       (kvT[:, 4:8, :], SC["kvT"][4:8].rearrange("h d t -> d h t"))], writes=[b_kvT])
        for kv, nm in enumerate(["nsa_cmp_k_w1", "nsa_cmp_v_w1"]):
            src = I[nm].rearrange("(l d) m -> d l m", d=64)
            S.dma("gpsimd", [(W1[kv][:, l0:l0 + 8, :], src[:, l0:l0 + 8, :]) for l0 in range(0, 32, 8)], writes=[b_W1[kv]])
        for kv, nm in enumerate(["nsa_cmp_k_w2", "nsa_cmp_v_w2"]):
            S.dma("gpsimd", [(W2[kv][:], I[nm].rearrange("(c p) m -> p c m", p=128))], writes=[b_W2[kv]])
        for kv, nm in enumerate(["nsa_cmp_pos_k", "nsa_cmp_pos_v"]):
            S.dma("sync", [(posf[:, kv, :], bass.AP(I[nm].tensor, 0, [[1, 64], [64, 32]]))], writes=[b_pos],
                  allow_slow_non_contiguous=True)
        S.copy("vector", posb[:], posf[:], reads=[b_pos], writes=[b_pos])
        S.dma("sync", [(ident[:], I["ident"])], writes=[b_c])
        S.dma("sync", [(ovf[:], I["overlap"].rearrange("(c p) j -> p c j", p=128))], writes=[b_c])
        S.dma("sync", [(gk[:], bass.AP(I["nsa_kc_g"].tensor, 0, [[1, 64], [1, 1]]))], writes=[b_c])
        S.memset("vector", mhalf[:], -0.5, writes=[b_c])
        for a in range(2):
            for b in range(2):
                S.memset("vector", hid[a][b][:], 0.0, writes=[b_hid[a][b]])
        S.memset("vector", vcA[:], 1.0, writes=[b_vcA])
        S.memset("vector", kccT[:], 0.0, writes=[b_kccT])

        for kv in range(2):
            for hh in range(2):
                col = kv * 2 + hh
                for l in range(32):
                    S.mm(pc1[:, col:col + 1], W1[kv][:, l, hh * 128:(hh + 1) * 128], posb[:, kv, l:l + 1],
                         l == 0, l == 31, reads=[b_W1[kv], b_pos], writes=[b_pc1])
                S.act(c1h[:, col:col + 1], pc1[:, col:col + 1], AF.Copy, reads=[b_pc1], writes=[b_c1h], scale=0.5)

        it = 0
        for kv in range(2):
            for g in range(4):
                par = it % 2
                for hh in range(2):
                    pb = hh
                    col = kv * 2 + hh
                    for l in range(32):
                        rhs = mkap(kvT[:, kv * 4 + g, l:l + 1], [[16, 255]])
                        S.mm(ph[pb][:, 0:255], W1[kv][:, l, hh * 128:(hh + 1) * 128], rhs, l == 0, l == 31,
                             reads=[b_W1[kv], b_kvT], writes=[b_ph[pb]])
                    S.act(th[hh][:, 0:255], ph[pb][:, 0:255], AF.Tanh, reads=[b_ph[pb], b_c1h], writes=[b_th[hh]],
                          scale=0.5, bias=c1h[:, col:col + 1])
                    S.act(uu[hh][:, 0:255], ph[pb][:, 0:255], AF.Identity, reads=[b_ph[pb], b_c1h], writes=[b_uu[hh]],
                          scale=0.5, bias=c1h[:, col:col + 1])
                    S.stt(hid[par][hh][:, 0:255], th[hh][:, 0:255], 1.0, uu[hh][:, 0:255], ALU.add, ALU.mult,
                          reads=[b_th[hh], b_uu[hh]], writes=[b_hid[par][hh]])
                for c in range(2):
                    o_ap = po[:, (c * 4 + g) * 64:(c * 4 + g + 1) * 64]
                    for hh in range(2):
                        S.mm(o_ap, hid[par][hh][:, c * 128:(c + 1) * 128], W2[kv][:, hh, :], hh == 0, hh == 1,
                             reads=[b_hid[par][hh], b_W2[kv]], writes=[b_po], skip_group_check=True)
                it += 1
            for c in range(2):
                src = po[:, c * 256:(c + 1) * 256]
                if kv == 0:
                    S.act(sq[:], src, AF.Square, reads=[b_po], writes=[b_sq])
                    S.reduce(ss[:], sq[:].rearrange("p (h d) -> p h d", d=64), ALU.add, AX.X, reads=[b_sq], writes=[b_ss])
                    S.ts("vector", ss[:], ss[:], 1.0 / 64, EPS, ALU.mult, ALU.add, reads=[b_ss], writes=[b_ss])
                    S.tt("gpsimd", rs[:], ss[:], mhalf[:, 0:4], ALU.pow, reads=[b_ss, b_c], writes=[b_rs])
                    S.tt("vector", kn[:].rearrange("p (h d) -> p h d", d=64), src.rearrange("p (h d) -> p h d", d=64),
                         bc(rs[:], 64), ALU.mult, reads=[b_po, b_rs], writes=[b_kn])
                    for g in range(4):
                        S.tr(tp[:, g * 128:(g + 1) * 128], kn[:, g * 64:(g + 1) * 64], ident[:],
                             reads=[b_kn, b_c], writes=[b_tp])
                    S.act(kccT[:, :, c * 128:(c + 1) * 128], tp[:, 0:512].rearrange("p (g t) -> p g t", t=128), AF.Copy,
                          reads=[b_tp, b_c], writes=[b_kccT], scale=gk[:, 0:1])
                else:
                    S.copy("vector", vcA[:, c, :, 0:64], src.rearrange("p (g d) -> p g d", d=64), reads=[b_po], writes=[b_vcA])
                    S.copy("vector", vcA[:, c, :, 65:129], mkap(ovf[:, c, 0:1], [[0, 4], [1, 64]]), reads=[b_c], writes=[b_vcA])
        S.dma("sync", [(SC["kccT"], kccT[:])], reads=[b_kccT])
        S.dma("sync", [(SC["vcA"], vcA[:])], reads=[b_vcA])
        S.finish(nc)


NEG_BIG = -30000.0


def declare_scratch3(nc, debug):
    kind = dict(kind="ExternalOutput") if debug else {}
    dbg = debug if isinstance(debug, (set, list, tuple)) else None
    d = {}
    d["outg"] = nc.dram_tensor("outg_s", [4096, 1024], BF16, **(kind if (dbg is None or "outg" in dbg) else {})).ap()
    return d


def phase3(nc, G, I, SC, jlist=None, stage=9):
    S = Sched(G)
    jlist = list(range(32)) if jlist is None else jlist
    with ExitStack() as es:
        def sb(name, shape, dt):
            return es.enter_context(nc.sbuf_tensor("p3_" + name, shape, dt))

        def ps(name, shape, dt):
            return es.enter_context(nc.psum_tensor("p3_" + name, shape, dt))

        KE = sb("KE", [128, 4, 4096], BF16)
        kwT = sb("kwT", [64, 4, 4096], BF16)
        vsA = sb("vsA", [128, 32, 260], BF16)
        vwA = sb("vwA", [128, 32, 260], BF16)
        kccT = sb("kccT", [64, 4, 256], BF16)
        vcA = sb("vcA", [128, 2, 4, 129], BF16)
        Ftab = sb("Ftab", [128, 32, 64], F32)
        ident = sb("ident", [128, 128], BF16)
        wbias = sb("wbias", [128, 2, 128], BF16)
        cmpb = [sb(f"cmpb{i}", [128, 2, 128], BF16) for i in range(2)]
        QS = [sb(f"QS{i}", [128, 512], BF16) for i in range(2)]
        gt = [sb(f"gt{i}", [128, 48], F32) for i in range(2)]
        szt = [sb(f"szt{i}", [128, 1024], BF16) for i in range(2)]
        NP = 3
        Pt = [sb(f"Pt{i}", [128, 512], BF16) for i in range(NP)]
        acc = sb("acc", [128, 16, 64], F32)
        tmp = sb("tmp", [128, 4, 64], F32)
        og = [sb(f"og{i}", [128, 1024], BF16) for i in range(2)]
        zc = sb("zc", [128, 4], F32)
        rz = sb("rz", [128, 4], F32)
        coef = sb("coef", [128, 4], F32)
        score = sb("score", [128, 64], F32)
        work = sb("work", [128, 64], F32)
        m8 = sb("m8", [128, 16], F32)
        selb = sb("selb", [128, 128], BF16)

        NS = 3
        pS = [ps(f"pS{i}", [128, 512], F32) for i in range(NS)]
        Oc = ps("Oc", [128, 4, 256], F32)
        Ow = ps("Ow", [128, 4, 128], F32)
        Os = ps("Os", [128, 4, 128], F32)
        tps = ps("tps", [128, 1024], BF16)

        b_c = Buf(); bK = {n: Buf() for n in ["KE", "kwT", "vsA", "vwA", "kccT", "vcA"]}
        b_cmpb = [Buf(), Buf()]; b_QSq = [Buf(), Buf()]; b_QSs = [Buf(), Buf()]
        b_gt = [Buf(), Buf()]; b_szt = [Buf(), Buf()]
        b_Pt = [Buf() for _ in range(NP)]; b_acc = Buf(); b_tmp = Buf(); b_og = [Buf(), Buf()]
        b_zc = Buf(); b_rz = Buf(); b_coef = Buf(); b_score = Buf(); b_work = Buf(); b_m8 = Buf(); b_selb = Buf()
        b_pS = [Buf() for _ in range(NS)]; b_Oc = Buf(); b_Ow = Buf(); b_Os = Buf(); b_tps = Buf()

        S.dma("sync", [(ident[:], I["ident"])], writes=[b_c])
        S.dma("sync", [(Ftab[:], I["Ftab"])], writes=[b_c])
        S.dma("sync", [(wbias[:], I["wbias"].rearrange("w i u -> i w u"))], writes=[b_c])
        S.dma("sync", [(kccT[:], SC["kccT"])], writes=[bK["kccT"]])
        S.dma("sync", [(vcA[:], SC["vcA"])], writes=[bK["vcA"]])
        S.dma("sync", [(KE[0:64, :, :], SC["kvT"][8:12].rearrange("h d t -> d h t"))] + [(KE[64:128, g, :], I["Eexp"]) for g in range(4)], writes=[bK["KE"]])
        S.dma("sync", [(kwT[:], SC["kvT"][12:16].rearrange("h d t -> d h t"))], writes=[bK["kwT"]])
        S.dma("sync", [(vsA[:, a:a + 4, :], SC["vsA"][a:a + 4].rearrange("t p c -> p t c")) for a in range(0, 32, 4)], writes=[bK["vsA"]])
        S.dma("sync", [(vwA[:, a:a + 4, :], SC["vwA"][a:a + 4].rearrange("t p c -> p t c")) for a in range(0, 32, 4)], writes=[bK["vwA"]])
        S.memset("vector", selb[:], 0.0, writes=[b_selb])

        cnt = {"s": 0, "p": 0, "it": 0}

        def score_tile(mms, extra_reads):
            si = cnt["s"] % NS; cnt["s"] += 1
            pi = cnt["p"] % NP; cnt["p"] += 1
            for k, (l, r) in enumerate(mms):
                S.mm(pS[si][:], l, r, k == 0, k == len(mms) - 1, reads=extra_reads, writes=[b_pS[si]])
            S.act(Pt[pi][:], pS[si][:], AF.Exp, reads=[b_pS[si]], writes=[b_Pt[pi]])
            return Pt[pi], b_Pt[pi]

        for jn, j in enumerate(jlist if stage >= 1 else []):
            jp = jn % 2
            t0 = j * 128
            S.dma("sync", [(gt[jp][:], SC["gates"][t0:t0 + 128, :])], writes=[b_gt[jp]])
            S.dma("sync", [(szt[jp][:], SC["sz"][t0:t0 + 128, :])], writes=[b_szt[jp]])
            cs = []
            if j <= 16:
                cs.append((0, True))
            else:
                cs.append((0, False))
            if j >= 16:
                cs.append((1, True))
            S.dma("sync", [(cmpb[jp][:, c, :], I["cmpb"][j, c]) for (c, hb) in cs if hb], writes=[b_cmpb[jp]])
            for g in range(4):
                it = cnt["it"]; cnt["it"] += 1
                qp = it % 2
                S.dma("sync", [(QS[qp][0:64, :].rearrange("p (h t) -> p h t", t=128),
                                SC["qT"][4 * g:4 * g + 4, :, t0:t0 + 128].rearrange("h d t -> d h t"))],
                      writes=[b_QSq[qp]])
                qtop = QS[qp][0:64, :]
                for ci, (c, hasb) in enumerate(cs):
                    mms = [(kccT[:, g, c * 128:(c + 1) * 128], qtop)]
                    rd = [bK["kccT"], b_QSq[qp]]
                    if hasb:
                        mms.append((ident[:], mkap(cmpb[jp][:, c, 0:1], [[0, 4], [1, 128]])))
                        rd = rd + [b_c, b_cmpb[jp]]
                    P, bP = score_tile(mms, rd)
                    for h in range(4):
                        S.mm(Oc[:, h, 0:129], P[:, h * 128:(h + 1) * 128], vcA[:, c, g, :],
                             ci == 0 and h in (0, 2), ci == len(cs) - 1,
                             reads=[bP, bK["vcA"]], writes=[b_Oc], skip_group_check=True)
                if stage < 2:
                    continue
                S.ts("vector", zc[:], Oc[:, :, 64], 1e-30, None, ALU.max, reads=[b_Oc], writes=[b_zc])
                S.op("vector", lambda e: e.reciprocal(out=rz[:], in_=zc[:]), reads=[b_zc], writes=[b_rz])
                S.stt(score[:], Oc[:, 0, 65:129], rz[:, 0:1], Ftab[:, j, :], ALU.mult, ALU.add,
                      reads=[b_Oc, b_rz, b_c], writes=[b_score])
                for h in range(1, 4):
                    S.stt(score[:], Oc[:, h, 65:129], rz[:, h:h + 1], score[:], ALU.mult, ALU.add,
                          reads=[b_Oc, b_rz, b_score], writes=[b_score])
                S.op("vector", lambda e: e.max(out=m8[:, 0:8], in_=score[:]), reads=[b_score], writes=[b_m8])
                S.op("vector", lambda e: e.match_replace(out=work[:], in_to_replace=m8[:, 0:8], in_values=score[:],
                                                         imm_value=-3.0e38),
                     reads=[b_score, b_m8], writes=[b_work])
                S.op("vector", lambda e: e.max(out=m8[:, 8:16], in_=work[:]), reads=[b_work], writes=[b_m8])
                S.ts("vector", selb[:, 64:128], score[:], m8[:, 15:16], NEG_BIG, ALU.is_lt, ALU.mult,
                     reads=[b_score, b_m8], writes=[b_selb])
                S.tr(tps[:, 0:128], selb[:], ident[:], reads=[b_selb, b_c], writes=[b_tps])
                S.copy("vector", QS[qp][64:128, :].rearrange("p (h t) -> p h t", t=128),
                       mkap(tps[64:128, 0:1], [[0, 4], [1, 128]]), reads=[b_tps], writes=[b_QSs[qp]])
                S.tt("vector", coef[:], rz[:], mkap(gt[jp][:, 12 * g:12 * g + 1], [[3, 4]]), ALU.mult,
                     reads=[b_rz, b_gt[jp]], writes=[b_coef])
                S.tt("vector", acc[:, 4 * g:4 * g + 4, :], Oc[:, :, 0:64], bc(coef[:], 64), ALU.mult,
                     reads=[b_Oc, b_coef], writes=[b_acc])
                if stage < 3:
                    continue
                kts = [kt for kt in range(j - 4, j + 1) if kt >= 0]
                for ki, kt in enumerate(kts):
                    mms = [(kwT[:, g, kt * 128:(kt + 1) * 128], qtop)]
                    rd = [bK["kwT"], b_QSq[qp]]
                    if kt == j:
                        mms.append((ident[:], mkap(wbias[:, 0, 0:1], [[0, 4], [1, 128]])))
                        rd = rd + [b_c]
                    elif kt == j - 4:
                        mms.append((ident[:], mkap(wbias[:, 1, 0:1], [[0, 4], [1, 128]])))
                        rd = rd + [b_c]
                    P, bP = score_tile(mms, rd)
                    for h in range(4):
                        S.mm(Ow[:, h, 0:65], P[:, h * 128:(h + 1) * 128], vwA[:, kt, g * 65:(g + 1) * 65],
                             ki == 0 and h == 0, ki == len(kts) - 1,
                             reads=[bP, bK["vwA"]], writes=[b_Ow], skip_group_check=True)
                S.op("vector", lambda e: e.reciprocal(out=rz[:], in_=Ow[:, :, 64]), reads=[b_Ow], writes=[b_rz])
                S.tt("vector", coef[:], rz[:], mkap(gt[jp][:, 12 * g + 2:12 * g + 3], [[3, 4]]), ALU.mult,
                     reads=[b_rz, b_gt[jp]], writes=[b_coef])
                S.tt("vector", tmp[:], Ow[:, :, 0:64], bc(coef[:], 64), ALU.mult,
                     reads=[b_Ow, b_coef], writes=[b_tmp])
                S.tt("vector", acc[:, 4 * g:4 * g + 4, :], acc[:, 4 * g:4 * g + 4, :], tmp[:], ALU.add,
                     reads=[b_tmp], writes=[b_acc])
                if stage < 4:
                    continue
                for kt in range(j + 1):
                    mms = [(KE[:, g, kt * 128:(kt + 1) * 128], QS[qp][:, :])]
                    rd = [bK["KE"], b_QSq[qp], b_QSs[qp]]
                    if kt == j:
                        mms.append((ident[:], mkap(wbias[:, 0, 0:1], [[0, 4], [1, 128]])))
                        rd = rd + [b_c]
                    P, bP = score_tile(mms, rd)
                    for h in range(4):
                        S.mm(Os[:, h, 0:65], P[:, h * 128:(h + 1) * 128], vsA[:, kt, g * 65:(g + 1) * 65],
                             kt == 0 and h == 0, kt == j,
                             reads=[bP, bK["vsA"]], writes=[b_Os], skip_group_check=True)
                S.op("vector", lambda e: e.reciprocal(out=rz[:], in_=Os[:, :, 64]), reads=[b_Os], writes=[b_rz])
                S.tt("vector", coef[:], rz[:], mkap(gt[jp][:, 12 * g + 1:12 * g + 2], [[3, 4]]), ALU.mult,
                     reads=[b_rz, b_gt[jp]], writes=[b_coef])
                S.tt("vector", tmp[:], Os[:, :, 0:64], bc(coef[:], 64), ALU.mult,
                     reads=[b_Os, b_coef], writes=[b_tmp])
                S.tt("vector", acc[:, 4 * g:4 * g + 4, :], acc[:, 4 * g:4 * g + 4, :], tmp[:], ALU.add,
                     reads=[b_tmp], writes=[b_acc])
            if stage < 5:
                continue
            S.tt("vector", og[jp][:], acc[:].rearrange("p h d -> p (h d)"), szt[jp][:], ALU.mult,
                 reads=[b_acc, b_szt[jp]], writes=[b_og[jp]])
            S.dma("sync", [(SC["outg"][t0:t0 + 128, :], og[jp][:])], reads=[b_og[jp]])
        S.finish(nc)


EPS = 1e-6


def declare_scratch4(nc, debug):
    kind = dict(kind="ExternalOutput") if debug else {}
    dbg = debug if isinstance(debug, (set, list, tuple)) else None
    d = {}
    d["x2"] = nc.dram_tensor("x2_s", [4096, 1024], F32, **(kind if (dbg is None or "x2" in dbg) else {})).ap()
    d["h2T"] = nc.dram_tensor("h2T_s", [8, 128, 4096], BF16, **(kind if (dbg is None or "h2T" in dbg) else {})).ap()
    return d


def epilogue(nc, G, pfx, src, KW, w_out, x_in, p_in, gw_in, pw_in, x_out, ident_in, norm_g_row=None, hT_out=None, ntiles=32):
    S = Sched(G)
    KC = KW // 128
    with ExitStack() as es:
        def sb(name, shape, dt):
            return es.enter_context(nc.sbuf_tensor(pfx + name, shape, dt))

        def ps(name, shape, dt):
            return es.enter_context(nc.psum_tensor(pfx + name, shape, dt))

        wo = sb("wo", [128, KC, 1024], BF16)
        gw = sb("gw", [128, 8, 1024], BF16)
        pw = sb("pw", [128, 2, 1024], BF16)
        ident = sb("ident", [128, 128], BF16)
        gb = sb("gb", [128, 1024], F32)
        mhalf = sb("mhalf", [128, 2], F32)
        ogt = [sb(f"ogt{i}", [128, KW], BF16) for i in range(2)]
        ogT = [sb(f"ogT{i}", [128, KC, 128], BF16) for i in range(2)]
        xt = [sb(f"xt{i}", [128, 1024], F32) for i in range(2)]
        pt = [sb(f"pt{i}", [128, 256], F32) for i in range(2)]
        ptb = sb("ptb", [128, 256], BF16)
        pT = [sb(f"pT{i}", [128, 2, 128], BF16) for i in range(2)]
        x1 = [sb(f"x1{i}", [128, 1024], F32) for i in range(2)]
        x1b = sb("x1b", [128, 1024], BF16)
        x1T = [sb(f"x1T{i}", [128, 8, 128], BF16) for i in range(2)]
        th = sb("th", [128, 1024], F32)
        t2 = sb("t2", [128, 1024], F32)
        x2 = [sb(f"x2{i}", [128, 1024], F32) for i in range(2)]
        junk = sb("junk", [128, 1024], BF16)
        ssx = sb("ssx", [128, 2], F32)
        hb = sb("hb", [128, 1024], BF16)
        hTt = [sb(f"hTt{i}", [128, 8, 512], BF16) for i in range(2)]

        tp = [ps(f"tp{i}", [128, 1024], BF16) for i in range(2)]
        py = [ps(f"py{i}", [128, 512], F32) for i in range(2)]
        pg = [ps(f"pg{i}", [128, 512], F32) for i in range(2)]
        pp = [ps(f"pp{i}", [128, 512], F32) for i in range(2)]

        b_w = Buf(); b_gw = Buf(); b_pw = Buf(); b_c = Buf()
        b_ogt = [Buf(), Buf()]; b_ogT = [Buf(), Buf()]; b_xt = [Buf(), Buf()]; b_pt = [Buf(), Buf()]; b_ptb = Buf()
        b_pT = [Buf(), Buf()]; b_x1 = [Buf(), Buf()]; b_x1b = Buf(); b_x1T = [Buf(), Buf()]; b_th = Buf(); b_t2 = Buf()
        b_x2 = [Buf(), Buf()]; b_junk = Buf(); b_ssx = Buf(); b_hb = Buf(); b_hTt = [Buf(), Buf()]
        b_tp = [Buf(), Buf()]; b_py = [Buf(), Buf()]; b_pg = [Buf(), Buf()]; b_pp = [Buf(), Buf()]

        S.dma("sync", [(ident[:], ident_in)], writes=[b_c])
        if norm_g_row is not None:
            S.dma("sync", [(gb[:], norm_g_row)], writes=[b_c])
        S.memset("vector", mhalf[:], -0.5, writes=[b_c])
        for kc in range(KC):
            S.dma("gpsimd", [(wo[:, kc, :], w_out[kc * 128:(kc + 1) * 128, :])], writes=[b_w])
        for kc in range(8):
            S.dma("gpsimd", [(gw[:, kc, :], gw_in[kc * 128:(kc + 1) * 128, :])], writes=[b_gw])
        for kc in range(2):
            S.dma("gpsimd", [(pw[:, kc, :], pw_in[kc * 128:(kc + 1) * 128, :])], writes=[b_pw])

        ntp = 0
        for ti in range(ntiles):
            k = ti % 2
            r0 = ti * 128
            S.dma("sync", [(ogt[k][:], src[r0:r0 + 128, :])], writes=[b_ogt[k]])
            S.dma("sync", [(xt[k][:], x_in[r0:r0 + 128, :])], writes=[b_xt[k]])
            S.dma("sync", [(pt[k][:], p_in[r0:r0 + 128, :])], writes=[b_pt[k]])
            for c0 in range(0, KC, 8):
                q = ntp % 2; ntp += 1
                for kc in range(8):
                    S.tr(tp[q][:, kc * 128:(kc + 1) * 128], ogt[k][:, (c0 + kc) * 128:(c0 + kc + 1) * 128], ident[:],
                         reads=[b_ogt[k], b_c], writes=[b_tp[q]])
                S.copy("vector" if c0 == 0 else "scalar", ogT[k][:, c0:c0 + 8, :].rearrange("p a b -> p (a b)"), tp[q][:],
                       reads=[b_tp[q]], writes=[b_ogT[k]])
            S.copy("gpsimd", ptb[:], pt[k][:], reads=[b_pt[k]], writes=[b_ptb])
            q = ntp % 2; ntp += 1
            for c in range(2):
                S.tr(tp[q][:, c * 128:(c + 1) * 128], ptb[:, c * 128:(c + 1) * 128], ident[:],
                     reads=[b_ptb, b_c], writes=[b_tp[q]])
            S.copy("vector", pT[k][:].rearrange("p a b -> p (a b)"), tp[q][:, 0:256], reads=[b_tp[q]], writes=[b_pT[k]])
            for nb in range(2):
                for kc in range(KC):
                    S.mm(py[nb][:], ogT[k][:, kc, :], wo[:, kc, nb * 512:(nb + 1) * 512], kc == 0, kc == KC - 1,
                         reads=[b_ogT[k], b_w], writes=[b_py[nb]])
                S.tt("vector", x1[k][:, nb * 512:(nb + 1) * 512], py[nb][:], xt[k][:, nb * 512:(nb + 1) * 512], ALU.add,
                     reads=[b_py[nb], b_xt[k]], writes=[b_x1[k]])
            S.copy("scalar", x1b[:], x1[k][:], reads=[b_x1[k]], writes=[b_x1b])
            q = ntp % 2; ntp += 1
            for kc in range(8):
                S.tr(tp[q][:, kc * 128:(kc + 1) * 128], x1b[:, kc * 128:(kc + 1) * 128], ident[:],
                     reads=[b_x1b, b_c], writes=[b_tp[q]])
            S.copy("scalar", x1T[k][:].rearrange("p a b -> p (a b)"), tp[q][:], reads=[b_tp[q]], writes=[b_x1T[k]])
            for nb in range(2):
                for kc in range(8):
                    S.mm(pg[nb][:], x1T[k][:, kc, :], gw[:, kc, nb * 512:(nb + 1) * 512], kc == 0, kc == 7,
                         reads=[b_x1T[k], b_gw], writes=[b_pg[nb]])
                for c in range(2):
                    S.mm(pp[nb][:], pT[k][:, c, :], pw[:, c, nb * 512:(nb + 1) * 512], c == 0, c == 1,
                         reads=[b_pT[k], b_pw], writes=[b_pp[nb]])
                sl = slice(nb * 512, (nb + 1) * 512)
                S.act(th[:, sl], pg[nb][:], AF.Tanh, reads=[b_pg[nb]], writes=[b_th], scale=0.5)
                S.stt(t2[:, sl], th[:, sl], 1.0, pp[nb][:], ALU.add, ALU.mult, reads=[b_th, b_pp[nb]], writes=[b_t2])
            S.stt(x2[k][:], t2[:], 0.5, x1[k][:], ALU.mult, ALU.add, reads=[b_t2, b_x1[k]], writes=[b_x2[k]])
            S.dma("sync", [(x_out[r0:r0 + 128, :], x2[k][:])], reads=[b_x2[k]])
            if hT_out is not None:
                tg, tl = divmod(ti, 4)
                sp = tg % 2
                S.act(junk[:], x2[k][:], AF.Square, reads=[b_x2[k]], writes=[b_junk, b_ssx], accum_out=ssx[:, 0:1])
                S.ts("vector", ssx[:, 0:1], ssx[:, 0:1], 1.0 / 1024, EPS, ALU.mult, ALU.add, reads=[b_ssx], writes=[b_ssx])
                S.tt("gpsimd", ssx[:, 1:2], ssx[:, 0:1], mhalf[:, 0:1], ALU.pow, reads=[b_ssx, b_c], writes=[b_ssx])
                S.stt(hb[:], x2[k][:], ssx[:, 1:2], gb[:], ALU.mult, ALU.mult, reads=[b_x2[k], b_ssx, b_c], writes=[b_hb])
                q = ntp % 2; ntp += 1
                for kc in range(8):
                    S.tr(tp[q][:, kc * 128:(kc + 1) * 128], hb[:, kc * 128:(kc + 1) * 128], ident[:],
                         reads=[b_hb, b_c], writes=[b_tp[q]])
                S.copy("scalar", hTt[sp][:, :, tl * 128:(tl + 1) * 128], tp[q][:].rearrange("p (a b) -> p a b", b=128),
                       reads=[b_tp[q]], writes=[b_hTt[sp]])
                if tl == 3:
                    S.dma("sync", [(hT_out.rearrange("k p t -> p k t")[:, :, tg * 512:(tg + 1) * 512], hTt[sp][:])],
                          reads=[b_hTt[sp]])
        S.finish(nc)


GN_EPS = 1e-5
RET_COLS = 6144


def declare_scratch5(nc, debug):
    kind = dict(kind="ExternalOutput") if debug else {}
    dbg = debug if isinstance(debug, (set, list, tuple)) else None
    d = {}
    d["og"] = nc.dram_tensor("og_s", [4096, 2048], BF16, **(kind if (dbg is None or "og" in dbg) else {})).ap()
    return d


def phase5(nc, G, I, SC, chunk_decay, ntiles=32):
    S = Sched(G)
    with ExitStack() as es:
        def sb(name, shape, dt):
            return es.enter_context(nc.sbuf_tensor("p5_" + name, shape, dt))

        def ps(name, shape, dt):
            return es.enter_context(nc.psum_tensor("p5_" + name, shape, dt))

        wr = sb("wr", [128, 8, RET_COLS], BF16)
        ident = sb("ident", [128, 128], BF16)
        decT = sb("decT", [128, 4, 128], F32)
        qdT = sb("qdT", [128, 4, 128], F32)
        kdec = sb("kdec", [128, 4], F32)
        mhalf = sb("mhalf", [128, 4], F32)
        st32 = sb("st32", [128, 4, 2, 512], F32)
        stb = sb("stb", [128, 4, 2, 512], BF16)
        hT = [sb(f"hT{i}", [128, 8, 128], BF16) for i in range(2)]
        rope = [sb(f"rope{i}", [128, 4, 128], F32) for i in range(2)]
        ra = sb("ra", [128, 2, 128], F32)
        rb = sb("rb", [128, 2, 128], F32)
        qr = sb("qr", [128, 4, 256], BF16)
        kr = sb("kr", [128, 4, 256], BF16)
        kd = sb("kd", [128, 4, 256], BF16)
        qT = sb("qT", [128, 4, 2, 128], BF16)
        qsT = sb("qsT", [128, 4, 2, 128], BF16)
        kT = sb("kT", [128, 4, 2, 128], BF16)
        vt = sb("vt", [128, 4, 512], BF16)
        zt = sb("zt", [128, 512], F32)
        zh = sb("zh", [128, 512], F32)
        szt = sb("szt", [128, 2048], BF16)
        attd = sb("attd", [128, 4, 128], BF16)
        o32 = sb("o32", [128, 4, 512], F32)
        junk = sb("junk", [128, 512], BF16)
        st = sb("st", [128, 8], F32)
        mv = sb("mv", [128, 16], F32)
        nbias = sb("nbias", [128, 4], F32)
        ogt = [sb(f"ogt{i}", [128, 2048], BF16) for i in range(2)]

        pj = [ps(f"pj{i}", [128, 512], F32) for i in range(2)]
        tp = ps("tp", [128, 1024], BF16)
        pa = ps("pa", [128, 4, 128], F32)
        po = [ps(f"po{i}", [128, 512], F32) for i in range(2)]
        pu = [ps(f"pu{i}", [128, 512], F32) for i in range(2)]

        b_w = Buf(); b_c = Buf(); b_st32 = Buf(); b_stb = Buf()
        b_hT = [Buf(), Buf()]; b_rope = [Buf(), Buf()]; b_ra = Buf(); b_rb = Buf()
        b_qr = Buf(); b_kr = Buf(); b_kd = Buf(); b_qT = Buf(); b_qsT = Buf(); b_kT = Buf(); b_vt = Buf()
        b_zt = Buf(); b_zh = Buf(); b_szt = Buf(); b_attd = Buf(); b_o32 = Buf(); b_junk = Buf()
        b_st = Buf(); b_mv = Buf(); b_nb = Buf(); b_ogt = [Buf(), Buf()]
        b_pj = [Buf(), Buf()]; b_tp = Buf(); b_pa = Buf(); b_po = [Buf(), Buf()]; b_pu = [Buf(), Buf()]

        S.dma("sync", [(ident[:], I["ident"])], writes=[b_c])
        S.dma("sync", [(decT[:], I["decT"])], writes=[b_c])
        S.dma("sync", [(qdT[:], I["qdT"])], writes=[b_c])
        S.dma("sync", [(kdec[:], I["kdec"])], writes=[b_c])
        S.memset("vector", mhalf[:], -0.5, writes=[b_c])
        S.memset("vector", st32[:], 0.0, writes=[b_st32])
        S.memset("vector", stb[:], 0.0, writes=[b_stb])
        for kc in range(8):
            S.dma("gpsimd", [(wr[:, kc, c0:c0 + 1536], I["ret_w_in"][kc * 128:(kc + 1) * 128, c0:c0 + 1536])
                             for c0 in range(0, RET_COLS, 1536)], writes=[b_w])

        nseg = 0
        npo = 0
        for ti in range(ntiles):
            k = ti % 2
            r0 = ti * 128
            S.dma("sync", [(hT[k][:], SC["h2T"][:, :, r0:r0 + 128].rearrange("k p t -> p k t"))], writes=[b_hT[k]])
            S.dma("sync", [(rope[k][:], I["rope"][r0:r0 + 128])], writes=[b_rope[k]])

            def proj(c0):
                nonlocal nseg
                pb = nseg % 2; nseg += 1
                for kc in range(8):
                    S.mm(pj[pb][:], hT[k][:, kc, :], wr[:, kc, c0:c0 + 512], kc == 0, kc == 7,
                         reads=[b_hT[k], b_w], writes=[b_pj[pb]])
                return pj[pb], b_pj[pb]

            for which in range(2):
                dst, b_dst = (qr, b_qr) if which == 0 else (kr, b_kr)
                cs, sn = (rope[k][:, 0, :], rope[k][:, 1, :]) if which == 0 else (rope[k][:, 2, :], rope[k][:, 3, :])
                csb = mkap(cs, [[0, 2], [1, 128]]); snb = mkap(sn, [[0, 2], [1, 128]])
                for half in range(2):
                    P, bP = proj(which * 1024 + half * 512)
                    x1 = mkap(P[:, 0:1], [[256, 2], [1, 128]])
                    x2 = mkap(P[:, 128:129], [[256, 2], [1, 128]])
                    o1 = dst[:, half * 2:half * 2 + 2, 0:128]
                    o2 = dst[:, half * 2:half * 2 + 2, 128:256]
                    S.tt("vector", ra[:], x1, csb, ALU.mult, reads=[bP, b_rope[k]], writes=[b_ra])
                    S.tt("vector", rb[:], x2, snb, ALU.mult, reads=[bP, b_rope[k]], writes=[b_rb])
                    S.tt("vector", o1, ra[:], rb[:], ALU.subtract, reads=[b_ra, b_rb], writes=[b_dst])
                    S.tt("vector", ra[:], x1, snb, ALU.mult, reads=[bP, b_rope[k]], writes=[b_ra])
                    S.tt("vector", rb[:], x2, csb, ALU.mult, reads=[bP, b_rope[k]], writes=[b_rb])
                    S.tt("vector", o2, ra[:], rb[:], ALU.add, reads=[b_ra, b_rb], writes=[b_dst])
            for h in range(4):
                S.ts("gpsimd", kd[:, h, :], kr[:, h, :], kdec[:, h:h + 1], None, ALU.mult, reads=[b_kr, b_c], writes=[b_kd])
            for which in range(2):
                src, b_src = (qr, b_qr) if which == 0 else (kr, b_kr)
                for h in range(4):
                    for dc in range(2):
                        S.tr(tp[:, (h * 2 + dc) * 128:(h * 2 + dc + 1) * 128], src[:, h, dc * 128:(dc + 1) * 128], ident[:],
                             reads=[b_src, b_c], writes=[b_tp])
                if which == 0:
                    S.copy("scalar", qT[:].rearrange("p h c t -> p (h c t)"), tp[:], reads=[b_tp], writes=[b_qT])
                    S.tt("vector", qsT[:], tp[:].rearrange("p (h c t) -> p h c t", h=4, c=2),
                         mkap(qdT[:, 0, 0:1], [[128, 4], [0, 2], [1, 128]]),
                         ALU.mult, reads=[b_tp, b_c], writes=[b_qsT])
                else:
                    S.copy("scalar", kT[:].rearrange("p h c t -> p (h c t)"), tp[:], reads=[b_tp], writes=[b_kT])
            for h in range(4):
                P, bP = proj(2048 + h * 512)
                S.copy("scalar", vt[:, h, :], P[:], reads=[bP], writes=[b_vt])
            for zc in range(4):
                P, bP = proj(4096 + zc * 512)
                S.act(zt[:], P[:], AF.Tanh, reads=[bP], writes=[b_zt], scale=0.5)
                S.act(zh[:], P[:], AF.Copy, reads=[bP], writes=[b_zh], scale=0.5)
                S.stt(szt[:, zc * 512:(zc + 1) * 512], zt[:], 1.0, zh[:], ALU.add, ALU.mult,
                      reads=[b_zt, b_zh], writes=[b_szt])
            for h in range(4):
                for dc in range(2):
                    S.mm(pa[:, h, :], kT[:, h, dc, :], qT[:, h, dc, :], h == 0 and dc == 0, dc == 1,
                         reads=[b_kT, b_qT], writes=[b_pa], skip_group_check=True)
            S.tt("vector", attd[:], pa[:], decT[:], ALU.mult, reads=[b_pa, b_c], writes=[b_attd])
            for h in range(4):
                ob = npo % 2; npo += 1
                S.mm(po[ob][:], attd[:, h, :], vt[:, h, :], True, False, reads=[b_attd, b_vt], writes=[b_po[ob]])
                for dc in range(2):
                    S.mm(po[ob][:], qsT[:, h, dc, :], stb[:, h, dc, :], False, dc == 1,
                         reads=[b_qsT, b_stb], writes=[b_po[ob]])
                for dc in range(2):
                    S.mm(pu[dc][:], kd[:, h, dc * 128:(dc + 1) * 128], vt[:, h, :], True, True,
                         reads=[b_kd, b_vt], writes=[b_pu[dc]])
                    S.stt(st32[:, h, dc, :], st32[:, h, dc, :], float(chunk_decay[h]), pu[dc][:], ALU.mult, ALU.add,
                          reads=[b_pu[dc]], writes=[b_st32])
                S.copy("gpsimd", stb[:, h, :, :], st32[:, h, :, :], reads=[b_st32], writes=[b_stb])
                S.act(o32[:, h, :], po[ob][:], AF.Copy, reads=[b_po[ob]], writes=[b_o32, b_st], accum_out=st[:, 2 * h:2 * h + 1])
                S.act(junk[:], po[ob][:], AF.Square, reads=[b_po[ob]], writes=[b_junk, b_st], accum_out=st[:, 2 * h + 1:2 * h + 2])
            sv = st[:].rearrange("p (h two) -> p h two", two=2)
            S.ts("vector", mv[:, 0:4], sv[:, :, 0], 1.0 / 512, None, ALU.mult, reads=[b_st], writes=[b_mv])
            S.ts("vector", mv[:, 4:8], sv[:, :, 1], 1.0 / 512, None, ALU.mult, reads=[b_st], writes=[b_mv])
            S.tt("vector", mv[:, 8:12], mv[:, 0:4], mv[:, 0:4], ALU.mult, reads=[b_mv], writes=[b_mv])
            S.tt("vector", mv[:, 8:12], mv[:, 4:8], mv[:, 8:12], ALU.subtract, reads=[b_mv], writes=[b_mv])
            S.ts("vector", mv[:, 8:12], mv[:, 8:12], GN_EPS, None, ALU.add, reads=[b_mv], writes=[b_mv])
            S.tt("gpsimd", mv[:, 12:16], mv[:, 8:12], mhalf[:], ALU.pow, reads=[b_mv, b_c], writes=[b_mv])
            S.stt(nbias[:], mv[:, 0:4], -1.0, mv[:, 12:16], ALU.mult, ALU.mult, reads=[b_mv], writes=[b_nb])
            for h in range(4):
                S.act(o32[:, h, :], o32[:, h, :], AF.Identity, reads=[b_mv, b_nb], writes=[b_o32],
                      scale=mv[:, 12 + h:13 + h], bias=nbias[:, h:h + 1])
                S.tt("gpsimd" if h % 2 else "vector", ogt[k][:, h * 512:(h + 1) * 512], o32[:, h, :], szt[:, h * 512:(h + 1) * 512],
                     ALU.mult, reads=[b_o32, b_szt], writes=[b_ogt[k]])
            S.dma("sync", [(SC["og"][r0:r0 + 128, :], ogt[k][:])], reads=[b_ogt[k]])
        S.finish(nc)


from concourse.bass_utils import run_bass_kernel_spmd

W_NAMES = ["norm_g", "nsa_w_in", "nsa_q_g", "nsa_kc_g", "nsa_ks_g", "nsa_kw_g", "nsa_cmp_pos_k", "nsa_cmp_pos_v",
           "nsa_cmp_k_w1", "nsa_cmp_k_w2", "nsa_cmp_v_w1", "nsa_cmp_v_w2", "nsa_w_out", "ret_w_in", "ret_w_out"]
_CACHE = {}


def build(debug=False):
    nc = bass.Bass("TRN2", target_bir_lowering=False)
    I = {}

    def din(name, shape, dt=F32):
        I[name] = nc.dram_tensor(name, list(shape), dt, kind="ExternalInput").ap()

    din("x", [4096, 1024]); din("p0", [4096, 256]); din("p1", [4096, 256]); din("norm_g", [2, 1024])
    din("nsa_w_in", [1024, 3632])
    for nm in ["nsa_q_g", "nsa_ks_g", "nsa_kw_g", "nsa_kc_g"]:
        din(nm, [1, 64])
    din("nsa_cmp_k_w1", [2048, 256]); din("nsa_cmp_v_w1", [2048, 256]); din("nsa_cmp_k_w2", [256, 64]); din("nsa_cmp_v_w2", [256, 64])
    din("nsa_cmp_pos_k", [32, 64]); din("nsa_cmp_pos_v", [32, 64])
    din("nsa_w_out", [1024, 1024]); din("ret_w_in", [1024, 6144]); din("ret_w_out", [2048, 1024])
    din("ple_w0", [256, 1024]); din("ple_w1", [256, 1024]); din("ple_gate_w0", [1024, 1024]); din("ple_gate_w1", [1024, 1024])
    din("ident", [128, 128], BF16); din("overlap", [256, 64]); din("Eexp", [64, 4096], BF16); din("Ftab", [128, 32, 64])
    din("cmpb", [32, 2, 128, 128], BF16); din("wbias", [2, 128, 128], BF16)
    din("decT", [128, 4, 128]); din("qdT", [128, 4, 128]); din("kdec", [128, 4]); din("rope", [4096, 4, 128])
    out = nc.dram_tensor("out", [4096, 1024], F32, kind="ExternalOutput").ap()
    SC = declare_scratch(nc, debug); SC.update(declare_scratch2(nc, debug)); SC.update(declare_scratch3(nc, debug))
    SC.update(declare_scratch4(nc, debug)); SC.update(declare_scratch5(nc, debug))
    C = make_consts()
    with ExitStack() as es:
        G = Glob(nc, es)
        phase1(nc, G, I, SC)
        phase2(nc, G, I, SC)
        phase3(nc, G, I, SC)
        g1 = bass.AP(I["norm_g"].tensor, 1024, [[0, 128], [1, 1024]])
        epilogue(nc, G, "e0_", SC["outg"], 1024, I["nsa_w_out"], I["x"], I["p0"], I["ple_gate_w0"], I["ple_w0"],
                 SC["x2"], I["ident"], norm_g_row=g1, hT_out=SC["h2T"])
        phase5(nc, G, I, SC, C["chunk_decay"])
        epilogue(nc, G, "e1_", SC["og"], 2048, I["ret_w_out"], SC["x2"], I["p1"], I["ple_gate_w1"], I["ple_w1"],
                 out, I["ident"])
    return nc


def make_in_maps(inputs, cores):
    C = make_consts()
    shared = {}
    f32 = lambda a: np.ascontiguousarray(np.asarray(a, dtype=np.float32))
    shared["norm_g"] = f32(inputs["norm_g"])
    for nm in ["nsa_w_in", "nsa_q_g", "nsa_kc_g", "nsa_ks_g", "nsa_kw_g", "nsa_cmp_pos_k", "nsa_cmp_pos_v",
               "nsa_cmp_k_w1", "nsa_cmp_k_w2", "nsa_cmp_v_w1", "nsa_cmp_v_w2", "nsa_w_out", "ret_w_in", "ret_w_out"]:
        a = f32(inputs[nm])[0]
        if a.ndim == 1:
            a = a[None, :]
        shared[nm] = np.ascontiguousarray(a)
    for l in range(2):
        shared[f"ple_w{l}"] = f32(inputs["ple_w"])[l]
        shared[f"ple_gate_w{l}"] = f32(inputs["ple_gate_w"])[l]
    for k in ["ident", "overlap", "Eexp", "Ftab", "cmpb", "wbias", "decT", "qdT", "kdec", "rope"]:
        shared[k] = C[k]
    x = f32(inputs["x"]); p = f32(inputs["p"])
    maps = []
    for b in cores:
        m = dict(shared)
        m["x"] = x[b]; m["p0"] = p[0, b]; m["p1"] = p[1, b]
        maps.append(m)
    return maps


def kernel(**inputs):
    if "nc" not in _CACHE:
        _CACHE["nc"] = build()
    nc = _CACHE["nc"]
    maps = make_in_maps(inputs, list(range(8)))
    res = run_bass_kernel_spmd(nc, maps, core_ids=list(range(8)))
    return np.stack([np.asarray(r["out"], dtype=np.float32) for r in res.results], axis=0)
```

```python
import numpy as np
import ml_dtypes
import concourse.bass as bass
import concourse.mybir as mybir
from contextlib import ExitStack

F32 = mybir.dt.float32
BF16 = mybir.dt.bfloat16
AF = mybir.ActivationFunctionType
ALU = mybir.AluOpType
AX = mybir.AxisListType
ENGS = ("sync", "scalar", "vector", "gpsimd", "tensor")
SKIP_SAME = {"tensor"}


class Buf:
    __slots__ = ("w", "r", "name", "dsem")

    def __init__(self, name=""):
        self.w = None
        self.r = {}
        self.name = name
        self.dsem = None


class Glob:
    def __init__(self, nc, es, n_dma_sems=96):
        self.nc = nc
        self.esem = {e: es.enter_context(nc.semaphore("es_" + e)) for e in ENGS}
        self.cnt = {e: 0 for e in ENGS}
        self.dsems = [es.enter_context(nc.semaphore(f"ds{i}")) for i in range(n_dma_sems)]
        self.dcnt = [0] * n_dma_sems
        self.next_dsem = 0
        self.seen = {e: {} for e in ENGS}

    def sem_of(self, key):
        if isinstance(key, str):
            return self.esem[key]
        return self.dsems[key]

    def alloc_dsem(self):
        i = self.next_dsem
        assert i < len(self.dsems), "out of dma semaphores"
        self.next_dsem += 1
        return i


class Sched:
    def __init__(self, G):
        self.G = G
        G.next_dsem = 0
        self.q = {e: [] for e in ENGS}
        self.pending_dma = []

    def _wait(self, eng, tok):
        if tok is None:
            return
        key, val = tok
        if eng == key and eng in SKIP_SAME:
            return
        seen = self.G.seen[eng]
        if seen.get(key, 0) >= val:
            return
        seen[key] = val
        sem = self.G.sem_of(key)
        self.q[eng].append(lambda e, sem=sem, val=val: e.wait_ge(sem, val))

    def _deps(self, eng, reads, writes, extra):
        for b in reads:
            self._wait(eng, b.w)
        for b in writes:
            self._wait(eng, b.w)
            for k, v in b.r.items():
                self._wait(eng, (k, v))
        for t in extra:
            self._wait(eng, t)

    @staticmethod
    def _mark(tok, reads, writes):
        k, v = tok
        for b in reads:
            if b.r.get(k, 0) < v:
                b.r[k] = v
        for b in writes:
            b.w = tok
            b.r = {}

    def op(self, eng, fn, reads=(), writes=(), extra=()):
        G = self.G
        self._deps(eng, reads, writes, extra)
        G.cnt[eng] += 1
        tok = (eng, G.cnt[eng])
        sem = G.esem[eng]
        self.q[eng].append(lambda e, fn=fn, sem=sem: fn(e).then_inc(sem, 1))
        self._mark(tok, reads, writes)
        return tok

    def dma(self, q, pairs, reads=(), writes=(), extra=(), **kw):
        G = self.G
        bufs = list(writes) + list(reads)
        b0 = bufs[0]
        if b0.dsem is None:
            b0.dsem = G.alloc_dsem()
        si = b0.dsem
        self._deps(q, reads, writes, extra)
        sem = G.dsems[si]
        for (o, i) in pairs:
            G.dcnt[si] += 16
            self.q[q].append(lambda e, o=o, i=i, sem=sem, kw=kw: e.dma_start(out=o, in_=i, **kw).then_inc(sem, 16))
        tok = (si, G.dcnt[si])
        self._mark(tok, reads, writes)
        self.pending_dma.append(tok)
        return tok

    def finish(self, nc):
        for t in self.pending_dma:
            self._wait("sync", t)
        with nc.Block() as block:
            for e in ENGS:
                lst = self.q[e]

                def body(eng, lst=lst):
                    for f in lst:
                        f(eng)
                getattr(block, e)(body)

    def act(self, out, in_, func, reads=(), writes=(), eng="scalar", **kw):
        return self.op(eng, lambda e: e.activation(out=out, in_=in_, func=func, **kw), reads, writes)

    def mm(self, out, lhsT, rhs, start, stop, reads=(), writes=(), **kw):
        return self.op("tensor", lambda e: e.matmul(out, lhsT, rhs, start=start, stop=stop, **kw), reads, writes)

    def tr(self, out, in_, ident, reads=(), writes=()):
        return self.op("tensor", lambda e: e.transpose(out, in_, ident), reads, writes)

    def tt(self, eng, out, in0, in1, op, reads=(), writes=()):
        return self.op(eng, lambda e: e.tensor_tensor(out=out, in0=in0, in1=in1, op=op), reads, writes)

    def ts(self, eng, out, in0, s1, s2, op0, op1=None, reads=(), writes=(), **kw):
        if op1 is None:
            return self.op(eng, lambda e: e.tensor_scalar(out=out, in0=in0, scalar1=s1, scalar2=None, op0=op0, **kw), reads, writes)
        return self.op(eng, lambda e: e.tensor_scalar(out=out, in0=in0, scalar1=s1, scalar2=s2, op0=op0, op1=op1, **kw), reads, writes)

    def stt(self, out, in0, scalar, in1, op0, op1, reads=(), writes=(), eng="vector"):
        return self.op(eng, lambda e: e.scalar_tensor_tensor(out=out, in0=in0, scalar=scalar, in1=in1, op0=op0, op1=op1), reads, writes)

    def copy(self, eng, out, in_, reads=(), writes=()):
        if eng == "scalar":
            return self.op(eng, lambda e: e.copy(out=out, in_=in_), reads, writes)
        return self.op(eng, lambda e: e.tensor_copy(out=out, in_=in_), reads, writes)

    def memset(self, eng, ap, val, writes=()):
        return self.op(eng, lambda e: e.memset(ap, val), (), writes)

    def reduce(self, out, in_, op, axis, reads=(), writes=(), eng="vector"):
        return self.op(eng, lambda e: e.tensor_reduce(out=out, in_=in_, axis=axis, op=op), reads, writes)


def bc(ap, n):
    return bass.AP(ap.tensor, ap.offset, [list(x) for x in ap.ap] + [[0, n]])


def mkap(ap, dims):
    return bass.AP(ap.tensor, ap.offset, [list(ap.ap[0])] + [list(d) for d in dims])


BIG = 30000.0
def make_consts():
    c = {}
    c["ident"] = np.eye(128, dtype=ml_dtypes.bfloat16)
    i = np.arange(256)[:, None]; j = np.arange(64)[None, :]
    ov = ((i * 16 < (j + 1) * 64) & (i * 16 + 32 > j * 64)).astype(np.float32)
    ov[255] = 0
    c["overlap"] = ov
    E = (np.arange(4096)[None, :] // 64 == np.arange(64)[:, None]).astype(np.float32)
    c["Eexp"] = E.astype(ml_dtypes.bfloat16)
    t = (np.arange(32)[None, :, None] * 128 + np.arange(128)[:, None, None])
    cur = t // 64
    b = np.arange(64)[None, None, :]
    valid = b <= cur
    forced = (b == 0) | (b == cur) | (b == cur - 1)
    F = np.where(valid, np.where(forced, 1e4, 0.0), -1e30).astype(np.float32)
    c["Ftab"] = np.ascontiguousarray(F)
    n = (np.arange(2)[None, :, None, None] * 128 + np.arange(128)[None, None, :, None])
    tt = (np.arange(32)[:, None, None, None] * 128 + np.arange(128)[None, None, None, :])
    cb = np.where(16 * n + 31 > tt, -BIG, 0.0).astype(np.float32)
    c["cmpb"] = cb.astype(ml_dtypes.bfloat16)
    ii = np.arange(128)[:, None]; uu = np.arange(128)[None, :]
    wb = np.stack([np.where(ii > uu, -BIG, 0.0), np.where(ii <= uu, -BIG, 0.0)]).astype(np.float32)
    c["wbias"] = wb.astype(ml_dtypes.bfloat16)
    H, C = 4, 128
    log_g = np.log(1.0 - 2.0 ** (-5.0 - np.arange(H, dtype=np.float64)))
    ix = np.arange(C, dtype=np.float64)
    diff = ix[:, None] - ix[None, :]
    intra = np.where(diff >= 0, np.exp(log_g[:, None, None] * np.maximum(diff, 0.0)), 0.0)
    c["decT"] = np.ascontiguousarray(intra.transpose(2, 0, 1)).astype(np.float32)
    q_decay = np.exp(log_g[:, None] * (ix + 1.0))
    c["qdT"] = np.ascontiguousarray(np.broadcast_to(q_decay[None], (128, H, C))).astype(np.float32)
    k_decay = np.exp(log_g[:, None] * (C - 1.0 - ix))
    c["kdec"] = np.ascontiguousarray(k_decay.T).astype(np.float32)
    c["chunk_decay"] = np.exp(log_g * C)
    half = 128
    inv = (np.float32(10000.0) ** (-np.linspace(0.0, 1.0, half, dtype=np.float32))).astype(np.float32)
    pos = np.arange(4096, dtype=np.float32)
    ang = (pos[:, None] * inv[None, :]).astype(np.float32).astype(np.float64)
    cs, sn = np.cos(ang), np.sin(ang)
    c["rope"] = np.ascontiguousarray(np.stack([cs, sn, cs / 16.0, sn / 16.0], axis=1)).astype(np.float32)
    return c


S_, D_ = 4096, 1024
NT = 32
EPS = 1e-6
NSA_COLS = 3632


def declare_scratch(nc, debug):
    kind = dict(kind="ExternalOutput") if debug else {}
    dbg = debug if isinstance(debug, (set, list, tuple)) else None
    d = {}
    d["qT"] = nc.dram_tensor("qT_s", [16, 64, S_], BF16, **(kind if (dbg is None or "qT" in dbg) else {})).ap()
    d["kvT"] = nc.dram_tensor("kvT_s", [16, 64, S_], BF16, **(kind if (dbg is None or "kvT" in dbg) else {})).ap()
    d["vsA"] = nc.dram_tensor("vsA_s", [NT, 128, 260], BF16, **(kind if (dbg is None or "vsA" in dbg) else {})).ap()
    d["vwA"] = nc.dram_tensor("vwA_s", [NT, 128, 260], BF16, **(kind if (dbg is None or "vwA" in dbg) else {})).ap()
    d["gates"] = nc.dram_tensor("gates_s", [S_, 48], F32, **(kind if (dbg is None or "gates" in dbg) else {})).ap()
    d["sz"] = nc.dram_tensor("sz_s", [S_, 1024], BF16, **(kind if (dbg is None or "sz" in dbg) else {})).ap()
    return d


def phase1(nc, G, I, SC, ntg=8):
    S = Sched(G)
    with ExitStack() as es:
        def sb(name, shape, dt):
            return es.enter_context(nc.sbuf_tensor("p1_" + name, shape, dt))

        def ps(name, shape, dt):
            return es.enter_context(nc.psum_tensor("p1_" + name, shape, dt))

        w_sb = sb("w_sb", [128, 8, NSA_COLS], BF16)
        gb = sb("gb", [128, 1024], F32)
        gcol = sb("gcol", [64, 4], F32)
        ident = sb("ident", [128, 128], BF16)
        mhalf = sb("mhalf", [128, 8], F32)
        xt = [sb(f"xt{i}", [128, 1024], F32) for i in range(2)]
        junk = sb("junk", [128, 1024], BF16)
        ssx = [sb(f"ssx{i}", [128, 2], F32) for i in range(2)]
        hb = [sb(f"hb{i}", [128, 1024], BF16) for i in range(2)]
        hT = [sb(f"hT{i}", [128, 8, 128], BF16) for i in range(2)]
        sq = [sb(f"sq{i}", [128, 512], F32) for i in range(2)]
        qf = [sb(f"qf{i}", [128, 512], F32) for i in range(2)]
        ss = [sb(f"ss{i}", [128, 8], F32) for i in range(2)]
        rs = [sb(f"rs{i}", [128, 8], F32) for i in range(2)]
        qn = [sb(f"qn{i}", [128, 512], BF16) for i in range(4)]
        zt = [sb(f"zt{i}", [128, 512], F32) for i in range(2)]
        zh = [sb(f"zh{i}", [128, 512], F32) for i in range(2)]
        gtmp = sb("gtmp", [128, 48], F32)
        qTt = [sb(f"qTt{i}", [64, 16, 512], BF16) for i in range(2)]
        kvTt = [sb(f"kvTt{i}", [64, 16, 512], BF16) for i in range(2)]
        vsAt = [sb(f"vsAt{i}", [128, 4, 260], BF16) for i in range(2)]
        vwAt = [sb(f"vwAt{i}", [128, 4, 260], BF16) for i in range(2)]
        szt = [sb(f"szt{i}", [128, 4, 1024], BF16) for i in range(2)]
        gt = [sb(f"gt{i}", [128, 4, 48], F32) for i in range(2)]

        pj = [ps(f"pj{i}", [128, 512], F32) for i in range(4)]
        tpa = ps("tpa", [128, 1024], BF16)
        tpq = [ps(f"tpq{i}", [64, 1024], BF16) for i in range(2)]

        B = lambda n: Buf(n)
        b_w = B("w"); b_c = B("consts")
        b_xt = [B("xt") for _ in range(2)]; b_junk = B("junk"); b_ssx = [B("ssx") for _ in range(2)]
        b_hb = [B("hb") for _ in range(2)]; b_hT = [B("hT") for _ in range(2)]
        b_sq = [B("sq") for _ in range(2)]; b_qf = [B("qf") for _ in range(2)]; b_ss = [B("ss") for _ in range(2)]; b_rs = [B("rs") for _ in range(2)]
        b_qn = [B("qn") for _ in range(4)]; b_zt = [B("zt") for _ in range(2)]; b_zh = [B("zh") for _ in range(2)]
        b_gtmp = B("gtmp")
        b_qTt = [B("qTt") for _ in range(2)]; b_kvTt = [B("kvTt") for _ in range(2)]
        b_vsAt = [B("vsAt") for _ in range(2)]; b_vwAt = [B("vwAt") for _ in range(2)]
        b_szt = [B("szt") for _ in range(2)]; b_gt = [B("gt") for _ in range(2)]
        b_pj = [B("pj") for _ in range(4)]; b_tpa = B("tpa"); b_tpq = [B("tpq") for _ in range(2)]

        S.dma("sync", [(ident[:], I["ident"])], writes=[b_c])
        S.dma("sync", [(gb[:], bass.AP(I["norm_g"].tensor, 0, [[0, 128], [1, 1024]]))], writes=[b_c])
        for j, nm in enumerate(["nsa_q_g", "nsa_ks_g", "nsa_kw_g"]):
            S.dma("sync", [(gcol[:, j:j + 1], bass.AP(I[nm].tensor, 0, [[1, 64], [1, 1]]))], writes=[b_c])
        S.ts("vector", gcol[:, 0:1], gcol[:, 0:1], 0.125, None, ALU.mult, reads=[b_c], writes=[b_c])
        S.memset("vector", mhalf[:], -0.5, writes=[b_c])
        for i in range(2):
            S.memset("vector", vsAt[i][:], 1.0, writes=[b_vsAt[i]])
            S.memset("vector", vwAt[i][:], 1.0, writes=[b_vwAt[i]])
        half = NSA_COLS // 2
        b_wk = [Buf() for _ in range(8)]
        wt = []
        for kc in range(8):
            wt.append(S.dma("gpsimd", [(w_sb[:, kc, c0:c0 + half], I["nsa_w_in"][kc * 128:(kc + 1) * 128, c0:c0 + half])
                                       for c0 in (0, half)], writes=[b_wk[kc]], extra=wt[-2:-1] if len(wt) >= 2 else ()))

        nseg = 0
        tq = 0
        nqn = 0
        defer = []

        def flush(keep):
            while len(defer) > keep:
                defer.pop(0)()

        def norm_heads(pj_ap, b_pjn, nh, out_ap3, b_out):
            nonlocal nseg
            k = nseg % 2
            nseg += 1
            n = nh * 64
            S.act(sq[k][:, 0:n], pj_ap, AF.Square, reads=[b_pjn], writes=[b_sq[k]])
            S.act(qf[k][:, 0:n], pj_ap, AF.Copy, reads=[b_pjn], writes=[b_qf[k]])
            S.reduce(ss[k][:, 0:nh], sq[k][:, 0:n].rearrange("p (h d) -> p h d", d=64), ALU.add, AX.X,
                     reads=[b_sq[k]], writes=[b_ss[k]])
            S.ts("vector", ss[k][:, 0:nh], ss[k][:, 0:nh], 1.0 / 64, EPS, ALU.mult, ALU.add,
                 reads=[b_ss[k]], writes=[b_ss[k]])
            S.tt("gpsimd", rs[k][:, 0:nh], ss[k][:, 0:nh], mhalf[:, 0:nh], ALU.pow,
                 reads=[b_ss[k], b_c], writes=[b_rs[k]])
            S.tt("vector", out_ap3, qf[k][:, 0:n].rearrange("p (h d) -> p h d", d=64), bc(rs[k][:, 0:nh], 64), ALU.mult,
                 reads=[b_qf[k], b_rs[k]], writes=[b_out])

        def front(ti, part):
            xp = ti % 2
            if part == 1:
                for kc in range(8):
                    S.tr(tpa[:, kc * 128:(kc + 1) * 128], hb[xp][:, kc * 128:(kc + 1) * 128], ident[:],
                         reads=[b_hb[xp], b_c], writes=[b_tpa])
                S.copy("scalar", hT[xp][:].rearrange("p a b -> p (a b)"), tpa[:], reads=[b_tpa], writes=[b_hT[xp]])
                return
            S.dma("sync", [(xt[xp][:], I["x"][ti * 128:(ti + 1) * 128, :])], writes=[b_xt[xp]])
            S.act(junk[:], xt[xp][:], AF.Square, reads=[b_xt[xp]], writes=[b_junk, b_ssx[xp]],
                  accum_out=ssx[xp][:, 0:1])
            S.ts("vector", ssx[xp][:, 0:1], ssx[xp][:, 0:1], 1.0 / 1024, EPS, ALU.mult, ALU.add,
                 reads=[b_ssx[xp]], writes=[b_ssx[xp]])
            S.tt("gpsimd", ssx[xp][:, 1:2], ssx[xp][:, 0:1], mhalf[:, 0:1], ALU.pow,
                 reads=[b_ssx[xp], b_c], writes=[b_ssx[xp]])
            S.stt(hb[xp][:], xt[xp][:], ssx[xp][:, 1:2], gb[:], ALU.mult, ALU.mult,
                  reads=[b_xt[xp], b_ssx[xp], b_c], writes=[b_hb[xp]])


        for tg in range(ntg):
            sp = tg % 2
            for tl in range(4):
                ti = tg * 4 + tl
                xp = ti % 2
                if ti == 0:
                    front(0, 0); front(0, 1)
                if ti + 1 < ntg * 4:
                    front(ti + 1, 0)
                segs = [(0, 512, "q0"), (512, 512, "q1"), (1024, 512, "kcvc"), (1536, 512, "ksvs"),
                        (2048, 512, "kwvw"), (2608, 512, "z0"), (3120, 512, "z1"), (2560, 48, "gl")]
                for si, (c0, n, kind) in enumerate(segs):
                    pb = si % 4
                    if si == 5 and ti + 1 < ntg * 4:
                        front(ti + 1, 1)
                    flush(2)
                    for kc in range(8):
                        S.mm(pj[pb][:, 0:n], hT[xp][:, kc, :], w_sb[:, kc, c0:c0 + n], kc == 0, kc == 7,
                             reads=[b_hT[xp], b_wk[kc]], writes=[b_pj[pb]])
                    if kind in ("q0", "q1"):
                        qk = nqn % 4; nqn += 1
                        norm_heads(pj[pb][:, 0:512], b_pj[pb], 8, qn[qk][:].rearrange("p (h d) -> p h d", d=64), b_qn[qk])
                        h0 = 0 if kind == "q0" else 8

                        def C(qk=qk, h0=h0, sp=sp, tl=tl):
                            nonlocal tq
                            tk = tq % 2; tq += 1
                            for h in range(8):
                                S.tr(tpq[tk][:, h * 128:(h + 1) * 128], qn[qk][:, h * 64:(h + 1) * 64], ident[:],
                                     reads=[b_qn[qk], b_c], writes=[b_tpq[tk]])
                            S.act(qTt[sp][:, h0:h0 + 8, tl * 128:(tl + 1) * 128],
                                  tpq[tk][:].rearrange("p (h t) -> p h t", t=128), AF.Copy,
                                  reads=[b_tpq[tk], b_c], writes=[b_qTt[sp]], scale=gcol[:, 0:1])
                        defer.append(C)
                    elif kind == "kcvc":
                        qk = nqn % 4; nqn += 1
                        S.copy("scalar", qn[qk][:], pj[pb][:, 0:512], reads=[b_pj[pb]], writes=[b_qn[qk]])

                        def C(qk=qk, sp=sp, tl=tl):
                            nonlocal tq
                            tk = tq % 2; tq += 1
                            for h in range(8):
                                S.tr(tpq[tk][:, h * 128:(h + 1) * 128], qn[qk][:, h * 64:(h + 1) * 64], ident[:],
                                     reads=[b_qn[qk], b_c], writes=[b_tpq[tk]])
                            S.copy("vector", kvTt[sp][:, 0:8, tl * 128:(tl + 1) * 128],
                                   tpq[tk][:].rearrange("p (h t) -> p h t", t=128),
                                   reads=[b_tpq[tk]], writes=[b_kvTt[sp]])
                        defer.append(C)
                    elif kind in ("ksvs", "kwvw"):
                        qk = nqn % 4; nqn += 1
                        norm_heads(pj[pb][:, 0:256], b_pj[pb], 4,
                                   qn[qk][:, 0:256].rearrange("p (h d) -> p h d", d=64), b_qn[qk])
                        r0, gc = (8, 1) if kind == "ksvs" else (12, 2)

                        def C(qk=qk, sp=sp, tl=tl, r0=r0, gc=gc):
                            nonlocal tq
                            tk = tq % 2; tq += 1
                            for h in range(4):
                                S.tr(tpq[tk][:, h * 128:(h + 1) * 128], qn[qk][:, h * 64:(h + 1) * 64], ident[:],
                                     reads=[b_qn[qk], b_c], writes=[b_tpq[tk]])
                            S.act(kvTt[sp][:, r0:r0 + 4, tl * 128:(tl + 1) * 128],
                                  tpq[tk][:, 0:512].rearrange("p (h t) -> p h t", t=128), AF.Copy,
                                  reads=[b_tpq[tk], b_c], writes=[b_kvTt[sp]], scale=gcol[:, gc:gc + 1])
                        defer.append(C)
                        vt, bvt = (vsAt, b_vsAt) if kind == "ksvs" else (vwAt, b_vwAt)
                        S.copy("vector", vt[sp][:, tl, :].rearrange("p (g c) -> p g c", c=65)[:, :, 0:64],
                               pj[pb][:, 256:512].rearrange("p (g c) -> p g c", c=64),
                               reads=[b_pj[pb]], writes=[bvt[sp]])
                    elif kind in ("z0", "z1"):
                        zk = nseg % 2; nseg += 1
                        S.act(zt[zk][:], pj[pb][:, 0:512], AF.Tanh, reads=[b_pj[pb]], writes=[b_zt[zk]], scale=0.5)
                        S.act(zh[zk][:], pj[pb][:, 0:512], AF.Copy, reads=[b_pj[pb]], writes=[b_zh[zk]], scale=0.5)
                        z0 = 0 if kind == "z0" else 512
                        S.stt(szt[sp][:, tl, z0:z0 + 512], zt[zk][:], 1.0, zh[zk][:], ALU.add, ALU.mult,
                              reads=[b_zt[zk], b_zh[zk]], writes=[b_szt[sp]])
                    else:
                        S.act(gtmp[:], pj[pb][:, 0:48], AF.Tanh, reads=[b_pj[pb]], writes=[b_gtmp], scale=0.5)
                        S.ts("vector", gt[sp][:, tl, :], gtmp[:], 0.5, 0.5, ALU.mult, ALU.add,
                             reads=[b_gtmp], writes=[b_gt[sp]])
            flush(0)
            t0 = tg * 512
            S.dma("sync", [(SC["qT"].rearrange("h d t -> d h t")[:, :, t0:t0 + 512], qTt[sp][:])], reads=[b_qTt[sp]])
            S.dma("sync", [(SC["kvT"].rearrange("h d t -> d h t")[:, :, t0:t0 + 512], kvTt[sp][:])], reads=[b_kvTt[sp]])
            S.dma("sync", [(SC["vsA"][tg * 4:(tg + 1) * 4].rearrange("t p c -> p t c"), vsAt[sp][:])], reads=[b_vsAt[sp]])
            S.dma("sync", [(SC["vwA"][tg * 4:(tg + 1) * 4].rearrange("t p c -> p t c"), vwAt[sp][:])], reads=[b_vwAt[sp]])
            S.dma("sync", [(SC["sz"][t0:t0 + 512, :].rearrange("(t p) c -> p t c", p=128), szt[sp][:])], reads=[b_szt[sp]])
            S.dma("sync", [(SC["gates"][t0:t0 + 512, :].rearrange("(t p) c -> p t c", p=128), gt[sp][:])], reads=[b_gt[sp]])
        S.finish(nc)


EPS = 1e-6


def declare_scratch2(nc, debug):
    kind = dict(kind="ExternalOutput") if debug else {}
    dbg = debug if isinstance(debug, (set, list, tuple)) else None
    d = {}
    d["kccT"] = nc.dram_tensor("kccT_s", [64, 4, 256], BF16, **(kind if (dbg is None or "kccT" in dbg) else {})).ap()
    d["vcA"] = nc.dram_tensor("vcA_s", [128, 2, 4, 128], BF16, **(kind if (dbg is None or "vcA" in dbg) else {})).ap()
    return d


def phase2(nc, G, I, SC):
    S = Sched(G)
    with ExitStack() as es:
        def sb(name, shape, dt):
            return es.enter_context(nc.sbuf_tensor("p2_" + name, shape, dt))

        def ps(name, shape, dt):
            return es.enter_context(nc.psum_tensor("p2_" + name, shape, dt))

        kvT = sb("kvT", [64, 8, 4096], BF16)
        W1 = [sb(f"W1{i}", [64, 32, 256], BF16) for i in range(2)]
        W2 = [sb(f"W2{i}", [128, 2, 64], BF16) for i in range(2)]
        posf = sb("posf", [64, 2, 32], F32)
        posb = sb("posb", [64, 2, 32], BF16)
        c1h = sb("c1h", [128, 4], F32)
        ident = sb("ident", [128, 128], BF16)
        ovf = sb("ovf", [128, 2, 64], F32)
        gk = sb("gk", [64, 1], F32)
        mhalf = sb("mhalf", [128, 8], F32)
        th = [sb(f"th{i}", [128, 256], F32) for i in range(2)]
        uu = [sb(f"uu{i}", [128, 256], F32) for i in range(2)]
        hid = [[sb(f"hid{a}{b}", [128, 256], BF16) for b in range(2)] for a in range(2)]
        sq = sb("sq", [128, 256], F32)
        ss = sb("ss", [128, 4], F32)
        rs = sb("rs", [128, 4], F32)
        kn = sb("kn", [128, 256], BF16)
        kccT = sb("kccT", [64, 4, 256], BF16)
        vcA = sb("vcA", [128, 2, 4, 128], BF16)

        ph = [ps(f"ph{i}", [128, 512], F32) for i in range(2)]
        pc1 = ps("pc1", [128, 512], F32)
        po = ps("po", [128, 512], F32)
        tp = ps("tp", [64, 1024], BF16)

        b_kvT = Buf(); b_W1 = [Buf(), Buf()]; b_W2 = [Buf(), Buf()]; b_c = Buf(); b_pos = Buf(); b_c1h = Buf()
        b_th = [Buf(), Buf()]; b_uu = [Buf(), Buf()]; b_hid = [[Buf(), Buf()], [Buf(), Buf()]]
        b_sq = Buf(); b_ss = Buf(); b_rs = Buf(); b_kn = Buf(); b_kccT = Buf(); b_vcA = Buf()
        b_ph = [Buf(), Buf()]; b_pc1 = Buf(); b_po = Buf(); b_tp = Buf()

        S.dma("sync", [(kvT[:, 0:4, :], SC["kvT"][0:4].rearrange("h d t -> d h t")),
                       (kvT[:, 4:8, :], SC["kvT"][4:8].rearrange("h d t -> d h t"))], writes=[b_kvT])
        for kv, nm in enumerate(["nsa_cmp_k_w1", "nsa_cmp_v_w1"]):
            src = I[nm].rearrange("(l d) m -> d l m", d=64)
            S.dma("gpsimd", [(W1[kv][:, l0:l0 + 8, :], src[:, l0:l0 + 8, :]) for l0 in range(0, 32, 8)], writes=[b_W1[kv]])
        for kv, nm in enumerate(["nsa_cmp_k_w2", "nsa_cmp_v_w2"]):
            S.dma("gpsimd", [(W2[kv][:], I[nm].rearrange("(c p) m -> p c m", p=128))], writes=[b_W2[kv]])
        for kv, nm in enumerate(["nsa_cmp_pos_k", "nsa_cmp_pos_v"]):
            S.dma("sync", [(posf[:, kv, :], bass.AP(I[nm].tensor, 0, [[1, 64], [64, 32]]))], writes=[b_pos],
                  allow_slow_non_contiguous=True)
        S.copy("vector", posb[:], posf[:], reads=[b_pos], writes=[b_pos])
        S.dma("sync", [(ident[:], I["ident"])], writes=[b_c])
        S.dma("sync", [(ovf[:], I["overlap"].rearrange("(c p) j -> p c j", p=128))], writes=[b_c])
        S.dma("sync", [(gk[:], bass.AP(I["nsa_kc_g"].tensor, 0, [[1, 64], [1, 1]]))], writes=[b_c])
        S.memset("vector", mhalf[:], -0.5, writes=[b_c])
        for a in range(2):
            for b in range(2):
                S.memset("vector", hid[a][b][:], 0.0, writes=[b_hid[a][b]])
        S.memset("vector", vcA[:], 1.0, writes=[b_vcA])
        S.memset("vector", kccT[:], 0.0, writes=[b_kccT])

        for kv in range(2):
            for hh in range(2):
                col = kv * 2 + hh
                for l in range(32):
                    S.mm(pc1[:, col:col + 1], W1[kv][:, l, hh * 128:(hh + 1) * 128], posb[:, kv, l:l + 1],
                         l == 0, l == 31, reads=[b_W1[kv], b_pos], writes=[b_pc1])
                S.act(c1h[:, col:col + 1], pc1[:, col:col + 1], AF.Copy, reads=[b_pc1], writes=[b_c1h], scale=0.5)

        it = 0
        for kv in range(2):
            for g in range(4):
                par = it % 2
                for hh in range(2):
                    pb = hh
                    col = kv * 2 + hh
                    for l in range(32):
                        rhs = mkap(kvT[:, kv * 4 + g, l:l + 1], [[16, 255]])
                        S.mm(ph[pb][:, 0:255], W1[kv][:, l, hh * 128:(hh + 1) * 128], rhs, l == 0, l == 31,
                             reads=[b_W1[kv], b_kvT], writes=[b_ph[pb]])
                    S.act(th[hh][:, 0:255], ph[pb][:, 0:255], AF.Tanh, reads=[b_ph[pb], b_c1h], writes=[b_th[hh]],
                          scale=0.5, bias=c1h[:, col:col + 1])
                    S.act(uu[hh][:, 0:255], ph[pb][:, 0:255], AF.Identity, reads=[b_ph[pb], b_c1h], writes=[b_uu[hh]],
                          scale=0.5, bias=c1h[:, col:col + 1])
                    S.stt(hid[par][hh][:, 0:255], th[hh][:, 0:255], 1.0, uu[hh][:, 0:255], ALU.add, ALU.mult,
                          reads=[b_th[hh], b_uu[hh]], writes=[b_hid[par][hh]])
                for c in range(2):
                    o_ap = po[:, (c * 4 + g) * 64:(c * 4 + g + 1) * 64]
                    for hh in range(2):
                        S.mm(o_ap, hid[par][hh][:, c * 128:(c + 1) * 128], W2[kv][:, hh, :], hh == 0, hh == 1,
                             reads=[b_hid[par][hh], b_W2[kv]], writes=[b_po], skip_group_check=True)
                it += 1
            for c in range(2):
                src = po[:, c * 256:(c + 1) * 256]
                if kv == 0:
                    S.act(sq[:], src, AF.Square, reads=[b_po], writes=[b_sq])
                    S.reduce(ss[:], sq[:].rearrange("p (h d) -> p h d", d=64), ALU.add, AX.X, reads=[b_sq], writes=[b_ss])
                    S.ts("vector", ss[:], ss[:], 1.0 / 64, EPS, ALU.mult, ALU.add, reads=[b_ss], writes=[b_ss])
                    S.tt("gpsimd", rs[:], ss[:], mhalf[:, 0:4], ALU.pow, reads=[b_ss, b_c], writes=[b_rs])
                    S.tt("vector", kn[:].rearrange("p (h d) -> p h d", d=64), src.rearrange("p (h d) -> p h d", d=64),
                         bc(rs[:], 64), ALU.mult, reads=[b_po, b_rs], writes=[b_kn])
                    for g in range(4):
                        S.tr(tp[:, g * 128:(g + 1) * 128], kn[:, g * 64:(g + 1) * 64], ident[:],
                             reads=[b_kn, b_c], writes=[b_tp])
                    S.act(kccT[:, :, c * 128:(c + 1) * 128], tp[:, 0:512].rearrange("p (g t) -> p g t", t=128), AF.Copy,
                          reads=[b_tp, b_c], writes=[b_kccT], scale=gk[:, 0:1])
                else:
                    S.copy("vector", vcA[:, c, :, 0:64], src.rearrange("p (g d) -> p g d", d=64), reads=[b_po], writes=[b_vcA])
                    S.copy("vector", vcA[:, c, :, 65:128], mkap(ovf[:, c, 0:1], [[0, 4], [1, 63]]), reads=[b_c], writes=[b_vcA])
        S.dma("sync", [(SC["kccT"], kccT[:])], reads=[b_kccT])
        S.dma("sync", [(SC["vcA"], vcA[:])], reads=[b_vcA])
        S.finish(nc)


NEG_BIG = -30000.0


def declare_scratch3(nc, debug):
    kind = dict(kind="ExternalOutput") if debug else {}
    dbg = debug if isinstance(debug, (set, list, tuple)) else None
    d = {}
    d["outg"] = nc.dram_tensor("outg_s", [4096, 1024], BF16, **(kind if (dbg is None or "outg" in dbg) else {})).ap()
    return d


def phase3(nc, G, I, SC, jlist=None, stage=9):
    S = Sched(G)
    jlist = list(range(32)) if jlist is None else jlist
    with ExitStack() as es:
        def sb(name, shape, dt):
            return es.enter_context(nc.sbuf_tensor("p3_" + name, shape, dt))

        def ps(name, shape, dt):
            return es.enter_context(nc.psum_tensor("p3_" + name, shape, dt))

        KE = sb("KE", [128, 4, 4096], BF16)
        kwT = sb("kwT", [64, 4, 4096], BF16)
        vsA = sb("vsA", [128, 32, 260], BF16)
        vwA = sb("vwA", [128, 32, 260], BF16)
        kccT = sb("kccT", [64, 4, 256], BF16)
        vcA = sb("vcA", [128, 2, 4, 128], BF16)
        Ftab = sb("Ftab", [128, 32, 64], F32)
        ident = sb("ident", [128, 128], BF16)
        wbias = sb("wbias", [128, 2, 128], BF16)
        cmpb = [sb(f"cmpb{i}", [128, 2, 128], BF16) for i in range(2)]
        QS = [sb(f"QS{i}", [128, 512], BF16) for i in range(2)]
        gt = [sb(f"gt{i}", [128, 48], F32) for i in range(2)]
        szt = [sb(f"szt{i}", [128, 1024], BF16) for i in range(2)]
        Pt = [sb(f"Pt{i}", [128, 2, 512], BF16) for i in range(2)]
        acc = sb("acc", [128, 16, 64], F32)
        tmp = sb("tmp", [128, 4, 64], F32)
        og = [sb(f"og{i}", [128, 1024], BF16) for i in range(2)]
        zc = sb("zc", [128, 4], F32)
        rz = sb("rz", [128, 4], F32)
        coef = sb("coef", [128, 4], F32)
        score = sb("score", [128, 64], F32)
        work = sb("work", [128, 64], F32)
        m8 = sb("m8", [128, 16], F32)
        selb = sb("selb", [128, 128], BF16)

        pS = [ps(f"pS{i}", [128, 2, 512], F32) for i in range(2)]
        Oc = ps("Oc", [128, 4, 128], F32)
        Ow = ps("Ow", [128, 4, 128], F32)
        Os = ps("Os", [128, 4, 128], F32)
        tps = ps("tps", [128, 1024], BF16)

        b_c = Buf(); bK = {n: Buf() for n in ["KE", "kwT", "vsA", "vwA", "kccT", "vcA"]}
        b_cmpb = [Buf(), Buf()]; b_QSq = [Buf(), Buf()]; b_QSs = [Buf(), Buf()]
        b_gt = [Buf(), Buf()]; b_szt = [Buf(), Buf()]
        b_Pt = [Buf() for _ in range(2)]; b_acc = Buf(); b_tmp = Buf(); b_og = [Buf(), Buf()]
        b_zc = Buf(); b_rz = Buf(); b_coef = Buf(); b_score = Buf(); b_work = Buf(); b_m8 = Buf(); b_selb = Buf()
        b_pS = [Buf() for _ in range(2)]; b_Oc = Buf(); b_Ow = Buf(); b_Os = Buf(); b_tps = Buf()

        S.dma("sync", [(ident[:], I["ident"])], writes=[b_c])
        S.dma("sync", [(Ftab[:], I["Ftab"])], writes=[b_c])
        S.dma("sync", [(wbias[:], I["wbias"].rearrange("w i u -> i w u"))], writes=[b_c])
        S.dma("sync", [(kccT[:], SC["kccT"])], writes=[bK["kccT"]])
        S.dma("sync", [(vcA[:], SC["vcA"])], writes=[bK["vcA"]])
        S.dma("sync", [(KE[0:64, :, :], SC["kvT"][8:12].rearrange("h d t -> d h t"))] + [(KE[64:128, g, :], I["Eexp"]) for g in range(4)], writes=[bK["KE"]])
        S.dma("sync", [(kwT[:], SC["kvT"][12:16].rearrange("h d t -> d h t"))], writes=[bK["kwT"]])
        S.dma("sync", [(vsA[:, a:a + 4, :], SC["vsA"][a:a + 4].rearrange("t p c -> p t c")) for a in range(0, 32, 4)], writes=[bK["vsA"]])
        S.dma("sync", [(vwA[:, a:a + 4, :], SC["vwA"][a:a + 4].rearrange("t p c -> p t c")) for a in range(0, 32, 4)], writes=[bK["vwA"]])
        S.memset("vector", selb[:], 0.0, writes=[b_selb])

        cnt = {"u": 0, "it": 0}

        units = []

        def add_units(tile_list, pre_first=None, pre_last=None, post_last=None):
            us = []
            for a in range(0, len(tile_list), 2):
                us.append(dict(tiles=tile_list[a:a + 2], pre=[], post=[]))
            if pre_first is not None:
                us[0]["pre"].append(pre_first)
            if pre_last is not None:
                us[-1]["pre"].append(pre_last)
            if post_last is not None:
                us[-1]["post"].append(post_last)
            units.extend(us)

        def emit_S(u):
            for f in u["pre"]:
                f()
            ui = cnt["u"] % 2; cnt["u"] += 1
            u["ui"] = ui
            for ti, t in enumerate(u["tiles"]):
                mms = t["mms"]
                for k, (l, r) in enumerate(mms):
                    S.mm(pS[ui][:, ti, :], l, r, k == 0, k == len(mms) - 1, reads=t["reads"], writes=[b_pS[ui]])

        def emit_EP(u):
            ui = u["ui"]
            n = len(u["tiles"])
            S.act(Pt[ui][:, 0:n, :], pS[ui][:, 0:n, :], AF.Exp, reads=[b_pS[ui]], writes=[b_Pt[ui]])
            for ti, t in enumerate(u["tiles"]):
                for h in range(4):
                    S.mm(t["O"](h), Pt[ui][:, ti, h * 128:(h + 1) * 128], t["V"], t["first"] and h == 0, t["last"],
                         reads=[b_Pt[ui], t["bV"]], writes=[t["bO"]], skip_group_check=True)
            for f in u["post"]:
                f()

        for jn, j in enumerate(jlist):
            jp = jn % 2
            t0 = j * 128
            cs = []
            if j <= 16:
                cs.append((0, True))
            else:
                cs.append((0, False))
            if j >= 16:
                cs.append((1, True))
            for g in range(4):
                it = cnt["it"]; cnt["it"] += 1
                qp = it % 2
                qtop = QS[qp][0:64, :]

                def pre_iter(jn=jn, j=j, jp=jp, t0=t0, g=g, qp=qp, cs=cs):
                    if g == 0:
                        S.dma("sync", [(gt[jp][:], SC["gates"][t0:t0 + 128, :])], writes=[b_gt[jp]])
                        S.dma("sync", [(szt[jp][:], SC["sz"][t0:t0 + 128, :])], writes=[b_szt[jp]])
                        S.dma("sync", [(cmpb[jp][:, c, :], I["cmpb"][j, c]) for (c, hb) in cs if hb], writes=[b_cmpb[jp]])
                    S.dma("sync", [(QS[qp][0:64, :].rearrange("p (h t) -> p h t", t=128),
                                    SC["qT"][4 * g:4 * g + 4, :, t0:t0 + 128].rearrange("h d t -> d h t"))],
                          writes=[b_QSq[qp]])

                ctiles = []
                for ci, (c, hasb) in enumerate(cs):
                    mms = [(kccT[:, g, c * 128:(c + 1) * 128], qtop)]
                    rd = [bK["kccT"], b_QSq[qp]]
                    if hasb:
                        mms.append((ident[:], mkap(cmpb[jp][:, c, 0:1], [[0, 4], [1, 128]])))
                        rd = rd + [b_c, b_cmpb[jp]]
                    ctiles.append(dict(mms=mms, reads=rd, O=(lambda h: Oc[:, h, :]), V=vcA[:, c, g, :], bV=bK["vcA"],
                                       bO=b_Oc, first=(ci == 0), last=(ci == len(cs) - 1)))

                def post_cmp(j=j, jp=jp, g=g, qp=qp):
                    S.ts("vector", zc[:], Oc[:, :, 64], 1e-30, None, ALU.max, reads=[b_Oc], writes=[b_zc])
                    S.op("vector", lambda e: e.reciprocal(out=rz[:], in_=zc[:]), reads=[b_zc], writes=[b_rz])
                    S.copy("vector", score[:], Ftab[:, j, :], reads=[b_c], writes=[b_score])
                    for h in range(4):
                        S.stt(score[:, 0:63], Oc[:, h, 65:128], rz[:, h:h + 1], score[:, 0:63], ALU.mult, ALU.add,
                              reads=[b_Oc, b_rz, b_score], writes=[b_score])
                    S.op("vector", lambda e: e.max(out=m8[:, 0:8], in_=score[:]), reads=[b_score], writes=[b_m8])
                    S.op("vector", lambda e: e.match_replace(out=work[:], in_to_replace=m8[:, 0:8], in_values=score[:],
                                                             imm_value=-3.0e38),
                         reads=[b_score, b_m8], writes=[b_work])
                    S.op("vector", lambda e: e.max(out=m8[:, 8:16], in_=work[:]), reads=[b_work], writes=[b_m8])
                    S.ts("vector", selb[:, 64:128], score[:], m8[:, 15:16], NEG_BIG, ALU.is_lt, ALU.mult,
                         reads=[b_score, b_m8], writes=[b_selb])
                    S.tt("vector", coef[:], rz[:], mkap(gt[jp][:, 12 * g:12 * g + 1], [[3, 4]]), ALU.mult,
                         reads=[b_rz, b_gt[jp]], writes=[b_coef])
                    S.tt("vector", acc[:, 4 * g:4 * g + 4, :], Oc[:, :, 0:64], bc(coef[:], 64), ALU.mult,
                         reads=[b_Oc, b_coef], writes=[b_acc])

                add_units(ctiles, pre_first=pre_iter, post_last=post_cmp)

                kts = [kt for kt in range(j - 4, j + 1) if kt >= 0]
                wtiles = []
                for ki, kt in enumerate(kts):
                    mms = [(kwT[:, g, kt * 128:(kt + 1) * 128], qtop)]
                    rd = [bK["kwT"], b_QSq[qp]]
                    if kt == j:
                        mms.append((ident[:], mkap(wbias[:, 0, 0:1], [[0, 4], [1, 128]])))
                        rd = rd + [b_c]
                    elif kt == j - 4:
                        mms.append((ident[:], mkap(wbias[:, 1, 0:1], [[0, 4], [1, 128]])))
                        rd = rd + [b_c]
                    wtiles.append(dict(mms=mms, reads=rd, O=(lambda h: Ow[:, h, 0:65]), V=vwA[:, kt, g * 65:(g + 1) * 65],
                                       bV=bK["vwA"], bO=b_Ow, first=(ki == 0), last=(ki == len(kts) - 1)))

                def pre_selT(qp=qp):
                    S.tr(tps[:, 0:128], selb[:], ident[:], reads=[b_selb, b_c], writes=[b_tps])
                    S.copy("vector", QS[qp][64:128, :].rearrange("p (h t) -> p h t", t=128),
                           mkap(tps[64:128, 0:1], [[0, 4], [1, 128]]), reads=[b_tps], writes=[b_QSs[qp]])

                def post_win(jp=jp, g=g):
                    S.op("vector", lambda e: e.reciprocal(out=rz[:], in_=Ow[:, :, 64]), reads=[b_Ow], writes=[b_rz])
                    S.tt("vector", coef[:], rz[:], mkap(gt[jp][:, 12 * g + 2:12 * g + 3], [[3, 4]]), ALU.mult,
                         reads=[b_rz, b_gt[jp]], writes=[b_coef])
                    S.tt("vector", tmp[:], Ow[:, :, 0:64], bc(coef[:], 64), ALU.mult,
                         reads=[b_Ow, b_coef], writes=[b_tmp])
                    S.tt("gpsimd", acc[:, 4 * g:4 * g + 4, :], acc[:, 4 * g:4 * g + 4, :], tmp[:], ALU.add,
                         reads=[b_tmp], writes=[b_acc])

                add_units(wtiles, post_last=post_win)

                stiles = []
                for kt in range(j + 1):
                    mms = [(KE[:, g, kt * 128:(kt + 1) * 128], QS[qp][:, :])]
                    rd = [bK["KE"], b_QSq[qp], b_QSs[qp]]
                    if kt == j:
                        mms.append((ident[:], mkap(wbias[:, 0, 0:1], [[0, 4], [1, 128]])))
                        rd = rd + [b_c]
                    stiles.append(dict(mms=mms, reads=rd, O=(lambda h: Os[:, h, 0:65]), V=vsA[:, kt, g * 65:(g + 1) * 65],
                                       bV=bK["vsA"], bO=b_Os, first=(kt == 0), last=(kt == j)))

                def post_sel(jp=jp, g=g, t0=t0):
                    S.op("vector", lambda e: e.reciprocal(out=rz[:], in_=Os[:, :, 64]), reads=[b_Os], writes=[b_rz])
                    S.tt("vector", coef[:], rz[:], mkap(gt[jp][:, 12 * g + 1:12 * g + 2], [[3, 4]]), ALU.mult,
                         reads=[b_rz, b_gt[jp]], writes=[b_coef])
                    S.tt("vector", tmp[:], Os[:, :, 0:64], bc(coef[:], 64), ALU.mult,
                         reads=[b_Os, b_coef], writes=[b_tmp])
                    S.tt("gpsimd", acc[:, 4 * g:4 * g + 4, :], acc[:, 4 * g:4 * g + 4, :], tmp[:], ALU.add,
                         reads=[b_tmp], writes=[b_acc])
                    if g == 3:
                        S.tt("gpsimd", og[jp][:], acc[:].rearrange("p h d -> p (h d)"), szt[jp][:], ALU.mult,
                             reads=[b_acc, b_szt[jp]], writes=[b_og[jp]])
                        S.dma("sync", [(SC["outg"][t0:t0 + 128, :], og[jp][:])], reads=[b_og[jp]])

                add_units(stiles, pre_first=pre_selT, post_last=post_sel)

        if units:
            emit_S(units[0])
            for i in range(len(units)):
                if i + 1 < len(units):
                    emit_S(units[i + 1])
                emit_EP(units[i])
        S.finish(nc)


EPS = 1e-6


def declare_scratch4(nc, debug):
    kind = dict(kind="ExternalOutput") if debug else {}
    dbg = debug if isinstance(debug, (set, list, tuple)) else None
    d = {}
    d["x2"] = nc.dram_tensor("x2_s", [4096, 1024], F32, **(kind if (dbg is None or "x2" in dbg) else {})).ap()
    d["h2T"] = nc.dram_tensor("h2T_s", [8, 128, 4096], BF16, **(kind if (dbg is None or "h2T" in dbg) else {})).ap()
    return d


def epilogue(nc, G, pfx, src, KW, w_out, x_in, p_in, gw_in, pw_in, x_out, ident_in, norm_g_row=None, hT_out=None, ntiles=32):
    S = Sched(G)
    KC = KW // 128
    with ExitStack() as es:
        def sb(name, shape, dt):
            return es.enter_context(nc.sbuf_tensor(pfx + name, shape, dt))

        def ps(name, shape, dt):
            return es.enter_context(nc.psum_tensor(pfx + name, shape, dt))

        wo = sb("wo", [128, KC, 1024], BF16)
        gw = sb("gw", [128, 8, 1024], BF16)
        pw = sb("pw", [128, 2, 1024], BF16)
        ident = sb("ident", [128, 128], BF16)
        gb = sb("gb", [128, 1024], F32)
        mhalf = sb("mhalf", [128, 2], F32)
        ogt = [sb(f"ogt{i}", [128, KW], BF16) for i in range(2)]
        ogT = [sb(f"ogT{i}", [128, KC, 128], BF16) for i in range(2)]
        xt = [sb(f"xt{i}", [128, 1024], F32) for i in range(2)]
        pt = [sb(f"pt{i}", [128, 256], F32) for i in range(2)]
        ptb = sb("ptb", [128, 256], BF16)
        pT = [sb(f"pT{i}", [128, 2, 128], BF16) for i in range(2)]
        x1 = [sb(f"x1{i}", [128, 1024], F32) for i in range(2)]
        x1b = [sb(f"x1b{i}", [128, 1024], BF16) for i in range(2)]
        x1T = [sb(f"x1T{i}", [128, 8, 128], BF16) for i in range(2)]
        th = sb("th", [128, 1024], F32)
        t2 = sb("t2", [128, 1024], F32)
        x2 = [sb(f"x2{i}", [128, 1024], F32) for i in range(2)]
        junk = sb("junk", [128, 1024], BF16)
        ssx = sb("ssx", [128, 2], F32)
        hb = sb("hb", [128, 1024], BF16)
        hTt = [sb(f"hTt{i}", [128, 8, 512], BF16) for i in range(2)]

        tp = [ps(f"tp{i}", [128, 1024], BF16) for i in range(2)]
        py = [ps(f"py{i}", [128, 512], F32) for i in range(2)]
        pg = [ps(f"pg{i}", [128, 512], F32) for i in range(2)]
        pp = [ps(f"pp{i}", [128, 512], F32) for i in range(2)]

        b_w = Buf(); b_gw = Buf(); b_pw = Buf(); b_c = Buf()
        b_ogt = [Buf(), Buf()]; b_ogT = [Buf(), Buf()]; b_xt = [Buf(), Buf()]; b_pt = [Buf(), Buf()]; b_ptb = Buf()
        b_pT = [Buf(), Buf()]; b_x1 = [Buf(), Buf()]; b_x1b = [Buf(), Buf()]; b_x1T = [Buf(), Buf()]; b_th = Buf(); b_t2 = Buf()
        b_x2 = [Buf(), Buf()]; b_junk = Buf(); b_ssx = Buf(); b_hb = Buf(); b_hTt = [Buf(), Buf()]
        b_tp = [Buf(), Buf()]; b_py = [Buf(), Buf()]; b_pg = [Buf(), Buf()]; b_pp = [Buf(), Buf()]

        S.dma("sync", [(ident[:], ident_in)], writes=[b_c])
        if norm_g_row is not None:
            S.dma("sync", [(gb[:], norm_g_row)], writes=[b_c])
        S.memset("vector", mhalf[:], -0.5, writes=[b_c])
        wt = []
        b_wk = [Buf() for _ in range(KC)]
        b_gwk = [Buf() for _ in range(8)]
        for kc in range(0, KC, 2):
            wt.append(S.dma("gpsimd", [(wo[:, kc + a, :], w_out[(kc + a) * 128:(kc + a + 1) * 128, :]) for a in range(2)],
                            writes=[b_wk[kc], b_wk[kc + 1]], extra=wt[-2:-1] if len(wt) >= 2 else ()))
        for kc in range(0, 8, 2):
            wt.append(S.dma("gpsimd", [(gw[:, kc + a, :], gw_in[(kc + a) * 128:(kc + a + 1) * 128, :]) for a in range(2)],
                            writes=[b_gwk[kc], b_gwk[kc + 1]], extra=wt[-2:-1]))
        wt.append(S.dma("gpsimd", [(pw[:, kc, :], pw_in[kc * 128:(kc + 1) * 128, :]) for kc in range(2)], writes=[b_pw], extra=wt[-2:-1]))

        ntp = 0

        def stageA(ti):
            nonlocal ntp
            k = ti % 2
            r0 = ti * 128
            S.dma("sync", [(ogt[k][:], src[r0:r0 + 128, :])], writes=[b_ogt[k]])
            S.dma("sync", [(xt[k][:], x_in[r0:r0 + 128, :])], writes=[b_xt[k]])
            S.dma("sync", [(pt[k][:], p_in[r0:r0 + 128, :])], writes=[b_pt[k]])
            for c0 in range(0, KC, 8):
                q = ntp % 2; ntp += 1
                for kc in range(8):
                    S.tr(tp[q][:, kc * 128:(kc + 1) * 128], ogt[k][:, (c0 + kc) * 128:(c0 + kc + 1) * 128], ident[:],
                         reads=[b_ogt[k], b_c], writes=[b_tp[q]])
                S.copy("vector" if c0 == 0 else "scalar", ogT[k][:, c0:c0 + 8, :].rearrange("p a b -> p (a b)"), tp[q][:],
                       reads=[b_tp[q]], writes=[b_ogT[k]])
            S.copy("gpsimd", ptb[:], pt[k][:], reads=[b_pt[k]], writes=[b_ptb])
            q = ntp % 2; ntp += 1
            for c in range(2):
                S.tr(tp[q][:, c * 128:(c + 1) * 128], ptb[:, c * 128:(c + 1) * 128], ident[:],
                     reads=[b_ptb, b_c], writes=[b_tp[q]])
            S.copy("vector", pT[k][:].rearrange("p a b -> p (a b)"), tp[q][:, 0:256], reads=[b_tp[q]], writes=[b_pT[k]])
            for nb in range(2):
                for kc in range(KC):
                    S.mm(py[nb][:], ogT[k][:, kc, :], wo[:, kc, nb * 512:(nb + 1) * 512], kc == 0, kc == KC - 1,
                         reads=[b_ogT[k], b_wk[kc]], writes=[b_py[nb]])
                S.tt("vector", x1[k][:, nb * 512:(nb + 1) * 512], py[nb][:], xt[k][:, nb * 512:(nb + 1) * 512], ALU.add,
                     reads=[b_py[nb], b_xt[k]], writes=[b_x1[k]])
            S.copy("scalar", x1b[k][:], x1[k][:], reads=[b_x1[k]], writes=[b_x1b[k]])

        def stageB(ti):
            nonlocal ntp
            k = ti % 2
            r0 = ti * 128
            q = ntp % 2; ntp += 1
            for kc in range(8):
                S.tr(tp[q][:, kc * 128:(kc + 1) * 128], x1b[k][:, kc * 128:(kc + 1) * 128], ident[:],
                     reads=[b_x1b[k], b_c], writes=[b_tp[q]])
            S.copy("scalar", x1T[k][:].rearrange("p a b -> p (a b)"), tp[q][:], reads=[b_tp[q]], writes=[b_x1T[k]])
            for nb in range(2):
                for kc in range(8):
                    S.mm(pg[nb][:], x1T[k][:, kc, :], gw[:, kc, nb * 512:(nb + 1) * 512], kc == 0, kc == 7,
                         reads=[b_x1T[k], b_gwk[kc]], writes=[b_pg[nb]])
                for c in range(2):
                    S.mm(pp[nb][:], pT[k][:, c, :], pw[:, c, nb * 512:(nb + 1) * 512], c == 0, c == 1,
                         reads=[b_pT[k], b_pw], writes=[b_pp[nb]])
                sl = slice(nb * 512, (nb + 1) * 512)
                S.act(th[:, sl], pg[nb][:], AF.Tanh, reads=[b_pg[nb]], writes=[b_th], scale=0.5)
                S.stt(t2[:, sl], th[:, sl], 1.0, pp[nb][:], ALU.add, ALU.mult, reads=[b_th, b_pp[nb]], writes=[b_t2])
            S.stt(x2[k][:], t2[:], 0.5, x1[k][:], ALU.mult, ALU.add, reads=[b_t2, b_x1[k]], writes=[b_x2[k]])
            S.dma("sync", [(x_out[r0:r0 + 128, :], x2[k][:])], reads=[b_x2[k]])
            if hT_out is not None:
                tg, tl = divmod(ti, 4)
                sp = tg % 2
                S.act(junk[:], x2[k][:], AF.Square, reads=[b_x2[k]], writes=[b_junk, b_ssx], accum_out=ssx[:, 0:1])
                S.ts("vector", ssx[:, 0:1], ssx[:, 0:1], 1.0 / 1024, EPS, ALU.mult, ALU.add, reads=[b_ssx], writes=[b_ssx])
                S.tt("gpsimd", ssx[:, 1:2], ssx[:, 0:1], mhalf[:, 0:1], ALU.pow, reads=[b_ssx, b_c], writes=[b_ssx])
                S.stt(hb[:], x2[k][:], ssx[:, 1:2], gb[:], ALU.mult, ALU.mult, reads=[b_x2[k], b_ssx, b_c], writes=[b_hb])
                q = ntp % 2; ntp += 1
                for kc in range(8):
                    S.tr(tp[q][:, kc * 128:(kc + 1) * 128], hb[:, kc * 128:(kc + 1) * 128], ident[:],
                         reads=[b_hb, b_c], writes=[b_tp[q]])
                S.copy("scalar", hTt[sp][:, :, tl * 128:(tl + 1) * 128], tp[q][:].rearrange("p (a b) -> p a b", b=128),
                       reads=[b_tp[q]], writes=[b_hTt[sp]])
                if tl == 3:
                    S.dma("sync", [(hT_out.rearrange("k p t -> p k t")[:, :, tg * 512:(tg + 1) * 512], hTt[sp][:])],
                          reads=[b_hTt[sp]])

        stageA(0)
        for ti in range(ntiles):
            if ti + 1 < ntiles:
                stageA(ti + 1)
            stageB(ti)
        S.finish(nc)


GN_EPS = 1e-5
RET_COLS = 6144


def declare_scratch5(nc, debug):
    kind = dict(kind="ExternalOutput") if debug else {}
    dbg = debug if isinstance(debug, (set, list, tuple)) else None
    d = {}
    d["og"] = nc.dram_tensor("og_s", [4096, 2048], BF16, **(kind if (dbg is None or "og" in dbg) else {})).ap()
    return d


def phase5(nc, G, I, SC, chunk_decay, ntiles=32):
    S = Sched(G)
    with ExitStack() as es:
        def sb(name, shape, dt):
            return es.enter_context(nc.sbuf_tensor("p5_" + name, shape, dt))

        def ps(name, shape, dt):
            return es.enter_context(nc.psum_tensor("p5_" + name, shape, dt))

        wr = sb("wr", [128, 8, RET_COLS], BF16)
        ident = sb("ident", [128, 128], BF16)
        decT = sb("decT", [128, 4, 128], F32)
        qdT = sb("qdT", [128, 4, 128], F32)
        kdec = sb("kdec", [128, 4], F32)
        mhalf = sb("mhalf", [128, 4], F32)
        st32 = sb("st32", [128, 4, 2, 512], F32)
        stb = sb("stb", [128, 4, 2, 512], BF16)
        hT = [sb(f"hT{i}", [128, 8, 128], BF16) for i in range(2)]
        rope = [sb(f"rope{i}", [128, 4, 128], F32) for i in range(2)]
        ra = sb("ra", [128, 2, 128], F32)
        rb = sb("rb", [128, 2, 128], F32)
        qr = sb("qr", [128, 4, 256], BF16)
        kr = sb("kr", [128, 4, 256], BF16)
        kd = sb("kd", [128, 4, 256], BF16)
        qT = sb("qT", [128, 4, 2, 128], BF16)
        qsT = sb("qsT", [128, 4, 2, 128], BF16)
        kT = sb("kT", [128, 4, 2, 128], BF16)
        vt = sb("vt", [128, 4, 512], BF16)
        zt = sb("zt", [128, 512], F32)
        zh = sb("zh", [128, 512], F32)
        szt = sb("szt", [128, 2048], BF16)
        attd = sb("attd", [128, 4, 128], BF16)
        o32 = sb("o32", [128, 4, 512], F32)
        junk = sb("junk", [128, 512], BF16)
        st = sb("st", [128, 8], F32)
        mv = sb("mv", [128, 16], F32)
        nbias = sb("nbias", [128, 4], F32)
        ogt = [sb(f"ogt{i}", [128, 2048], BF16) for i in range(2)]

        pj = [ps(f"pj{i}", [128, 512], F32) for i in range(2)]
        tp = ps("tp", [128, 1024], BF16)
        pa = ps("pa", [128, 4, 128], F32)
        po = [ps(f"po{i}", [128, 512], F32) for i in range(2)]
        pu = [ps(f"pu{i}", [128, 512], F32) for i in range(2)]

        b_w = Buf(); b_c = Buf(); b_st32 = Buf(); b_stb = Buf()
        b_hT = [Buf(), Buf()]; b_rope = [Buf(), Buf()]; b_ra = Buf(); b_rb = Buf()
        b_qr = Buf(); b_kr = Buf(); b_kd = Buf(); b_qT = Buf(); b_qsT = Buf(); b_kT = Buf(); b_vt = Buf()
        b_zt = Buf(); b_zh = Buf(); b_szt = Buf(); b_attd = Buf(); b_o32 = Buf(); b_junk = Buf()
        b_st = Buf(); b_mv = Buf(); b_nb = Buf(); b_ogt = [Buf(), Buf()]
        b_pj = [Buf(), Buf()]; b_tp = Buf(); b_pa = Buf(); b_po = [Buf(), Buf()]; b_pu = [Buf(), Buf()]

        S.dma("sync", [(ident[:], I["ident"])], writes=[b_c])
        S.dma("sync", [(decT[:], I["decT"])], writes=[b_c])
        S.dma("sync", [(qdT[:], I["qdT"])], writes=[b_c])
        S.dma("sync", [(kdec[:], I["kdec"])], writes=[b_c])
        S.memset("vector", mhalf[:], -0.5, writes=[b_c])
        S.memset("vector", st32[:], 0.0, writes=[b_st32])
        S.memset("vector", stb[:], 0.0, writes=[b_stb])
        b_wk = [Buf() for _ in range(8)]
        wt = []
        for kc in range(8):
            wt.append(S.dma("gpsimd", [(wr[:, kc, c0:c0 + 1536], I["ret_w_in"][kc * 128:(kc + 1) * 128, c0:c0 + 1536])
                                       for c0 in range(0, RET_COLS, 1536)], writes=[b_wk[kc]], extra=wt[-2:-1] if len(wt) >= 2 else ()))

        nseg = 0
        npo = 0
        for ti in range(ntiles):
            k = ti % 2
            r0 = ti * 128
            S.dma("sync", [(hT[k][:], SC["h2T"][:, :, r0:r0 + 128].rearrange("k p t -> p k t"))], writes=[b_hT[k]])
            S.dma("sync", [(rope[k][:], I["rope"][r0:r0 + 128])], writes=[b_rope[k]])

            def proj(c0):
                nonlocal nseg
                pb = nseg % 2; nseg += 1
                for kc in range(8):
                    S.mm(pj[pb][:], hT[k][:, kc, :], wr[:, kc, c0:c0 + 512], kc == 0, kc == 7,
                         reads=[b_hT[k], b_wk[kc]], writes=[b_pj[pb]])
                return pj[pb], b_pj[pb]

            for which in range(2):
                dst, b_dst = (qr, b_qr) if which == 0 else (kr, b_kr)
                cs, sn = (rope[k][:, 0, :], rope[k][:, 1, :]) if which == 0 else (rope[k][:, 2, :], rope[k][:, 3, :])
                csb = mkap(cs, [[0, 2], [1, 128]]); snb = mkap(sn, [[0, 2], [1, 128]])
                for half in range(2):
                    P, bP = proj(which * 1024 + half * 512)
                    x1 = mkap(P[:, 0:1], [[256, 2], [1, 128]])
                    x2 = mkap(P[:, 128:129], [[256, 2], [1, 128]])
                    o1 = dst[:, half * 2:half * 2 + 2, 0:128]
                    o2 = dst[:, half * 2:half * 2 + 2, 128:256]
                    S.tt("vector", ra[:], x1, csb, ALU.mult, reads=[bP, b_rope[k]], writes=[b_ra])
                    S.tt("vector", rb[:], x2, snb, ALU.mult, reads=[bP, b_rope[k]], writes=[b_rb])
                    S.tt("vector", o1, ra[:], rb[:], ALU.subtract, reads=[b_ra, b_rb], writes=[b_dst])
                    S.tt("vector", ra[:], x1, snb, ALU.mult, reads=[bP, b_rope[k]], writes=[b_ra])
                    S.tt("vector", rb[:], x2, csb, ALU.mult, reads=[bP, b_rope[k]], writes=[b_rb])
                    S.tt("vector", o2, ra[:], rb[:], ALU.add, reads=[b_ra, b_rb], writes=[b_dst])
            for h in range(4):
                S.ts("gpsimd", kd[:, h, :], kr[:, h, :], kdec[:, h:h + 1], None, ALU.mult, reads=[b_kr, b_c], writes=[b_kd])
            for h in range(4):
                P, bP = proj(2048 + h * 512)
                S.copy("scalar", vt[:, h, :], P[:], reads=[bP], writes=[b_vt])
            for zc in range(4):
                P, bP = proj(4096 + zc * 512)
                S.act(zt[:], P[:], AF.Tanh, reads=[bP], writes=[b_zt], scale=0.5)
                S.act(zh[:], P[:], AF.Copy, reads=[bP], writes=[b_zh], scale=0.5)
                S.stt(szt[:, zc * 512:(zc + 1) * 512], zt[:], 1.0, zh[:], ALU.add, ALU.mult,
                      reads=[b_zt, b_zh], writes=[b_szt])
            for which in range(2):
                src, b_src = (qr, b_qr) if which == 0 else (kr, b_kr)
                for h in range(4):
                    for dc in range(2):
                        S.tr(tp[:, (h * 2 + dc) * 128:(h * 2 + dc + 1) * 128], src[:, h, dc * 128:(dc + 1) * 128], ident[:],
                             reads=[b_src, b_c], writes=[b_tp])
                if which == 0:
                    S.copy("scalar", qT[:].rearrange("p h c t -> p (h c t)"), tp[:], reads=[b_tp], writes=[b_qT])
                    S.tt("vector", qsT[:], tp[:].rearrange("p (h c t) -> p h c t", h=4, c=2),
                         mkap(qdT[:, 0, 0:1], [[128, 4], [0, 2], [1, 128]]),
                         ALU.mult, reads=[b_tp, b_c], writes=[b_qsT])
                else:
                    S.copy("scalar", kT[:].rearrange("p h c t -> p (h c t)"), tp[:], reads=[b_tp], writes=[b_kT])
            for h in range(4):
                for dc in range(2):
                    S.mm(pa[:, h, :], kT[:, h, dc, :], qT[:, h, dc, :], h == 0 and dc == 0, dc == 1,
                         reads=[b_kT, b_qT], writes=[b_pa], skip_group_check=True)
            S.tt("vector", attd[:], pa[:], decT[:], ALU.mult, reads=[b_pa, b_c], writes=[b_attd])
            for h in range(4):
                ob = npo % 2; npo += 1
                S.mm(po[ob][:], attd[:, h, :], vt[:, h, :], True, False, reads=[b_attd, b_vt], writes=[b_po[ob]])
                for dc in range(2):
                    S.mm(po[ob][:], qsT[:, h, dc, :], stb[:, h, dc, :], False, dc == 1,
                         reads=[b_qsT, b_stb], writes=[b_po[ob]])
                for dc in range(2):
                    S.mm(pu[dc][:], kd[:, h, dc * 128:(dc + 1) * 128], vt[:, h, :], True, True,
                         reads=[b_kd, b_vt], writes=[b_pu[dc]])
                    S.stt(st32[:, h, dc, :], st32[:, h, dc, :], float(chunk_decay[h]), pu[dc][:], ALU.mult, ALU.add,
                          reads=[b_pu[dc]], writes=[b_st32])
                S.copy("gpsimd", stb[:, h, :, :], st32[:, h, :, :], reads=[b_st32], writes=[b_stb])
                S.act(o32[:, h, :], po[ob][:], AF.Copy, reads=[b_po[ob]], writes=[b_o32, b_st], accum_out=st[:, 2 * h:2 * h + 1])
                S.act(junk[:], po[ob][:], AF.Square, reads=[b_po[ob]], writes=[b_junk, b_st], accum_out=st[:, 2 * h + 1:2 * h + 2])
            sv = st[:].rearrange("p (h two) -> p h two", two=2)
            S.ts("vector", mv[:, 0:4], sv[:, :, 0], 1.0 / 512, None, ALU.mult, reads=[b_st], writes=[b_mv])
            S.ts("vector", mv[:, 4:8], sv[:, :, 1], 1.0 / 512, None, ALU.mult, reads=[b_st], writes=[b_mv])
            S.tt("vector", mv[:, 8:12], mv[:, 0:4], mv[:, 0:4], ALU.mult, reads=[b_mv], writes=[b_mv])
            S.tt("vector", mv[:, 8:12], mv[:, 4:8], mv[:, 8:12], ALU.subtract, reads=[b_mv], writes=[b_mv])
            S.ts("vector", mv[:, 8:12], mv[:, 8:12], GN_EPS, None, ALU.add, reads=[b_mv], writes=[b_mv])
            S.tt("gpsimd", mv[:, 12:16], mv[:, 8:12], mhalf[:], ALU.pow, reads=[b_mv, b_c], writes=[b_mv])
            S.stt(nbias[:], mv[:, 0:4], -1.0, mv[:, 12:16], ALU.mult, ALU.mult, reads=[b_mv], writes=[b_nb])
            for h in range(4):
                S.act(o32[:, h, :], o32[:, h, :], AF.Identity, reads=[b_mv, b_nb], writes=[b_o32],
                      scale=mv[:, 12 + h:13 + h], bias=nbias[:, h:h + 1])
                S.tt("gpsimd" if h % 2 else "vector", ogt[k][:, h * 512:(h + 1) * 512], o32[:, h, :], szt[:, h * 512:(h + 1) * 512],
                     ALU.mult, reads=[b_o32, b_szt], writes=[b_ogt[k]])
            S.dma("sync", [(SC["og"][r0:r0 + 128, :], ogt[k][:])], reads=[b_ogt[k]])
        S.finish(nc)


from concourse.bass_utils import run_bass_kernel_spmd

W_NAMES = ["norm_g", "nsa_w_in", "nsa_q_g", "nsa_kc_g", "nsa_ks_g", "nsa_kw_g", "nsa_cmp_pos_k", "nsa_cmp_pos_v",
           "nsa_cmp_k_w1", "nsa_cmp_k_w2", "nsa_cmp_v_w1", "nsa_cmp_v_w2", "nsa_w_out", "ret_w_in", "ret_w_out"]
_CACHE = {}


def build(debug=False, upto=6):
    nc = bass.Bass("TRN2", target_bir_lowering=False)
    I = {}

    def din(name, shape, dt=F32):
        I[name] = nc.dram_tensor(name, list(shape), dt, kind="ExternalInput").ap()

    din("x", [4096, 1024]); din("p0", [4096, 256]); din("p1", [4096, 256]); din("norm_g", [2, 1024])
    din("nsa_w_in", [1024, 3632])
    for nm in ["nsa_q_g", "nsa_ks_g", "nsa_kw_g", "nsa_kc_g"]:
        din(nm, [1, 64])
    din("nsa_cmp_k_w1", [2048, 256]); din("nsa_cmp_v_w1", [2048, 256]); din("nsa_cmp_k_w2", [256, 64]); din("nsa_cmp_v_w2", [256, 64])
    din("nsa_cmp_pos_k", [32, 64]); din("nsa_cmp_pos_v", [32, 64])
    din("nsa_w_out", [1024, 1024]); din("ret_w_in", [1024, 6144]); din("ret_w_out", [2048, 1024])
    din("ple_w0", [256, 1024]); din("ple_w1", [256, 1024]); din("ple_gate_w0", [1024, 1024]); din("ple_gate_w1", [1024, 1024])
    din("ident", [128, 128], BF16); din("overlap", [256, 64]); din("Eexp", [64, 4096], BF16); din("Ftab", [128, 32, 64])
    din("cmpb", [32, 2, 128, 128], BF16); din("wbias", [2, 128, 128], BF16)
    din("decT", [128, 4, 128]); din("qdT", [128, 4, 128]); din("kdec", [128, 4]); din("rope", [4096, 4, 128])
    out = nc.dram_tensor("out", [4096, 1024], F32, kind="ExternalOutput").ap()
    SC = declare_scratch(nc, debug); SC.update(declare_scratch2(nc, debug)); SC.update(declare_scratch3(nc, debug))
    SC.update(declare_scratch4(nc, debug)); SC.update(declare_scratch5(nc, debug))
    C = make_consts()
    with ExitStack() as es:
        G = Glob(nc, es)
        phase1(nc, G, I, SC)
        if upto >= 2:
            phase2(nc, G, I, SC)
        if upto >= 3:
            phase3(nc, G, I, SC)
        g1 = bass.AP(I["norm_g"].tensor, 1024, [[0, 128], [1, 1024]])
        if upto >= 4:
          epilogue(nc, G, "e0_", SC["outg"], 1024, I["nsa_w_out"], I["x"], I["p0"], I["ple_gate_w0"], I["ple_w0"],
                 SC["x2"], I["ident"], norm_g_row=g1, hT_out=SC["h2T"])
        if upto >= 5:
            phase5(nc, G, I, SC, C["chunk_decay"])
        if upto >= 6:
          epilogue(nc, G, "e1_", SC["og"], 2048, I["ret_w_out"], SC["x2"], I["p1"], I["ple_gate_w1"], I["ple_w1"],
                 out, I["ident"])
    return nc


def make_in_maps(inputs, cores):
    C = make_consts()
    shared = {}
    f32 = lambda a: np.ascontiguousarray(np.asarray(a, dtype=np.float32))
    shared["norm_g"] = f32(inputs["norm_g"])
    for nm in ["nsa_w_in", "nsa_q_g", "nsa_kc_g", "nsa_ks_g", "nsa_kw_g", "nsa_cmp_pos_k", "nsa_cmp_pos_v",
               "nsa_cmp_k_w1", "nsa_cmp_k_w2", "nsa_cmp_v_w1", "nsa_cmp_v_w2", "nsa_w_out", "ret_w_in", "ret_w_out"]:
        a = f32(inputs[nm])[0]
        if a.ndim == 1:
            a = a[None, :]
        shared[nm] = np.ascontiguousarray(a)
    for l in range(2):
        shared[f"ple_w{l}"] = f32(inputs["ple_w"])[l]
        shared[f"ple_gate_w{l}"] = f32(inputs["ple_gate_w"])[l]
    for k in ["ident", "overlap", "Eexp", "Ftab", "cmpb", "wbias", "decT", "qdT", "kdec", "rope"]:
        shared[k] = C[k]
    x = f32(inputs["x"]); p = f32(inputs["p"])
    maps = []
    for b in cores:
        m = dict(shared)
        m["x"] = x[b]; m["p0"] = p[0, b]; m["p1"] = p[1, b]
        maps.append(m)
    return maps


def kernel(**inputs):
    if "nc" not in _CACHE:
        _CACHE["nc"] = build()
    nc = _CACHE["nc"]
    maps = make_in_maps(inputs, list(range(8)))
    res = run_bass_kernel_spmd(nc, maps, core_ids=list(range(8)))
    return np.stack([np.asarray(r["out"], dtype=np.float32) for r in res.results], axis=0)
```

```python
import numpy as np
import ml_dtypes
import concourse.bass as bass
import concourse.mybir as mybir
from contextlib import ExitStack

F32 = mybir.dt.float32
BF16 = mybir.dt.bfloat16
AF = mybir.ActivationFunctionType
ALU = mybir.AluOpType
AX = mybir.AxisListType
ENGS = ("sync", "scalar", "vector", "gpsimd", "tensor")
SKIP_SAME = {"tensor"}


class Buf:
    __slots__ = ("w", "r", "name", "dsem")

    def __init__(self, name=""):
        self.w = None
        self.r = {}
        self.name = name
        self.dsem = None


class Glob:
    def __init__(self, nc, es, n_dma_sems=96):
        self.nc = nc
        self.esem = {e: es.enter_context(nc.semaphore("es_" + e)) for e in ENGS}
        self.cnt = {e: 0 for e in ENGS}
        self.dsems = [es.enter_context(nc.semaphore(f"ds{i}")) for i in range(n_dma_sems)]
        self.dcnt = [0] * n_dma_sems
        self.next_dsem = 0
        self.seen = {e: {} for e in ENGS}

    def sem_of(self, key):
        if isinstance(key, str):
            return self.esem[key]
        return self.dsems[key]

    def alloc_dsem(self):
        i = self.next_dsem
        assert i < len(self.dsems), "out of dma semaphores"
        self.next_dsem += 1
        return i


class Sched:
    def __init__(self, G):
        self.G = G
        G.next_dsem = 0
        self.q = {e: [] for e in ENGS}
        self.pending_dma = []

    def _wait(self, eng, tok):
        if tok is None:
            return
        key, val = tok
        if eng == key and eng in SKIP_SAME:
            return
        seen = self.G.seen[eng]
        if seen.get(key, 0) >= val:
            return
        seen[key] = val
        sem = self.G.sem_of(key)
        self.q[eng].append(lambda e, sem=sem, val=val: e.wait_ge(sem, val))

    def _deps(self, eng, reads, writes, extra):
        for b in reads:
            self._wait(eng, b.w)
        for b in writes:
            self._wait(eng, b.w)
            for k, v in b.r.items():
                self._wait(eng, (k, v))
        for t in extra:
            self._wait(eng, t)

    @staticmethod
    def _mark(tok, reads, writes):
        k, v = tok
        for b in reads:
            if b.r.get(k, 0) < v:
                b.r[k] = v
        for b in writes:
            b.w = tok
            b.r = {}

    def op(self, eng, fn, reads=(), writes=(), extra=()):
        G = self.G
        self._deps(eng, reads, writes, extra)
        G.cnt[eng] += 1
        tok = (eng, G.cnt[eng])
        sem = G.esem[eng]
        self.q[eng].append(lambda e, fn=fn, sem=sem: fn(e).then_inc(sem, 1))
        self._mark(tok, reads, writes)
        return tok

    def dma(self, q, pairs, reads=(), writes=(), extra=(), **kw):
        G = self.G
        bufs = list(writes) + list(reads)
        b0 = bufs[0]
        if b0.dsem is None:
            b0.dsem = G.alloc_dsem()
        si = b0.dsem
        self._deps(q, reads, writes, extra)
        sem = G.dsems[si]
        for (o, i) in pairs:
            G.dcnt[si] += 16
            self.q[q].append(lambda e, o=o, i=i, sem=sem, kw=kw: e.dma_start(out=o, in_=i, **kw).then_inc(sem, 16))
        tok = (si, G.dcnt[si])
        self._mark(tok, reads, writes)
        self.pending_dma.append(tok)
        return tok

    def finish(self, nc):
        for t in self.pending_dma:
            self._wait("sync", t)
            self._wait("gpsimd", t)
        with nc.Block() as block:
            for e in ENGS:
                lst = self.q[e]

                def body(eng, lst=lst):
                    for f in lst:
                        f(eng)
                getattr(block, e)(body)

    def act(self, out, in_, func, reads=(), writes=(), eng="scalar", **kw):
        return self.op(eng, lambda e: e.activation(out=out, in_=in_, func=func, **kw), reads, writes)

    def mm(self, out, lhsT, rhs, start, stop, reads=(), writes=(), **kw):
        return self.op("tensor", lambda e: e.matmul(out, lhsT, rhs, start=start, stop=stop, **kw), reads, writes)

    def tr(self, out, in_, ident, reads=(), writes=()):
        return self.op("tensor", lambda e: e.transpose(out, in_, ident), reads, writes)

    def tt(self, eng, out, in0, in1, op, reads=(), writes=()):
        return self.op(eng, lambda e: e.tensor_tensor(out=out, in0=in0, in1=in1, op=op), reads, writes)

    def ts(self, eng, out, in0, s1, s2, op0, op1=None, reads=(), writes=(), **kw):
        if op1 is None:
            return self.op(eng, lambda e: e.tensor_scalar(out=out, in0=in0, scalar1=s1, scalar2=None, op0=op0, **kw), reads, writes)
        return self.op(eng, lambda e: e.tensor_scalar(out=out, in0=in0, scalar1=s1, scalar2=s2, op0=op0, op1=op1, **kw), reads, writes)

    def stt(self, out, in0, scalar, in1, op0, op1, reads=(), writes=(), eng="vector"):
        return self.op(eng, lambda e: e.scalar_tensor_tensor(out=out, in0=in0, scalar=scalar, in1=in1, op0=op0, op1=op1), reads, writes)

    def copy(self, eng, out, in_, reads=(), writes=()):
        if eng == "scalar":
            return self.op(eng, lambda e: e.copy(out=out, in_=in_), reads, writes)
        return self.op(eng, lambda e: e.tensor_copy(out=out, in_=in_), reads, writes)

    def memset(self, eng, ap, val, writes=()):
        return self.op(eng, lambda e: e.memset(ap, val), (), writes)

    def reduce(self, out, in_, op, axis, reads=(), writes=(), eng="vector"):
        return self.op(eng, lambda e: e.tensor_reduce(out=out, in_=in_, axis=axis, op=op), reads, writes)


def bc(ap, n):
    return bass.AP(ap.tensor, ap.offset, [list(x) for x in ap.ap] + [[0, n]])


def mkap(ap, dims):
    return bass.AP(ap.tensor, ap.offset, [list(ap.ap[0])] + [list(d) for d in dims])


BIG = 30000.0
def make_consts():
    c = {}
    c["ident"] = np.eye(128, dtype=ml_dtypes.bfloat16)
    i = np.arange(256)[:, None]; j = np.arange(64)[None, :]
    ov = ((i * 16 < (j + 1) * 64) & (i * 16 + 32 > j * 64)).astype(np.float32)
    ov[255] = 0
    c["overlap"] = ov
    E = (np.arange(4096)[None, :] // 64 == np.arange(64)[:, None]).astype(np.float32)
    c["Eexp"] = E.astype(ml_dtypes.bfloat16)
    t = (np.arange(32)[None, :, None] * 128 + np.arange(128)[:, None, None])
    cur = t // 64
    b = np.arange(64)[None, None, :]
    valid = b <= cur
    forced = (b == 0) | (b == cur) | (b == cur - 1)
    F = np.where(valid, np.where(forced, 1e4, 0.0), -1e30).astype(np.float32)
    c["Ftab"] = np.ascontiguousarray(F)
    n = (np.arange(2)[None, :, None, None] * 128 + np.arange(128)[None, None, :, None])
    tt = (np.arange(32)[:, None, None, None] * 128 + np.arange(128)[None, None, None, :])
    cb = np.where(16 * n + 31 > tt, -BIG, 0.0).astype(np.float32)
    c["cmpb"] = cb.astype(ml_dtypes.bfloat16)
    ii = np.arange(128)[:, None]; uu = np.arange(128)[None, :]
    wb = np.stack([np.where(ii > uu, -BIG, 0.0), np.where(ii <= uu, -BIG, 0.0)]).astype(np.float32)
    c["wbias"] = wb.astype(ml_dtypes.bfloat16)
    H, C = 4, 128
    log_g = np.log(1.0 - 2.0 ** (-5.0 - np.arange(H, dtype=np.float64)))
    ix = np.arange(C, dtype=np.float64)
    diff = ix[:, None] - ix[None, :]
    intra = np.where(diff >= 0, np.exp(log_g[:, None, None] * np.maximum(diff, 0.0)), 0.0)
    c["decT"] = np.ascontiguousarray(intra.transpose(2, 0, 1)).astype(np.float32)
    q_decay = np.exp(log_g[:, None] * (ix + 1.0))
    c["qdT"] = np.ascontiguousarray(np.broadcast_to(q_decay[None], (128, H, C))).astype(np.float32)
    k_decay = np.exp(log_g[:, None] * (C - 1.0 - ix))
    c["kdec"] = np.ascontiguousarray(k_decay.T).astype(np.float32)
    c["chunk_decay"] = np.exp(log_g * C)
    half = 128
    inv = (np.float32(10000.0) ** (-np.linspace(0.0, 1.0, half, dtype=np.float32))).astype(np.float32)
    pos = np.arange(4096, dtype=np.float32)
    ang = (pos[:, None] * inv[None, :]).astype(np.float32).astype(np.float64)
    cs, sn = np.cos(ang), np.sin(ang)
    c["rope"] = np.ascontiguousarray(np.stack([cs, sn, cs / 16.0, sn / 16.0], axis=1)).astype(np.float32)
    return c


S_, D_ = 4096, 1024
NT = 32
EPS = 1e-6
NSA_COLS = 3632


def declare_scratch(nc, debug):
    kind = dict(kind="ExternalOutput") if debug else {}
    dbg = debug if isinstance(debug, (set, list, tuple)) else None
    d = {}
    d["qT"] = nc.dram_tensor("qT_s", [16, 64, S_], BF16, **(kind if (dbg is None or "qT" in dbg) else {})).ap()
    d["kvT"] = nc.dram_tensor("kvT_s", [16, 64, S_], BF16, **(kind if (dbg is None or "kvT" in dbg) else {})).ap()
    d["vsA"] = nc.dram_tensor("vsA_s", [NT, 128, 260], BF16, **(kind if (dbg is None or "vsA" in dbg) else {})).ap()
    d["vwA"] = nc.dram_tensor("vwA_s", [NT, 128, 260], BF16, **(kind if (dbg is None or "vwA" in dbg) else {})).ap()
    d["gates"] = nc.dram_tensor("gates_s", [S_, 48], F32, **(kind if (dbg is None or "gates" in dbg) else {})).ap()
    d["sz"] = nc.dram_tensor("sz_s", [S_, 1024], BF16, **(kind if (dbg is None or "sz" in dbg) else {})).ap()
    return d


def phase1(nc, G, I, SC, ntg=8):
    S = Sched(G)
    with ExitStack() as es:
        def sb(name, shape, dt):
            return es.enter_context(nc.sbuf_tensor("p1_" + name, shape, dt))

        def ps(name, shape, dt):
            return es.enter_context(nc.psum_tensor("p1_" + name, shape, dt))

        w_sb = sb("w_sb", [128, 8, NSA_COLS], BF16)
        gb = sb("gb", [128, 1024], F32)
        gcol = sb("gcol", [64, 4], F32)
        ident = sb("ident", [128, 128], BF16)
        mhalf = sb("mhalf", [128, 8], F32)
        xt = [sb(f"xt{i}", [128, 1024], F32) for i in range(2)]
        junk = sb("junk", [128, 1024], BF16)
        ssx = [sb(f"ssx{i}", [128, 2], F32) for i in range(2)]
        hb = [sb(f"hb{i}", [128, 1024], BF16) for i in range(2)]
        hT = [sb(f"hT{i}", [128, 8, 128], BF16) for i in range(2)]
        sq = [sb(f"sq{i}", [128, 512], F32) for i in range(2)]
        qf = [sb(f"qf{i}", [128, 512], F32) for i in range(2)]
        ss = [sb(f"ss{i}", [128, 8], F32) for i in range(2)]
        rs = [sb(f"rs{i}", [128, 8], F32) for i in range(2)]
        qn = [sb(f"qn{i}", [128, 512], BF16) for i in range(4)]
        zt = [sb(f"zt{i}", [128, 512], F32) for i in range(2)]
        zh = [sb(f"zh{i}", [128, 512], F32) for i in range(2)]
        gtmp = sb("gtmp", [128, 48], F32)
        qTt = [sb(f"qTt{i}", [64, 16, 512], BF16) for i in range(2)]
        kvTt = [sb(f"kvTt{i}", [64, 16, 512], BF16) for i in range(2)]
        vsAt = [sb(f"vsAt{i}", [128, 4, 260], BF16) for i in range(2)]
        vwAt = [sb(f"vwAt{i}", [128, 4, 260], BF16) for i in range(2)]
        szt = [sb(f"szt{i}", [128, 4, 1024], BF16) for i in range(2)]
        gt = [sb(f"gt{i}", [128, 4, 48], F32) for i in range(2)]

        pj = [ps(f"pj{i}", [128, 512], F32) for i in range(4)]
        tpa = ps("tpa", [128, 1024], BF16)
        tpq = [ps(f"tpq{i}", [64, 1024], BF16) for i in range(2)]

        B = lambda n: Buf(n)
        b_w = B("w"); b_c = B("consts")
        b_xt = [B("xt") for _ in range(2)]; b_junk = B("junk"); b_ssx = [B("ssx") for _ in range(2)]
        b_hb = [B("hb") for _ in range(2)]; b_hT = [B("hT") for _ in range(2)]
        b_sq = [B("sq") for _ in range(2)]; b_qf = [B("qf") for _ in range(2)]; b_ss = [B("ss") for _ in range(2)]; b_rs = [B("rs") for _ in range(2)]
        b_qn = [B("qn") for _ in range(4)]; b_zt = [B("zt") for _ in range(2)]; b_zh = [B("zh") for _ in range(2)]
        b_gtmp = B("gtmp")
        b_qTt = [B("qTt") for _ in range(2)]; b_kvTt = [B("kvTt") for _ in range(2)]
        b_vsAt = [B("vsAt") for _ in range(2)]; b_vwAt = [B("vwAt") for _ in range(2)]
        b_szt = [B("szt") for _ in range(2)]; b_gt = [B("gt") for _ in range(2)]
        b_pj = [B("pj") for _ in range(4)]; b_tpa = B("tpa"); b_tpq = [B("tpq") for _ in range(2)]

        S.dma("sync", [(ident[:], I["ident"])], writes=[b_c])
        S.dma("sync", [(gb[:], bass.AP(I["norm_g"].tensor, 0, [[0, 128], [1, 1024]]))], writes=[b_c])
        for j, nm in enumerate(["nsa_q_g", "nsa_ks_g", "nsa_kw_g"]):
            S.dma("sync", [(gcol[:, j:j + 1], bass.AP(I[nm].tensor, 0, [[1, 64], [1, 1]]))], writes=[b_c])
        S.ts("vector", gcol[:, 0:1], gcol[:, 0:1], 0.125, None, ALU.mult, reads=[b_c], writes=[b_c])
        S.memset("vector", mhalf[:], -0.5, writes=[b_c])
        for i in range(2):
            S.memset("vector", vsAt[i][:], 1.0, writes=[b_vsAt[i]])
            S.memset("vector", vwAt[i][:], 1.0, writes=[b_vwAt[i]])
        half = NSA_COLS // 2
        b_wk = [Buf() for _ in range(8)]
        wt = []
        for kc in range(8):
            wt.append(S.dma("gpsimd", [(w_sb[:, kc, c0:c0 + half], I["nsa_w_in"][kc * 128:(kc + 1) * 128, c0:c0 + half])
                                       for c0 in (0, half)], writes=[b_wk[kc]], extra=wt[-2:-1] if len(wt) >= 2 else ()))

        nseg = 0
        tq = 0
        nqn = 0
        defer = []

        def flush(keep):
            while len(defer) > keep:
                defer.pop(0)()

        def norm_heads(pj_ap, b_pjn, nh, out_ap3, b_out):
            nonlocal nseg
            k = nseg % 2
            nseg += 1
            n = nh * 64
            S.act(sq[k][:, 0:n], pj_ap, AF.Square, reads=[b_pjn], writes=[b_sq[k]])
            S.act(qf[k][:, 0:n], pj_ap, AF.Copy, reads=[b_pjn], writes=[b_qf[k]])
            S.reduce(ss[k][:, 0:nh], sq[k][:, 0:n].rearrange("p (h d) -> p h d", d=64), ALU.add, AX.X,
                     reads=[b_sq[k]], writes=[b_ss[k]])
            S.ts("vector", ss[k][:, 0:nh], ss[k][:, 0:nh], 1.0 / 64, EPS, ALU.mult, ALU.add,
                 reads=[b_ss[k]], writes=[b_ss[k]])
            S.tt("gpsimd", rs[k][:, 0:nh], ss[k][:, 0:nh], mhalf[:, 0:nh], ALU.pow,
                 reads=[b_ss[k], b_c], writes=[b_rs[k]])
            S.tt("vector", out_ap3, qf[k][:, 0:n].rearrange("p (h d) -> p h d", d=64), bc(rs[k][:, 0:nh], 64), ALU.mult,
                 reads=[b_qf[k], b_rs[k]], writes=[b_out])

        def front(ti, part):
            xp = ti % 2
            if part == 1:
                for kc in range(8):
                    S.tr(tpa[:, kc * 128:(kc + 1) * 128], hb[xp][:, kc * 128:(kc + 1) * 128], ident[:],
                         reads=[b_hb[xp], b_c], writes=[b_tpa])
                S.copy("scalar", hT[xp][:].rearrange("p a b -> p (a b)"), tpa[:], reads=[b_tpa], writes=[b_hT[xp]])
                return
            S.dma("sync", [(xt[xp][:], I["x"][ti * 128:(ti + 1) * 128, :])], writes=[b_xt[xp]])
            S.act(junk[:], xt[xp][:], AF.Square, reads=[b_xt[xp]], writes=[b_junk, b_ssx[xp]],
                  accum_out=ssx[xp][:, 0:1])
            S.ts("vector", ssx[xp][:, 0:1], ssx[xp][:, 0:1], 1.0 / 1024, EPS, ALU.mult, ALU.add,
                 reads=[b_ssx[xp]], writes=[b_ssx[xp]])
            S.tt("gpsimd", ssx[xp][:, 1:2], ssx[xp][:, 0:1], mhalf[:, 0:1], ALU.pow,
                 reads=[b_ssx[xp], b_c], writes=[b_ssx[xp]])
            S.stt(hb[xp][:], xt[xp][:], ssx[xp][:, 1:2], gb[:], ALU.mult, ALU.mult,
                  reads=[b_xt[xp], b_ssx[xp], b_c], writes=[b_hb[xp]])


        for tg in range(ntg):
            sp = tg % 2
            for tl in range(4):
                ti = tg * 4 + tl
                xp = ti % 2
                if ti == 0:
                    front(0, 0); front(0, 1)
                if ti + 1 < ntg * 4:
                    front(ti + 1, 0)
                segs = [(0, 512, "q0"), (512, 512, "q1"), (1024, 512, "kcvc"), (1536, 512, "ksvs"),
                        (2048, 512, "kwvw"), (2608, 512, "z0"), (3120, 512, "z1"), (2560, 48, "gl")]
                for si, (c0, n, kind) in enumerate(segs):
                    pb = si % 4
                    if si == 5 and ti + 1 < ntg * 4:
                        front(ti + 1, 1)
                    flush(2)
                    for kc in range(8):
                        S.mm(pj[pb][:, 0:n], hT[xp][:, kc, :], w_sb[:, kc, c0:c0 + n], kc == 0, kc == 7,
                             reads=[b_hT[xp], b_wk[kc]], writes=[b_pj[pb]])
                    if kind in ("q0", "q1"):
                        qk = nqn % 4; nqn += 1
                        norm_heads(pj[pb][:, 0:512], b_pj[pb], 8, qn[qk][:].rearrange("p (h d) -> p h d", d=64), b_qn[qk])
                        h0 = 0 if kind == "q0" else 8

                        def C(qk=qk, h0=h0, sp=sp, tl=tl):
                            nonlocal tq
                            tk = tq % 2; tq += 1
                            for h in range(8):
                                S.tr(tpq[tk][:, h * 128:(h + 1) * 128], qn[qk][:, h * 64:(h + 1) * 64], ident[:],
                                     reads=[b_qn[qk], b_c], writes=[b_tpq[tk]])
                            S.act(qTt[sp][:, h0:h0 + 8, tl * 128:(tl + 1) * 128],
                                  tpq[tk][:].rearrange("p (h t) -> p h t", t=128), AF.Copy,
                                  reads=[b_tpq[tk], b_c], writes=[b_qTt[sp]], scale=gcol[:, 0:1])
                        defer.append(C)
                    elif kind == "kcvc":
                        qk = nqn % 4; nqn += 1
                        S.copy("scalar", qn[qk][:], pj[pb][:, 0:512], reads=[b_pj[pb]], writes=[b_qn[qk]])

                        def C(qk=qk, sp=sp, tl=tl):
                            nonlocal tq
                            tk = tq % 2; tq += 1
                            for h in range(8):
                                S.tr(tpq[tk][:, h * 128:(h + 1) * 128], qn[qk][:, h * 64:(h + 1) * 64], ident[:],
                                     reads=[b_qn[qk], b_c], writes=[b_tpq[tk]])
                            S.copy("vector", kvTt[sp][:, 0:8, tl * 128:(tl + 1) * 128],
                                   tpq[tk][:].rearrange("p (h t) -> p h t", t=128),
                                   reads=[b_tpq[tk]], writes=[b_kvTt[sp]])
                        defer.append(C)
                    elif kind in ("ksvs", "kwvw"):
                        qk = nqn % 4; nqn += 1
                        norm_heads(pj[pb][:, 0:256], b_pj[pb], 4,
                                   qn[qk][:, 0:256].rearrange("p (h d) -> p h d", d=64), b_qn[qk])
                        r0, gc = (8, 1) if kind == "ksvs" else (12, 2)

                        def C(qk=qk, sp=sp, tl=tl, r0=r0, gc=gc):
                            nonlocal tq
                            tk = tq % 2; tq += 1
                            for h in range(4):
                                S.tr(tpq[tk][:, h * 128:(h + 1) * 128], qn[qk][:, h * 64:(h + 1) * 64], ident[:],
                                     reads=[b_qn[qk], b_c], writes=[b_tpq[tk]])
                            S.act(kvTt[sp][:, r0:r0 + 4, tl * 128:(tl + 1) * 128],
                                  tpq[tk][:, 0:512].rearrange("p (h t) -> p h t", t=128), AF.Copy,
                                  reads=[b_tpq[tk], b_c], writes=[b_kvTt[sp]], scale=gcol[:, gc:gc + 1])
                        defer.append(C)
                        vt, bvt = (vsAt, b_vsAt) if kind == "ksvs" else (vwAt, b_vwAt)
                        S.copy("vector", vt[sp][:, tl, :].rearrange("p (g c) -> p g c", c=65)[:, :, 0:64],
                               pj[pb][:, 256:512].rearrange("p (g c) -> p g c", c=64),
                               reads=[b_pj[pb]], writes=[bvt[sp]])
                    elif kind in ("z0", "z1"):
                        zk = nseg % 2; nseg += 1
                        S.act(zt[zk][:], pj[pb][:, 0:512], AF.Tanh, reads=[b_pj[pb]], writes=[b_zt[zk]], scale=0.5)
                        S.act(zh[zk][:], pj[pb][:, 0:512], AF.Copy, reads=[b_pj[pb]], writes=[b_zh[zk]], scale=0.5)
                        z0 = 0 if kind == "z0" else 512
                        S.stt(szt[sp][:, tl, z0:z0 + 512], zt[zk][:], 1.0, zh[zk][:], ALU.add, ALU.mult,
                              reads=[b_zt[zk], b_zh[zk]], writes=[b_szt[sp]])
                    else:
                        S.act(gtmp[:], pj[pb][:, 0:48], AF.Tanh, reads=[b_pj[pb]], writes=[b_gtmp], scale=0.5)
                        S.ts("vector", gt[sp][:, tl, :], gtmp[:], 0.5, 0.5, ALU.mult, ALU.add,
                             reads=[b_gtmp], writes=[b_gt[sp]])
            flush(0)
            t0 = tg * 512
            S.dma("sync", [(SC["qT"].rearrange("h d t -> d h t")[:, :, t0:t0 + 512], qTt[sp][:])], reads=[b_qTt[sp]])
            S.dma("sync", [(SC["kvT"].rearrange("h d t -> d h t")[:, :, t0:t0 + 512], kvTt[sp][:])], reads=[b_kvTt[sp]])
            S.dma("sync", [(SC["vsA"][tg * 4:(tg + 1) * 4].rearrange("t p c -> p t c"), vsAt[sp][:])], reads=[b_vsAt[sp]])
            S.dma("sync", [(SC["vwA"][tg * 4:(tg + 1) * 4].rearrange("t p c -> p t c"), vwAt[sp][:])], reads=[b_vwAt[sp]])
            S.dma("sync", [(SC["sz"][t0:t0 + 512, :].rearrange("(t p) c -> p t c", p=128), szt[sp][:])], reads=[b_szt[sp]])
            S.dma("sync", [(SC["gates"][t0:t0 + 512, :].rearrange("(t p) c -> p t c", p=128), gt[sp][:])], reads=[b_gt[sp]])
        S.finish(nc)


EPS = 1e-6


def declare_scratch2(nc, debug):
    kind = dict(kind="ExternalOutput") if debug else {}
    dbg = debug if isinstance(debug, (set, list, tuple)) else None
    d = {}
    d["kccT"] = nc.dram_tensor("kccT_s", [64, 4, 256], BF16, **(kind if (dbg is None or "kccT" in dbg) else {})).ap()
    d["vcA"] = nc.dram_tensor("vcA_s", [128, 2, 4, 128], BF16, **(kind if (dbg is None or "vcA" in dbg) else {})).ap()
    return d


def phase2(nc, G, I, SC):
    S = Sched(G)
    with ExitStack() as es:
        def sb(name, shape, dt):
            return es.enter_context(nc.sbuf_tensor("p2_" + name, shape, dt))

        def ps(name, shape, dt):
            return es.enter_context(nc.psum_tensor("p2_" + name, shape, dt))

        kvT = sb("kvT", [64, 8, 4096], BF16)
        W1 = [sb(f"W1{i}", [64, 32, 256], BF16) for i in range(2)]
        W2 = [sb(f"W2{i}", [128, 2, 64], BF16) for i in range(2)]
        posf = sb("posf", [64, 2, 32], F32)
        posb = sb("posb", [64, 2, 32], BF16)
        c1h = sb("c1h", [128, 4], F32)
        ident = sb("ident", [128, 128], BF16)
        ovf = sb("ovf", [128, 2, 64], F32)
        gk = sb("gk", [64, 1], F32)
        mhalf = sb("mhalf", [128, 8], F32)
        th = [sb(f"th{i}", [128, 256], F32) for i in range(2)]
        uu = [sb(f"uu{i}", [128, 256], F32) for i in range(2)]
        hid = [[sb(f"hid{a}{b}", [128, 256], BF16) for b in range(2)] for a in range(2)]
        sq = sb("sq", [128, 256], F32)
        ss = sb("ss", [128, 4], F32)
        rs = sb("rs", [128, 4], F32)
        kn = sb("kn", [128, 256], BF16)
        kccT = sb("kccT", [64, 4, 256], BF16)
        vcA = sb("vcA", [128, 2, 4, 128], BF16)

        ph = [ps(f"ph{i}", [128, 512], F32) for i in range(2)]
        pc1 = ps("pc1", [128, 512], F32)
        po = ps("po", [128, 512], F32)
        tp = ps("tp", [64, 1024], BF16)

        b_kvT = Buf(); b_W1 = [Buf(), Buf()]; b_W2 = [Buf(), Buf()]; b_c = Buf(); b_pos = Buf(); b_c1h = Buf()
        b_th = [Buf(), Buf()]; b_uu = [Buf(), Buf()]; b_hid = [[Buf(), Buf()], [Buf(), Buf()]]
        b_sq = Buf(); b_ss = Buf(); b_rs = Buf(); b_kn = Buf(); b_kccT = Buf(); b_vcA = Buf()
        b_ph = [Buf(), Buf()]; b_pc1 = Buf(); b_po = Buf(); b_tp = Buf()

        S.dma("sync", [(kvT[:, 0:4, :], SC["kvT"][0:4].rearrange("h d t -> d h t")),
                       (kvT[:, 4:8, :], SC["kvT"][4:8].rearrange("h d t -> d h t"))], writes=[b_kvT])
        for kv, nm in enumerate(["nsa_cmp_k_w1", "nsa_cmp_v_w1"]):
            src = I[nm].rearrange("(l d) m -> d l m", d=64)
            S.dma("gpsimd", [(W1[kv][:, l0:l0 + 8, :], src[:, l0:l0 + 8, :]) for l0 in range(0, 32, 8)], writes=[b_W1[kv]])
        for kv, nm in enumerate(["nsa_cmp_k_w2", "nsa_cmp_v_w2"]):
            S.dma("gpsimd", [(W2[kv][:], I[nm].rearrange("(c p) m -> p c m", p=128))], writes=[b_W2[kv]])
        for kv, nm in enumerate(["nsa_cmp_pos_k", "nsa_cmp_pos_v"]):
            S.dma("sync", [(posf[:, kv, :], bass.AP(I[nm].tensor, 0, [[1, 64], [64, 32]]))], writes=[b_pos],
                  allow_slow_non_contiguous=True)
        S.copy("vector", posb[:], posf[:], reads=[b_pos], writes=[b_pos])
        S.dma("sync", [(ident[:], I["ident"])], writes=[b_c])
        S.dma("sync", [(ovf[:], I["overlap"].rearrange("(c p) j -> p c j", p=128))], writes=[b_c])
        S.dma("sync", [(gk[:], bass.AP(I["nsa_kc_g"].tensor, 0, [[1, 64], [1, 1]]))], writes=[b_c])
        S.memset("vector", mhalf[:], -0.5, writes=[b_c])
        for a in range(2):
            for b in range(2):
                S.memset("vector", hid[a][b][:], 0.0, writes=[b_hid[a][b]])
        S.memset("vector", vcA[:], 1.0, writes=[b_vcA])
        S.memset("vector", kccT[:], 0.0, writes=[b_kccT])

        for kv in range(2):
            for hh in range(2):
                col = kv * 2 + hh
                for l in range(32):
                    S.mm(pc1[:, col:col + 1], W1[kv][:, l, hh * 128:(hh + 1) * 128], posb[:, kv, l:l + 1],
                         l == 0, l == 31, reads=[b_W1[kv], b_pos], writes=[b_pc1])
                S.act(c1h[:, col:col + 1], pc1[:, col:col + 1], AF.Copy, reads=[b_pc1], writes=[b_c1h], scale=0.5)

        it = 0
        for kv in range(2):
            for g in range(4):
                par = it % 2
                for hh in range(2):
                    pb = hh
                    col = kv * 2 + hh
                    for l in range(32):
                        rhs = mkap(kvT[:, kv * 4 + g, l:l + 1], [[16, 255]])
                        S.mm(ph[pb][:, 0:255], W1[kv][:, l, hh * 128:(hh + 1) * 128], rhs, l == 0, l == 31,
                             reads=[b_W1[kv], b_kvT], writes=[b_ph[pb]])
                    S.act(th[hh][:, 0:255], ph[pb][:, 0:255], AF.Tanh, reads=[b_ph[pb], b_c1h], writes=[b_th[hh]],
                          scale=0.5, bias=c1h[:, col:col + 1])
                    S.act(uu[hh][:, 0:255], ph[pb][:, 0:255], AF.Identity, reads=[b_ph[pb], b_c1h], writes=[b_uu[hh]],
                          scale=0.5, bias=c1h[:, col:col + 1])
                    S.stt(hid[par][hh][:, 0:255], th[hh][:, 0:255], 1.0, uu[hh][:, 0:255], ALU.add, ALU.mult,
                          reads=[b_th[hh], b_uu[hh]], writes=[b_hid[par][hh]])
                for c in range(2):
                    o_ap = po[:, (c * 4 + g) * 64:(c * 4 + g + 1) * 64]
                    for hh in range(2):
                        S.mm(o_ap, hid[par][hh][:, c * 128:(c + 1) * 128], W2[kv][:, hh, :], hh == 0, hh == 1,
                             reads=[b_hid[par][hh], b_W2[kv]], writes=[b_po], skip_group_check=True)
                it += 1
            for c in range(2):
                src = po[:, c * 256:(c + 1) * 256]
                if kv == 0:
                    S.act(sq[:], src, AF.Square, reads=[b_po], writes=[b_sq])
                    S.reduce(ss[:], sq[:].rearrange("p (h d) -> p h d", d=64), ALU.add, AX.X, reads=[b_sq], writes=[b_ss])
                    S.ts("vector", ss[:], ss[:], 1.0 / 64, EPS, ALU.mult, ALU.add, reads=[b_ss], writes=[b_ss])
                    S.tt("gpsimd", rs[:], ss[:], mhalf[:, 0:4], ALU.pow, reads=[b_ss, b_c], writes=[b_rs])
                    S.tt("vector", kn[:].rearrange("p (h d) -> p h d", d=64), src.rearrange("p (h d) -> p h d", d=64),
                         bc(rs[:], 64), ALU.mult, reads=[b_po, b_rs], writes=[b_kn])
                    for g in range(4):
                        S.tr(tp[:, g * 128:(g + 1) * 128], kn[:, g * 64:(g + 1) * 64], ident[:],
                             reads=[b_kn, b_c], writes=[b_tp])
                    S.act(kccT[:, :, c * 128:(c + 1) * 128], tp[:, 0:512].rearrange("p (g t) -> p g t", t=128), AF.Copy,
                          reads=[b_tp, b_c], writes=[b_kccT], scale=gk[:, 0:1])
                else:
                    S.copy("vector", vcA[:, c, :, 0:64], src.rearrange("p (g d) -> p g d", d=64), reads=[b_po], writes=[b_vcA])
                    S.copy("vector", vcA[:, c, :, 65:128], mkap(ovf[:, c, 0:1], [[0, 4], [1, 63]]), reads=[b_c], writes=[b_vcA])
        S.dma("sync", [(SC["kccT"], kccT[:])], reads=[b_kccT])
        S.dma("sync", [(SC["vcA"], vcA[:])], reads=[b_vcA])
        S.finish(nc)


NEG_BIG = -30000.0


def declare_scratch3(nc, debug):
    kind = dict(kind="ExternalOutput") if debug else {}
    dbg = debug if isinstance(debug, (set, list, tuple)) else None
    d = {}
    d["outg"] = nc.dram_tensor("outg_s", [4096, 1024], BF16, **(kind if (dbg is None or "outg" in dbg) else {})).ap()
    return d


def phase3(nc, G, I, SC, jlist=None, stage=9):
    S = Sched(G)
    jlist = list(range(32)) if jlist is None else jlist
    with ExitStack() as es:
        def sb(name, shape, dt):
            return es.enter_context(nc.sbuf_tensor("p3_" + name, shape, dt))

        def ps(name, shape, dt):
            return es.enter_context(nc.psum_tensor("p3_" + name, shape, dt))

        KE = sb("KE", [128, 4, 4096], BF16)
        kwT = sb("kwT", [64, 4, 4096], BF16)
        vsA = sb("vsA", [128, 32, 260], BF16)
        vwA = sb("vwA", [128, 32, 260], BF16)
        kccT = sb("kccT", [64, 4, 256], BF16)
        vcA = sb("vcA", [128, 2, 4, 128], BF16)
        Ftab = sb("Ftab", [128, 32, 64], F32)
        ident = sb("ident", [128, 128], BF16)
        wbias = sb("wbias", [128, 2, 128], BF16)
        cmpb = [sb(f"cmpb{i}", [128, 2, 128], BF16) for i in range(2)]
        QS = [sb(f"QS{i}", [128, 512], BF16) for i in range(2)]
        gt = [sb(f"gt{i}", [128, 48], F32) for i in range(2)]
        szt = [sb(f"szt{i}", [128, 1024], BF16) for i in range(2)]
        Pt = [sb(f"Pt{i}", [128, 2, 512], BF16) for i in range(2)]
        acc = sb("acc", [128, 16, 64], F32)
        tmp = sb("tmp", [128, 4, 64], F32)
        og = [sb(f"og{i}", [128, 1024], BF16) for i in range(2)]
        zc = sb("zc", [128, 4], F32)
        rz = sb("rz", [128, 4], F32)
        coef = sb("coef", [128, 4], F32)
        score = sb("score", [128, 64], F32)
        work = sb("work", [128, 64], F32)
        m8 = sb("m8", [128, 16], F32)
        selb = sb("selb", [128, 128], BF16)

        pS = [ps(f"pS{i}", [128, 2, 512], F32) for i in range(2)]
        Oc = ps("Oc", [128, 4, 128], F32)
        Ow = ps("Ow", [128, 4, 128], F32)
        Os = ps("Os", [128, 4, 128], F32)
        tps = ps("tps", [128, 1024], BF16)

        b_c = Buf(); bK = {n: Buf() for n in ["KE", "kwT", "vsA", "vwA", "kccT", "vcA"]}
        b_cmpb = [Buf(), Buf()]; b_QSq = [Buf(), Buf()]; b_QSs = [Buf(), Buf()]
        b_gt = [Buf(), Buf()]; b_szt = [Buf(), Buf()]
        b_Pt = [Buf() for _ in range(2)]; b_acc = Buf(); b_tmp = Buf(); b_og = [Buf(), Buf()]
        b_zc = Buf(); b_rz = Buf(); b_coef = Buf(); b_score = Buf(); b_work = Buf(); b_m8 = Buf(); b_selb = Buf()
        b_pS = [Buf() for _ in range(2)]; b_Oc = Buf(); b_Ow = Buf(); b_Os = Buf(); b_tps = Buf()

        S.dma("sync", [(ident[:], I["ident"])], writes=[b_c])
        S.dma("sync", [(Ftab[:], I["Ftab"])], writes=[b_c])
        S.dma("sync", [(wbias[:], I["wbias"].rearrange("w i u -> i w u"))], writes=[b_c])
        S.dma("sync", [(kccT[:], SC["kccT"])], writes=[bK["kccT"]])
        S.dma("sync", [(vcA[:], SC["vcA"])], writes=[bK["vcA"]])
        S.dma("sync", [(KE[0:64, :, :], SC["kvT"][8:12].rearrange("h d t -> d h t"))] + [(KE[64:128, g, :], I["Eexp"]) for g in range(4)], writes=[bK["KE"]])
        S.dma("sync", [(kwT[:], SC["kvT"][12:16].rearrange("h d t -> d h t"))], writes=[bK["kwT"]])
        S.dma("sync", [(vsA[:, a:a + 4, :], SC["vsA"][a:a + 4].rearrange("t p c -> p t c")) for a in range(0, 32, 4)], writes=[bK["vsA"]])
        S.dma("sync", [(vwA[:, a:a + 4, :], SC["vwA"][a:a + 4].rearrange("t p c -> p t c")) for a in range(0, 32, 4)], writes=[bK["vwA"]])
        S.memset("vector", selb[:], 0.0, writes=[b_selb])

        cnt = {"u": 0, "it": 0}

        units = []

        def add_units(tile_list, pre_first=None, pre_last=None, post_last=None):
            us = []
            for a in range(0, len(tile_list), 2):
                us.append(dict(tiles=tile_list[a:a + 2], pre=[], post=[]))
            if pre_first is not None:
                us[0]["pre"].append(pre_first)
            if pre_last is not None:
                us[-1]["pre"].append(pre_last)
            if post_last is not None:
                us[-1]["post"].append(post_last)
            units.extend(us)

        def emit_S(u):
            for f in u["pre"]:
                f()
            ui = cnt["u"] % 2; cnt["u"] += 1
            u["ui"] = ui
            for ti, t in enumerate(u["tiles"]):
                mms = t["mms"]
                for k, (l, r) in enumerate(mms):
                    S.mm(pS[ui][:, ti, :], l, r, k == 0, k == len(mms) - 1, reads=t["reads"], writes=[b_pS[ui]])

        def emit_EP(u):
            ui = u["ui"]
            n = len(u["tiles"])
            S.act(Pt[ui][:, 0:n, :], pS[ui][:, 0:n, :], AF.Exp, reads=[b_pS[ui]], writes=[b_Pt[ui]])
            for ti, t in enumerate(u["tiles"]):
                for h in range(4):
                    S.mm(t["O"](h), Pt[ui][:, ti, h * 128:(h + 1) * 128], t["V"], t["first"] and h == 0, t["last"],
                         reads=[b_Pt[ui], t["bV"]], writes=[t["bO"]], skip_group_check=True)
            for f in u["post"]:
                f()

        for jn, j in enumerate(jlist):
            jp = jn % 2
            t0 = j * 128
            cs = []
            if j <= 16:
                cs.append((0, True))
            else:
                cs.append((0, False))
            if j >= 16:
                cs.append((1, True))
            for g in range(4):
                it = cnt["it"]; cnt["it"] += 1
                qp = it % 2
                qtop = QS[qp][0:64, :]

                def pre_iter(jn=jn, j=j, jp=jp, t0=t0, g=g, qp=qp, cs=cs):
                    if g == 0:
                        S.dma("sync", [(gt[jp][:], SC["gates"][t0:t0 + 128, :])], writes=[b_gt[jp]])
                        S.dma("sync", [(szt[jp][:], SC["sz"][t0:t0 + 128, :])], writes=[b_szt[jp]])
                        S.dma("sync", [(cmpb[jp][:, c, :], I["cmpb"][j, c]) for (c, hb) in cs if hb], writes=[b_cmpb[jp]])
                    S.dma("sync", [(QS[qp][0:64, :].rearrange("p (h t) -> p h t", t=128),
                                    SC["qT"][4 * g:4 * g + 4, :, t0:t0 + 128].rearrange("h d t -> d h t"))],
                          writes=[b_QSq[qp]])

                ctiles = []
                for ci, (c, hasb) in enumerate(cs):
                    mms = [(kccT[:, g, c * 128:(c + 1) * 128], qtop)]
                    rd = [bK["kccT"], b_QSq[qp]]
                    if hasb:
                        mms.append((ident[:], mkap(cmpb[jp][:, c, 0:1], [[0, 4], [1, 128]])))
                        rd = rd + [b_c, b_cmpb[jp]]
                    ctiles.append(dict(mms=mms, reads=rd, O=(lambda h: Oc[:, h, :]), V=vcA[:, c, g, :], bV=bK["vcA"],
                                       bO=b_Oc, first=(ci == 0), last=(ci == len(cs) - 1)))

                def post_cmp(j=j, jp=jp, g=g, qp=qp):
                    S.ts("vector", zc[:], Oc[:, :, 64], 1e-30, None, ALU.max, reads=[b_Oc], writes=[b_zc])
                    S.op("vector", lambda e: e.reciprocal(out=rz[:], in_=zc[:]), reads=[b_zc], writes=[b_rz])
                    S.copy("vector", score[:], Ftab[:, j, :], reads=[b_c], writes=[b_score])
                    for h in range(4):
                        S.stt(score[:, 0:63], Oc[:, h, 65:128], rz[:, h:h + 1], score[:, 0:63], ALU.mult, ALU.add,
                              reads=[b_Oc, b_rz, b_score], writes=[b_score])
                    S.op("vector", lambda e: e.max(out=m8[:, 0:8], in_=score[:]), reads=[b_score], writes=[b_m8])
                    S.op("vector", lambda e: e.match_replace(out=work[:], in_to_replace=m8[:, 0:8], in_values=score[:],
                                                             imm_value=-3.0e38),
                         reads=[b_score, b_m8], writes=[b_work])
                    S.op("vector", lambda e: e.max(out=m8[:, 8:16], in_=work[:]), reads=[b_work], writes=[b_m8])
                    S.ts("vector", selb[:, 64:128], score[:], m8[:, 15:16], NEG_BIG, ALU.is_lt, ALU.mult,
                         reads=[b_score, b_m8], writes=[b_selb])
                    S.tt("vector", coef[:], rz[:], mkap(gt[jp][:, 12 * g:12 * g + 1], [[3, 4]]), ALU.mult,
                         reads=[b_rz, b_gt[jp]], writes=[b_coef])
                    S.tt("vector", acc[:, 4 * g:4 * g + 4, :], Oc[:, :, 0:64], bc(coef[:], 64), ALU.mult,
                         reads=[b_Oc, b_coef], writes=[b_acc])

                add_units(ctiles, pre_first=pre_iter, post_last=post_cmp)

                kts = [kt for kt in range(j - 4, j + 1) if kt >= 0]
                wtiles = []
                for ki, kt in enumerate(kts):
                    mms = [(kwT[:, g, kt * 128:(kt + 1) * 128], qtop)]
                    rd = [bK["kwT"], b_QSq[qp]]
                    if kt == j:
                        mms.append((ident[:], mkap(wbias[:, 0, 0:1], [[0, 4], [1, 128]])))
                        rd = rd + [b_c]
                    elif kt == j - 4:
                        mms.append((ident[:], mkap(wbias[:, 1, 0:1], [[0, 4], [1, 128]])))
                        rd = rd + [b_c]
                    wtiles.append(dict(mms=mms, reads=rd, O=(lambda h: Ow[:, h, 0:65]), V=vwA[:, kt, g * 65:(g + 1) * 65],
                                       bV=bK["vwA"], bO=b_Ow, first=(ki == 0), last=(ki == len(kts) - 1)))

                def pre_selT(qp=qp):
                    S.tr(tps[:, 0:128], selb[:], ident[:], reads=[b_selb, b_c], writes=[b_tps])
                    S.copy("vector", QS[qp][64:128, :].rearrange("p (h t) -> p h t", t=128),
                           mkap(tps[64:128, 0:1], [[0, 4], [1, 128]]), reads=[b_tps], writes=[b_QSs[qp]])

                def post_win(jp=jp, g=g):
                    S.op("vector", lambda e: e.reciprocal(out=rz[:], in_=Ow[:, :, 64]), reads=[b_Ow], writes=[b_rz])
                    S.tt("vector", coef[:], rz[:], mkap(gt[jp][:, 12 * g + 2:12 * g + 3], [[3, 4]]), ALU.mult,
                         reads=[b_rz, b_gt[jp]], writes=[b_coef])
                    S.tt("vector", tmp[:], Ow[:, :, 0:64], bc(coef[:], 64), ALU.mult,
                         reads=[b_Ow, b_coef], writes=[b_tmp])
                    S.tt("gpsimd", acc[:, 4 * g:4 * g + 4, :], acc[:, 4 * g:4 * g + 4, :], tmp[:], ALU.add,
                         reads=[b_tmp], writes=[b_acc])

                add_units(wtiles, post_last=post_win)

                stiles = []
                for kt in range(j + 1):
                    mms = [(KE[:, g, kt * 128:(kt + 1) * 128], QS[qp][:, :])]
                    rd = [bK["KE"], b_QSq[qp], b_QSs[qp]]
                    if kt == j:
                        mms.append((ident[:], mkap(wbias[:, 0, 0:1], [[0, 4], [1, 128]])))
                        rd = rd + [b_c]
                    stiles.append(dict(mms=mms, reads=rd, O=(lambda h: Os[:, h, 0:65]), V=vsA[:, kt, g * 65:(g + 1) * 65],
                                       bV=bK["vsA"], bO=b_Os, first=(kt == 0), last=(kt == j)))

                def post_sel(jp=jp, g=g, t0=t0):
                    S.op("vector", lambda e: e.reciprocal(out=rz[:], in_=Os[:, :, 64]), reads=[b_Os], writes=[b_rz])
                    S.tt("vector", coef[:], rz[:], mkap(gt[jp][:, 12 * g + 1:12 * g + 2], [[3, 4]]), ALU.mult,
                         reads=[b_rz, b_gt[jp]], writes=[b_coef])
                    S.tt("vector", tmp[:], Os[:, :, 0:64], bc(coef[:], 64), ALU.mult,
                         reads=[b_Os, b_coef], writes=[b_tmp])
                    S.tt("gpsimd", acc[:, 4 * g:4 * g + 4, :], acc[:, 4 * g:4 * g + 4, :], tmp[:], ALU.add,
                         reads=[b_tmp], writes=[b_acc])
                    if g == 3:
                        S.tt("gpsimd", og[jp][:], acc[:].rearrange("p h d -> p (h d)"), szt[jp][:], ALU.mult,
                             reads=[b_acc, b_szt[jp]], writes=[b_og[jp]])
                        S.dma("gpsimd", [(SC["outg"][t0:t0 + 128, :], og[jp][:])], reads=[b_og[jp]])

                add_units(stiles, pre_first=pre_selT, post_last=post_sel)

        if units:
            emit_S(units[0])
            for i in range(len(units)):
                if i + 1 < len(units):
                    emit_S(units[i + 1])
                emit_EP(units[i])
        S.finish(nc)


EPS = 1e-6


def declare_scratch4(nc, debug):
    kind = dict(kind="ExternalOutput") if debug else {}
    dbg = debug if isinstance(debug, (set, list, tuple)) else None
    d = {}
    d["x2"] = nc.dram_tensor("x2_s", [4096, 1024], F32, **(kind if (dbg is None or "x2" in dbg) else {})).ap()
    d["h2T"] = nc.dram_tensor("h2T_s", [8, 128, 4096], BF16, **(kind if (dbg is None or "h2T" in dbg) else {})).ap()
    return d


def epilogue(nc, G, pfx, src, KW, w_out, x_in, p_in, gw_in, pw_in, x_out, ident_in, norm_g_row=None, hT_out=None, ntiles=32):
    S = Sched(G)
    KC = KW // 128
    with ExitStack() as es:
        def sb(name, shape, dt):
            return es.enter_context(nc.sbuf_tensor(pfx + name, shape, dt))

        def ps(name, shape, dt):
            return es.enter_context(nc.psum_tensor(pfx + name, shape, dt))

        wo = sb("wo", [128, KC, 1024], BF16)
        gw = sb("gw", [128, 8, 1024], BF16)
        pw = sb("pw", [128, 2, 1024], BF16)
        ident = sb("ident", [128, 128], BF16)
        gb = sb("gb", [128, 1024], F32)
        mhalf = sb("mhalf", [128, 2], F32)
        ogt = [sb(f"ogt{i}", [128, KW], BF16) for i in range(2)]
        ogT = [sb(f"ogT{i}", [128, KC, 128], BF16) for i in range(2)]
        xt = [sb(f"xt{i}", [128, 1024], F32) for i in range(2)]
        pt = [sb(f"pt{i}", [128, 256], F32) for i in range(2)]
        ptb = sb("ptb", [128, 256], BF16)
        pT = [sb(f"pT{i}", [128, 2, 128], BF16) for i in range(2)]
        x1 = [sb(f"x1{i}", [128, 1024], F32) for i in range(2)]
        x1b = [sb(f"x1b{i}", [128, 1024], BF16) for i in range(2)]
        x1T = [sb(f"x1T{i}", [128, 8, 128], BF16) for i in range(2)]
        th = sb("th", [128, 1024], F32)
        t2 = sb("t2", [128, 1024], F32)
        x2 = [sb(f"x2{i}", [128, 1024], F32) for i in range(2)]
        junk = sb("junk", [128, 1024], BF16)
        ssx = sb("ssx", [128, 2], F32)
        hb = sb("hb", [128, 1024], BF16)
        hTt = [sb(f"hTt{i}", [128, 8, 512], BF16) for i in range(2)]

        tp = [ps(f"tp{i}", [128, 1024], BF16) for i in range(2)]
        py = [ps(f"py{i}", [128, 512], F32) for i in range(2)]
        pg = [ps(f"pg{i}", [128, 512], F32) for i in range(2)]
        pp = [ps(f"pp{i}", [128, 512], F32) for i in range(2)]

        b_w = Buf(); b_gw = Buf(); b_pw = Buf(); b_c = Buf()
        b_ogt = [Buf(), Buf()]; b_ogT = [Buf(), Buf()]; b_xt = [Buf(), Buf()]; b_pt = [Buf(), Buf()]; b_ptb = Buf()
        b_pT = [Buf(), Buf()]; b_x1 = [Buf(), Buf()]; b_x1b = [Buf(), Buf()]; b_x1T = [Buf(), Buf()]; b_th = Buf(); b_t2 = Buf()
        b_x2 = [Buf(), Buf()]; b_junk = Buf(); b_ssx = Buf(); b_hb = Buf(); b_hTt = [Buf(), Buf()]
        b_tp = [Buf(), Buf()]; b_py = [Buf(), Buf()]; b_pg = [Buf(), Buf()]; b_pp = [Buf(), Buf()]

        S.dma("sync", [(ident[:], ident_in)], writes=[b_c])
        if norm_g_row is not None:
            S.dma("sync", [(gb[:], norm_g_row)], writes=[b_c])
        S.memset("vector", mhalf[:], -0.5, writes=[b_c])
        wt = []
        b_wk = [Buf() for _ in range(KC)]
        b_gwk = [Buf() for _ in range(8)]
        for kc in range(0, KC, 2):
            wt.append(S.dma("gpsimd", [(wo[:, kc + a, :], w_out[(kc + a) * 128:(kc + a + 1) * 128, :]) for a in range(2)],
                            writes=[b_wk[kc], b_wk[kc + 1]], extra=wt[-2:-1] if len(wt) >= 2 else ()))
        for kc in range(0, 8, 2):
            wt.append(S.dma("gpsimd", [(gw[:, kc + a, :], gw_in[(kc + a) * 128:(kc + a + 1) * 128, :]) for a in range(2)],
                            writes=[b_gwk[kc], b_gwk[kc + 1]], extra=wt[-2:-1]))
        wt.append(S.dma("gpsimd", [(pw[:, kc, :], pw_in[kc * 128:(kc + 1) * 128, :]) for kc in range(2)], writes=[b_pw], extra=wt[-2:-1]))

        ntp = 0

        def stageA(ti):
            nonlocal ntp
            k = ti % 2
            r0 = ti * 128
            S.dma("sync", [(ogt[k][:], src[r0:r0 + 128, :])], writes=[b_ogt[k]])
            S.dma("sync", [(xt[k][:], x_in[r0:r0 + 128, :])], writes=[b_xt[k]])
            S.dma("sync", [(pt[k][:], p_in[r0:r0 + 128, :])], writes=[b_pt[k]])
            for c0 in range(0, KC, 8):
                q = ntp % 2; ntp += 1
                for kc in range(8):
                    S.tr(tp[q][:, kc * 128:(kc + 1) * 128], ogt[k][:, (c0 + kc) * 128:(c0 + kc + 1) * 128], ident[:],
                         reads=[b_ogt[k], b_c], writes=[b_tp[q]])
                S.copy("vector" if c0 == 0 else "scalar", ogT[k][:, c0:c0 + 8, :].rearrange("p a b -> p (a b)"), tp[q][:],
                       reads=[b_tp[q]], writes=[b_ogT[k]])
            S.copy("gpsimd", ptb[:], pt[k][:], reads=[b_pt[k]], writes=[b_ptb])
            q = ntp % 2; ntp += 1
            for c in range(2):
                S.tr(tp[q][:, c * 128:(c + 1) * 128], ptb[:, c * 128:(c + 1) * 128], ident[:],
                     reads=[b_ptb, b_c], writes=[b_tp[q]])
            S.copy("vector", pT[k][:].rearrange("p a b -> p (a b)"), tp[q][:, 0:256], reads=[b_tp[q]], writes=[b_pT[k]])
            for nb in range(2):
                for kc in range(KC):
                    S.mm(py[nb][:], ogT[k][:, kc, :], wo[:, kc, nb * 512:(nb + 1) * 512], kc == 0, kc == KC - 1,
                         reads=[b_ogT[k], b_wk[kc]], writes=[b_py[nb]])
                S.tt("vector", x1[k][:, nb * 512:(nb + 1) * 512], py[nb][:], xt[k][:, nb * 512:(nb + 1) * 512], ALU.add,
                     reads=[b_py[nb], b_xt[k]], writes=[b_x1[k]])
            S.copy("scalar", x1b[k][:], x1[k][:], reads=[b_x1[k]], writes=[b_x1b[k]])

        def stageB(ti):
            nonlocal ntp
            k = ti % 2
            r0 = ti * 128
            q = ntp % 2; ntp += 1
            for kc in range(8):
                S.tr(tp[q][:, kc * 128:(kc + 1) * 128], x1b[k][:, kc * 128:(kc + 1) * 128], ident[:],
                     reads=[b_x1b[k], b_c], writes=[b_tp[q]])
            S.copy("scalar", x1T[k][:].rearrange("p a b -> p (a b)"), tp[q][:], reads=[b_tp[q]], writes=[b_x1T[k]])
            for nb in range(2):
                for kc in range(8):
                    S.mm(pg[nb][:], x1T[k][:, kc, :], gw[:, kc, nb * 512:(nb + 1) * 512], kc == 0, kc == 7,
                         reads=[b_x1T[k], b_gwk[kc]], writes=[b_pg[nb]])
                for c in range(2):
                    S.mm(pp[nb][:], pT[k][:, c, :], pw[:, c, nb * 512:(nb + 1) * 512], c == 0, c == 1,
                         reads=[b_pT[k], b_pw], writes=[b_pp[nb]])
                sl = slice(nb * 512, (nb + 1) * 512)
                S.act(th[:, sl], pg[nb][:], AF.Tanh, reads=[b_pg[nb]], writes=[b_th], scale=0.5)
                S.stt(t2[:, sl], th[:, sl], 1.0, pp[nb][:], ALU.add, ALU.mult, reads=[b_th, b_pp[nb]], writes=[b_t2])
            S.stt(x2[k][:], t2[:], 0.5, x1[k][:], ALU.mult, ALU.add, reads=[b_t2, b_x1[k]], writes=[b_x2[k]])
            S.dma("gpsimd", [(x_out[r0:r0 + 128, :], x2[k][:])], reads=[b_x2[k]])
            if hT_out is not None:
                tg, tl = divmod(ti, 4)
                sp = tg % 2
                S.act(junk[:], x2[k][:], AF.Square, reads=[b_x2[k]], writes=[b_junk, b_ssx], accum_out=ssx[:, 0:1])
                S.ts("vector", ssx[:, 0:1], ssx[:, 0:1], 1.0 / 1024, EPS, ALU.mult, ALU.add, reads=[b_ssx], writes=[b_ssx])
                S.tt("gpsimd", ssx[:, 1:2], ssx[:, 0:1], mhalf[:, 0:1], ALU.pow, reads=[b_ssx, b_c], writes=[b_ssx])
                S.stt(hb[:], x2[k][:], ssx[:, 1:2], gb[:], ALU.mult, ALU.mult, reads=[b_x2[k], b_ssx, b_c], writes=[b_hb])
                q = ntp % 2; ntp += 1
                for kc in range(8):
                    S.tr(tp[q][:, kc * 128:(kc + 1) * 128], hb[:, kc * 128:(kc + 1) * 128], ident[:],
                         reads=[b_hb, b_c], writes=[b_tp[q]])
                S.copy("scalar", hTt[sp][:, :, tl * 128:(tl + 1) * 128], tp[q][:].rearrange("p (a b) -> p a b", b=128),
                       reads=[b_tp[q]], writes=[b_hTt[sp]])
                if tl == 3:
                    S.dma("gpsimd", [(hT_out.rearrange("k p t -> p k t")[:, :, tg * 512:(tg + 1) * 512], hTt[sp][:])],
                          reads=[b_hTt[sp]])

        stageA(0)
        for ti in range(ntiles):
            if ti + 1 < ntiles:
                stageA(ti + 1)
            stageB(ti)
        S.finish(nc)


GN_EPS = 1e-5
RET_COLS = 6144


def declare_scratch5(nc, debug):
    kind = dict(kind="ExternalOutput") if debug else {}
    dbg = debug if isinstance(debug, (set, list, tuple)) else None
    d = {}
    d["og"] = nc.dram_tensor("og_s", [4096, 2048], BF16, **(kind if (dbg is None or "og" in dbg) else {})).ap()
    return d


def phase5(nc, G, I, SC, chunk_decay, ntiles=32):
    S = Sched(G)
    with ExitStack() as es:
        def sb(name, shape, dt):
            return es.enter_context(nc.sbuf_tensor("p5_" + name, shape, dt))

        def ps(name, shape, dt):
            return es.enter_context(nc.psum_tensor("p5_" + name, shape, dt))

        wr = sb("wr", [128, 8, RET_COLS], BF16)
        ident = sb("ident", [128, 128], BF16)
        decT = sb("decT", [128, 4, 128], F32)
        qdT = sb("qdT", [128, 4, 128], F32)
        kdec = sb("kdec", [128, 4], F32)
        mhalf = sb("mhalf", [128, 4], F32)
        st32 = sb("st32", [128, 4, 2, 512], F32)
        stb = sb("stb", [128, 4, 2, 512], BF16)
        hT = [sb(f"hT{i}", [128, 8, 128], BF16) for i in range(2)]
        rope = [sb(f"rope{i}", [128, 4, 128], F32) for i in range(2)]
        ra = sb("ra", [128, 2, 128], F32)
        rb = sb("rb", [128, 2, 128], F32)
        qr2 = [sb(f"qr{i}", [128, 4, 256], BF16) for i in range(2)]
        kr2 = [sb(f"kr{i}", [128, 4, 256], BF16) for i in range(2)]
        kd2 = [sb(f"kd{i}", [128, 4, 256], BF16) for i in range(2)]
        qT = sb("qT", [128, 4, 2, 128], BF16)
        qsT = sb("qsT", [128, 4, 2, 128], BF16)
        kT = sb("kT", [128, 4, 2, 128], BF16)
        vt = sb("vt", [128, 4, 512], BF16)
        zt = sb("zt", [128, 512], F32)
        zh = sb("zh", [128, 512], F32)
        szt = sb("szt", [128, 2048], BF16)
        attd = sb("attd", [128, 4, 128], BF16)
        o32 = sb("o32", [128, 4, 512], F32)
        junk = sb("junk", [128, 512], BF16)
        st = sb("st", [128, 8], F32)
        mv = sb("mv", [128, 16], F32)
        nbias = sb("nbias", [128, 4], F32)
        ogt = [sb(f"ogt{i}", [128, 2048], BF16) for i in range(2)]

        pj = [ps(f"pj{i}", [128, 512], F32) for i in range(2)]
        tp = ps("tp", [128, 1024], BF16)
        pa = ps("pa", [128, 4, 128], F32)
        po = [ps(f"po{i}", [128, 512], F32) for i in range(2)]
        pu = [ps(f"pu{i}", [128, 512], F32) for i in range(2)]

        b_w = Buf(); b_c = Buf(); b_st32h = [Buf() for _ in range(4)]; b_stbh = [Buf() for _ in range(4)]
        b_hT = [Buf(), Buf()]; b_rope = [Buf(), Buf()]; b_ra = Buf(); b_rb = Buf()
        b_qr2 = [Buf(), Buf()]; b_kr2 = [Buf(), Buf()]; b_kd2 = [Buf(), Buf()]; b_qT = Buf(); b_qsT = Buf(); b_kT = Buf(); b_vt = Buf()
        b_zt = Buf(); b_zh = Buf(); b_szt = Buf(); b_attd = Buf(); b_o32 = Buf(); b_junk = Buf()
        b_st = Buf(); b_mv = Buf(); b_nb = Buf(); b_ogt = [Buf(), Buf()]
        b_pj = [Buf(), Buf()]; b_tp = Buf(); b_pa = Buf(); b_po = [Buf(), Buf()]; b_pu = [Buf(), Buf()]

        S.dma("sync", [(ident[:], I["ident"])], writes=[b_c])
        S.dma("sync", [(decT[:], I["decT"])], writes=[b_c])
        S.dma("sync", [(qdT[:], I["qdT"])], writes=[b_c])
        S.dma("sync", [(kdec[:], I["kdec"])], writes=[b_c])
        S.memset("vector", mhalf[:], -0.5, writes=[b_c])
        S.memset("vector", st32[:], 0.0, writes=b_st32h)
        S.memset("vector", stb[:], 0.0, writes=b_stbh)
        b_wk = [Buf() for _ in range(8)]
        wt = []
        for kc in range(8):
            wt.append(S.dma("gpsimd", [(wr[:, kc, c0:c0 + 1536], I["ret_w_in"][kc * 128:(kc + 1) * 128, c0:c0 + 1536])
                                       for c0 in range(0, RET_COLS, 1536)], writes=[b_wk[kc]], extra=wt[-2:-1] if len(wt) >= 2 else ()))

        nseg = 0
        npo = 0

        def proj(k, c0):
            nonlocal nseg
            pb = nseg % 2; nseg += 1
            for kc in range(8):
                S.mm(pj[pb][:], hT[k][:, kc, :], wr[:, kc, c0:c0 + 512], kc == 0, kc == 7,
                     reads=[b_hT[k], b_wk[kc]], writes=[b_pj[pb]])
            return pj[pb], b_pj[pb]

        def stageA(ti):
            k = ti % 2
            r0 = ti * 128
            qr, kr, kd = qr2[k], kr2[k], kd2[k]
            b_qr, b_kr, b_kd = b_qr2[k], b_kr2[k], b_kd2[k]
            S.dma("sync", [(hT[k][:], SC["h2T"][:, :, r0:r0 + 128].rearrange("k p t -> p k t"))], writes=[b_hT[k]])
            S.dma("sync", [(rope[k][:], I["rope"][r0:r0 + 128])], writes=[b_rope[k]])
            for which in range(2):
                dst, b_dst = (qr, b_qr) if which == 0 else (kr, b_kr)
                cs, sn = (rope[k][:, 0, :], rope[k][:, 1, :]) if which == 0 else (rope[k][:, 2, :], rope[k][:, 3, :])
                csb = mkap(cs, [[0, 2], [1, 128]]); snb = mkap(sn, [[0, 2], [1, 128]])
                for half in range(2):
                    P, bP = proj(k, which * 1024 + half * 512)
                    x1 = mkap(P[:, 0:1], [[256, 2], [1, 128]])
                    x2 = mkap(P[:, 128:129], [[256, 2], [1, 128]])
                    o1 = dst[:, half * 2:half * 2 + 2, 0:128]
                    o2 = dst[:, half * 2:half * 2 + 2, 128:256]
                    S.tt("vector", ra[:], x1, csb, ALU.mult, reads=[bP, b_rope[k]], writes=[b_ra])
                    S.tt("vector", rb[:], x2, snb, ALU.mult, reads=[bP, b_rope[k]], writes=[b_rb])
                    S.tt("vector", o1, ra[:], rb[:], ALU.subtract, reads=[b_ra, b_rb], writes=[b_dst])
                    S.tt("vector", ra[:], x1, snb, ALU.mult, reads=[bP, b_rope[k]], writes=[b_ra])
                    S.tt("vector", rb[:], x2, csb, ALU.mult, reads=[bP, b_rope[k]], writes=[b_rb])
                    S.tt("vector", o2, ra[:], rb[:], ALU.add, reads=[b_ra, b_rb], writes=[b_dst])
            for h in range(4):
                S.act(kd[:, h, :], kr[:, h, :], AF.Copy, reads=[b_kr, b_c], writes=[b_kd], scale=kdec[:, h:h + 1])

        def stageB(ti):
            nonlocal npo
            k = ti % 2
            r0 = ti * 128
            qr, kr, kd = qr2[k], kr2[k], kd2[k]
            b_qr, b_kr, b_kd = b_qr2[k], b_kr2[k], b_kd2[k]
            for h in range(4):
                P, bP = proj(k, 2048 + h * 512)
                S.copy("scalar", vt[:, h, :], P[:], reads=[bP], writes=[b_vt])
            for zc in range(4):
                P, bP = proj(k, 4096 + zc * 512)
                S.act(zt[:], P[:], AF.Tanh, reads=[bP], writes=[b_zt], scale=0.5)
                S.act(zh[:], P[:], AF.Copy, reads=[bP], writes=[b_zh], scale=0.5)
                S.stt(szt[:, zc * 512:(zc + 1) * 512], zt[:], 1.0, zh[:], ALU.add, ALU.mult,
                      reads=[b_zt, b_zh], writes=[b_szt])
            for which in range(2):
                src, b_src = (qr, b_qr) if which == 0 else (kr, b_kr)
                for h in range(4):
                    for dc in range(2):
                        S.tr(tp[:, (h * 2 + dc) * 128:(h * 2 + dc + 1) * 128], src[:, h, dc * 128:(dc + 1) * 128], ident[:],
                             reads=[b_src, b_c], writes=[b_tp])
                if which == 0:
                    S.copy("scalar", qT[:].rearrange("p h c t -> p (h c t)"), tp[:], reads=[b_tp], writes=[b_qT])
                    S.tt("vector", qsT[:], tp[:].rearrange("p (h c t) -> p h c t", h=4, c=2),
                         mkap(qdT[:, 0, 0:1], [[128, 4], [0, 2], [1, 128]]),
                         ALU.mult, reads=[b_tp, b_c], writes=[b_qsT])
                else:
                    S.copy("scalar", kT[:].rearrange("p h c t -> p (h c t)"), tp[:], reads=[b_tp], writes=[b_kT])
            for h in range(4):
                for dc in range(2):
                    S.mm(pa[:, h, :], kT[:, h, dc, :], qT[:, h, dc, :], h == 0 and dc == 0, dc == 1,
                         reads=[b_kT, b_qT], writes=[b_pa], skip_group_check=True)
            S.tt("vector", attd[:], pa[:], decT[:], ALU.mult, reads=[b_pa, b_c], writes=[b_attd])
            for h in range(4):
                ob = npo % 2; npo += 1
                S.mm(po[ob][:], attd[:, h, :], vt[:, h, :], True, False, reads=[b_attd, b_vt], writes=[b_po[ob]])
                for dc in range(2):
                    S.mm(po[ob][:], qsT[:, h, dc, :], stb[:, h, dc, :], False, dc == 1,
                         reads=[b_qsT, b_stbh[h]], writes=[b_po[ob]])
                for dc in range(2):
                    S.mm(pu[dc][:], kd[:, h, dc * 128:(dc + 1) * 128], vt[:, h, :], True, True,
                         reads=[b_kd, b_vt], writes=[b_pu[dc]])
                    S.stt(st32[:, h, dc, :], st32[:, h, dc, :], float(chunk_decay[h]), pu[dc][:], ALU.mult, ALU.add,
                          reads=[b_pu[dc]], writes=[b_st32h[h]])
                S.copy("scalar", stb[:, h, :, :].rearrange("p c e -> p (c e)"), st32[:, h, :, :].rearrange("p c e -> p (c e)"), reads=[b_st32h[h]], writes=[b_stbh[h]])
                S.act(o32[:, h, :], po[ob][:], AF.Copy, reads=[b_po[ob]], writes=[b_o32, b_st], accum_out=st[:, 2 * h:2 * h + 1])
                S.act(junk[:], po[ob][:], AF.Square, reads=[b_po[ob]], writes=[b_junk, b_st], accum_out=st[:, 2 * h + 1:2 * h + 2])
            sv = st[:].rearrange("p (h two) -> p h two", two=2)
            S.ts("vector", mv[:, 0:4], sv[:, :, 0], 1.0 / 512, None, ALU.mult, reads=[b_st], writes=[b_mv])
            S.ts("vector", mv[:, 4:8], sv[:, :, 1], 1.0 / 512, None, ALU.mult, reads=[b_st], writes=[b_mv])
            S.tt("vector", mv[:, 8:12], mv[:, 0:4], mv[:, 0:4], ALU.mult, reads=[b_mv], writes=[b_mv])
            S.tt("vector", mv[:, 8:12], mv[:, 4:8], mv[:, 8:12], ALU.subtract, reads=[b_mv], writes=[b_mv])
            S.ts("vector", mv[:, 8:12], mv[:, 8:12], GN_EPS, None, ALU.add, reads=[b_mv], writes=[b_mv])
            S.tt("gpsimd", mv[:, 12:16], mv[:, 8:12], mhalf[:], ALU.pow, reads=[b_mv, b_c], writes=[b_mv])
            S.stt(nbias[:], mv[:, 0:4], -1.0, mv[:, 12:16], ALU.mult, ALU.mult, reads=[b_mv], writes=[b_nb])
            for h in range(4):
                S.act(o32[:, h, :], o32[:, h, :], AF.Identity, reads=[b_mv, b_nb], writes=[b_o32],
                      scale=mv[:, 12 + h:13 + h], bias=nbias[:, h:h + 1])
                S.tt("vector", ogt[k][:, h * 512:(h + 1) * 512], o32[:, h, :], szt[:, h * 512:(h + 1) * 512],
                     ALU.mult, reads=[b_o32, b_szt], writes=[b_ogt[k]])
            S.dma("gpsimd", [(SC["og"][r0:r0 + 128, :], ogt[k][:])], reads=[b_ogt[k]])

        stageA(0)
        for ti in range(ntiles):
            if ti + 1 < ntiles:
                stageA(ti + 1)
            stageB(ti)
        S.finish(nc)


from concourse.bass_utils import run_bass_kernel_spmd

W_NAMES = ["norm_g", "nsa_w_in", "nsa_q_g", "nsa_kc_g", "nsa_ks_g", "nsa_kw_g", "nsa_cmp_pos_k", "nsa_cmp_pos_v",
           "nsa_cmp_k_w1", "nsa_cmp_k_w2", "nsa_cmp_v_w1", "nsa_cmp_v_w2", "nsa_w_out", "ret_w_in", "ret_w_out"]
_CACHE = {}


def build(debug=False, upto=6):
    nc = bass.Bass("TRN2", target_bir_lowering=False)
    I = {}

    def din(name, shape, dt=F32):
        I[name] = nc.dram_tensor(name, list(shape), dt, kind="ExternalInput").ap()

    din("x", [4096, 1024]); din("p0", [4096, 256]); din("p1", [4096, 256]); din("norm_g", [2, 1024])
    din("nsa_w_in", [1024, 3632])
    for nm in ["nsa_q_g", "nsa_ks_g", "nsa_kw_g", "nsa_kc_g"]:
        din(nm, [1, 64])
    din("nsa_cmp_k_w1", [2048, 256]); din("nsa_cmp_v_w1", [2048, 256]); din("nsa_cmp_k_w2", [256, 64]); din("nsa_cmp_v_w2", [256, 64])
    din("nsa_cmp_pos_k", [32, 64]); din("nsa_cmp_pos_v", [32, 64])
    din("nsa_w_out", [1024, 1024]); din("ret_w_in", [1024, 6144]); din("ret_w_out", [2048, 1024])
    din("ple_w0", [256, 1024]); din("ple_w1", [256, 1024]); din("ple_gate_w0", [1024, 1024]); din("ple_gate_w1", [1024, 1024])
    din("ident", [128, 128], BF16); din("overlap", [256, 64]); din("Eexp", [64, 4096], BF16); din("Ftab", [128, 32, 64])
    din("cmpb", [32, 2, 128, 128], BF16); din("wbias", [2, 128, 128], BF16)
    din("decT", [128, 4, 128]); din("qdT", [128, 4, 128]); din("kdec", [128, 4]); din("rope", [4096, 4, 128])
    out = nc.dram_tensor("out", [4096, 1024], F32, kind="ExternalOutput").ap()
    SC = declare_scratch(nc, debug); SC.update(declare_scratch2(nc, debug)); SC.update(declare_scratch3(nc, debug))
    SC.update(declare_scratch4(nc, debug)); SC.update(declare_scratch5(nc, debug))
    C = make_consts()
    with ExitStack() as es:
        G = Glob(nc, es)
        phase1(nc, G, I, SC)
        if upto >= 2:
            phase2(nc, G, I, SC)
        if upto >= 3:
            phase3(nc, G, I, SC)
        g1 = bass.AP(I["norm_g"].tensor, 1024, [[0, 128], [1, 1024]])
        if upto >= 4:
          epilogue(nc, G, "e0_", SC["outg"], 1024, I["nsa_w_out"], I["x"], I["p0"], I["ple_gate_w0"], I["ple_w0"],
                 SC["x2"], I["ident"], norm_g_row=g1, hT_out=SC["h2T"])
        if upto >= 5:
            phase5(nc, G, I, SC, C["chunk_decay"])
        if upto >= 6:
          epilogue(nc, G, "e1_", SC["og"], 2048, I["ret_w_out"], SC["x2"], I["p1"], I["ple_gate_w1"], I["ple_w1"],
                 out, I["ident"])
    return nc


def make_in_maps(inputs, cores):
    C = make_consts()
    shared = {}
    f32 = lambda a: np.ascontiguousarray(np.asarray(a, dtype=np.float32))
    shared["norm_g"] = f32(inputs["norm_g"])
    for nm in ["nsa_w_in", "nsa_q_g", "nsa_kc_g", "nsa_ks_g", "nsa_kw_g", "nsa_cmp_pos_k", "nsa_cmp_pos_v",
               "nsa_cmp_k_w1", "nsa_cmp_k_w2", "nsa_cmp_v_w1", "nsa_cmp_v_w2", "nsa_w_out", "ret_w_in", "ret_w_out"]:
        a = f32(inputs[nm])[0]
        if a.ndim == 1:
            a = a[None, :]
        shared[nm] = np.ascontiguousarray(a)
    for l in range(2):
        shared[f"ple_w{l}"] = f32(inputs["ple_w"])[l]
        shared[f"ple_gate_w{l}"] = f32(inputs["ple_gate_w"])[l]
    for k in ["ident", "overlap", "Eexp", "Ftab", "cmpb", "wbias", "decT", "qdT", "kdec", "rope"]:
        shared[k] = C[k]
    x = f32(inputs["x"]); p = f32(inputs["p"])
    maps = []
    for b in cores:
        m = dict(shared)
        m["x"] = x[b]; m["p0"] = p[0, b]; m["p1"] = p[1, b]
        maps.append(m)
    return maps


def kernel(**inputs):
    if "nc" not in _CACHE:
        _CACHE["nc"] = build()
    nc = _CACHE["nc"]
    maps = make_in_maps(inputs, list(range(8)))
    res = run_bass_kernel_spmd(nc, maps, core_ids=list(range(8)))
    return np.stack([np.asarray(r["out"], dtype=np.float32) for r in res.results], axis=0)
```

```python
import numpy as np
import ml_dtypes
import concourse.bass as bass
import concourse.mybir as mybir
from contextlib import ExitStack

F32 = mybir.dt.float32
BF16 = mybir.dt.bfloat16
AF = mybir.ActivationFunctionType
ALU = mybir.AluOpType
AX = mybir.AxisListType
ENGS = ("sync", "scalar", "vector", "gpsimd", "tensor")
SKIP_SAME = {"tensor"}


class Buf:
    __slots__ = ("w", "r", "name", "dsem")

    def __init__(self, name=""):
        self.w = None
        self.r = {}
        self.name = name
        self.dsem = None


class Glob:
    def __init__(self, nc, es, n_dma_sems=96):
        self.nc = nc
        self.esem = {e: es.enter_context(nc.semaphore("es_" + e)) for e in ENGS}
        self.cnt = {e: 0 for e in ENGS}
        self.dsems = [es.enter_context(nc.semaphore(f"ds{i}")) for i in range(n_dma_sems)]
        self.dcnt = [0] * n_dma_sems
        self.next_dsem = 0
        self.next_dsem_sw = 0
        self.n_hw = 64
        self.seen = {e: {} for e in ENGS}

    def sem_of(self, key):
        if isinstance(key, str):
            return self.esem[key]
        return self.dsems[key]

    def alloc_dsem(self, q="sync"):
        if q == "gpsimd":
            i = self.n_hw + self.next_dsem_sw
            assert i < len(self.dsems), "out of sw dma semaphores"
            self.next_dsem_sw += 1
            return i
        i = self.next_dsem
        assert i < self.n_hw, "out of hw dma semaphores"
        self.next_dsem += 1
        return i


class Sched:
    def __init__(self, G):
        self.G = G
        G.next_dsem = 0
        G.next_dsem_sw = 0
        self.q = {e: [] for e in ENGS}
        self.pending_dma = []

    def _wait(self, eng, tok):
        if tok is None:
            return
        key, val = tok
        if eng == key and eng in SKIP_SAME:
            return
        seen = self.G.seen[eng]
        if seen.get(key, 0) >= val:
            return
        seen[key] = val
        sem = self.G.sem_of(key)
        self.q[eng].append(lambda e, sem=sem, val=val: e.wait_ge(sem, val))

    def _deps(self, eng, reads, writes, extra):
        for b in reads:
            self._wait(eng, b.w)
        for b in writes:
            self._wait(eng, b.w)
            for k, v in b.r.items():
                self._wait(eng, (k, v))
        for t in extra:
            self._wait(eng, t)

    @staticmethod
    def _mark(tok, reads, writes):
        k, v = tok
        for b in reads:
            if b.r.get(k, 0) < v:
                b.r[k] = v
        for b in writes:
            b.w = tok
            b.r = {}

    def op(self, eng, fn, reads=(), writes=(), extra=()):
        G = self.G
        self._deps(eng, reads, writes, extra)
        G.cnt[eng] += 1
        tok = (eng, G.cnt[eng])
        sem = G.esem[eng]
        self.q[eng].append(lambda e, fn=fn, sem=sem: fn(e).then_inc(sem, 1))
        self._mark(tok, reads, writes)
        return tok

    def dma(self, q, pairs, reads=(), writes=(), extra=(), **kw):
        G = self.G
        bufs = list(writes) + list(reads)
        b0 = bufs[0]
        if b0.dsem is None:
            b0.dsem = (q, G.alloc_dsem(q))
        assert b0.dsem[0] == q, "buffer used with both DMA queue types"
        si = b0.dsem[1]
        self._deps(q, reads, writes, extra)
        sem = G.dsems[si]
        for (o, i) in pairs:
            G.dcnt[si] += 16
            self.q[q].append(lambda e, o=o, i=i, sem=sem, kw=kw: e.dma_start(out=o, in_=i, **kw).then_inc(sem, 16))
        tok = (si, G.dcnt[si])
        self._mark(tok, reads, writes)
        self.pending_dma.append(tok)
        return tok

    def finish(self, nc):
        for t in self.pending_dma:
            self._wait("sync", t)
            self._wait("gpsimd", t)
        with nc.Block() as block:
            for e in ENGS:
                lst = self.q[e]

                def body(eng, lst=lst):
                    for f in lst:
                        f(eng)
                getattr(block, e)(body)

    def act(self, out, in_, func, reads=(), writes=(), eng="scalar", **kw):
        return self.op(eng, lambda e: e.activation(out=out, in_=in_, func=func, **kw), reads, writes)

    def mm(self, out, lhsT, rhs, start, stop, reads=(), writes=(), **kw):
        return self.op("tensor", lambda e: e.matmul(out, lhsT, rhs, start=start, stop=stop, **kw), reads, writes)

    def tr(self, out, in_, ident, reads=(), writes=()):
        return self.op("tensor", lambda e: e.transpose(out, in_, ident), reads, writes)

    def tt(self, eng, out, in0, in1, op, reads=(), writes=()):
        return self.op(eng, lambda e: e.tensor_tensor(out=out, in0=in0, in1=in1, op=op), reads, writes)

    def ts(self, eng, out, in0, s1, s2, op0, op1=None, reads=(), writes=(), **kw):
        if op1 is None:
            return self.op(eng, lambda e: e.tensor_scalar(out=out, in0=in0, scalar1=s1, scalar2=None, op0=op0, **kw), reads, writes)
        return self.op(eng, lambda e: e.tensor_scalar(out=out, in0=in0, scalar1=s1, scalar2=s2, op0=op0, op1=op1, **kw), reads, writes)

    def stt(self, out, in0, scalar, in1, op0, op1, reads=(), writes=(), eng="vector"):
        return self.op(eng, lambda e: e.scalar_tensor_tensor(out=out, in0=in0, scalar=scalar, in1=in1, op0=op0, op1=op1), reads, writes)

    def copy(self, eng, out, in_, reads=(), writes=()):
        if eng == "scalar":
            return self.op(eng, lambda e: e.copy(out=out, in_=in_), reads, writes)
        return self.op(eng, lambda e: e.tensor_copy(out=out, in_=in_), reads, writes)

    def memset(self, eng, ap, val, writes=()):
        return self.op(eng, lambda e: e.memset(ap, val), (), writes)

    def reduce(self, out, in_, op, axis, reads=(), writes=(), eng="vector"):
        return self.op(eng, lambda e: e.tensor_reduce(out=out, in_=in_, axis=axis, op=op), reads, writes)


def bc(ap, n):
    return bass.AP(ap.tensor, ap.offset, [list(x) for x in ap.ap] + [[0, n]])


def mkap(ap, dims):
    return bass.AP(ap.tensor, ap.offset, [list(ap.ap[0])] + [list(d) for d in dims])


BIG = 30000.0
def make_consts():
    c = {}
    c["ident"] = np.eye(128, dtype=ml_dtypes.bfloat16)
    i = np.arange(256)[:, None]; j = np.arange(64)[None, :]
    ov = ((i * 16 < (j + 1) * 64) & (i * 16 + 32 > j * 64)).astype(np.float32)
    ov[255] = 0
    c["overlap"] = ov
    E = (np.arange(4096)[None, :] // 64 == np.arange(64)[:, None]).astype(np.float32)
    c["Eexp"] = E.astype(ml_dtypes.bfloat16)
    t = (np.arange(32)[None, :, None] * 128 + np.arange(128)[:, None, None])
    cur = t // 64
    b = np.arange(64)[None, None, :]
    valid = b <= cur
    forced = (b == 0) | (b == cur) | (b == cur - 1)
    F = np.where(valid, np.where(forced, 1e4, 0.0), -1e30).astype(np.float32)
    c["Ftab"] = np.ascontiguousarray(F)
    n = (np.arange(2)[None, :, None, None] * 128 + np.arange(128)[None, None, :, None])
    tt = (np.arange(32)[:, None, None, None] * 128 + np.arange(128)[None, None, None, :])
    cb = np.where(16 * n + 31 > tt, -BIG, 0.0).astype(np.float32)
    c["cmpb"] = cb.astype(ml_dtypes.bfloat16)
    ii = np.arange(128)[:, None]; uu = np.arange(128)[None, :]
    wb = np.stack([np.where(ii > uu, -BIG, 0.0), np.where(ii <= uu, -BIG, 0.0)]).astype(np.float32)
    c["wbias"] = wb.astype(ml_dtypes.bfloat16)
    H, C = 4, 128
    log_g = np.log(1.0 - 2.0 ** (-5.0 - np.arange(H, dtype=np.float64)))
    ix = np.arange(C, dtype=np.float64)
    diff = ix[:, None] - ix[None, :]
    intra = np.where(diff >= 0, np.exp(log_g[:, None, None] * np.maximum(diff, 0.0)), 0.0)
    c["decT"] = np.ascontiguousarray(intra.transpose(2, 0, 1)).astype(np.float32)
    q_decay = np.exp(log_g[:, None] * (ix + 1.0))
    c["qdT"] = np.ascontiguousarray(np.broadcast_to(q_decay[None], (128, H, C))).astype(np.float32)
    k_decay = np.exp(log_g[:, None] * (C - 1.0 - ix))
    c["kdec"] = np.ascontiguousarray(k_decay.T).astype(np.float32)
    c["chunk_decay"] = np.exp(log_g * C)
    half = 128
    inv = (np.float32(10000.0) ** (-np.linspace(0.0, 1.0, half, dtype=np.float32))).astype(np.float32)
    pos = np.arange(4096, dtype=np.float32)
    ang = (pos[:, None] * inv[None, :]).astype(np.float32).astype(np.float64)
    cs, sn = np.cos(ang), np.sin(ang)
    c["rope"] = np.ascontiguousarray(np.stack([cs, sn, cs / 16.0, sn / 16.0], axis=1)).astype(np.float32)
    return c


S_, D_ = 4096, 1024
NT = 32
EPS = 1e-6
NSA_COLS = 3632


def declare_scratch(nc, debug):
    kind = dict(kind="ExternalOutput") if debug else {}
    dbg = debug if isinstance(debug, (set, list, tuple)) else None
    d = {}
    d["qT"] = nc.dram_tensor("qT_s", [16, 64, S_], BF16, **(kind if (dbg is None or "qT" in dbg) else {})).ap()
    d["kvT"] = nc.dram_tensor("kvT_s", [16, 64, S_], BF16, **(kind if (dbg is None or "kvT" in dbg) else {})).ap()
    d["vsA"] = nc.dram_tensor("vsA_s", [NT, 128, 260], BF16, **(kind if (dbg is None or "vsA" in dbg) else {})).ap()
    d["vwA"] = nc.dram_tensor("vwA_s", [NT, 128, 260], BF16, **(kind if (dbg is None or "vwA" in dbg) else {})).ap()
    d["gates"] = nc.dram_tensor("gates_s", [S_, 48], F32, **(kind if (dbg is None or "gates" in dbg) else {})).ap()
    d["sz"] = nc.dram_tensor("sz_s", [S_, 1024], BF16, **(kind if (dbg is None or "sz" in dbg) else {})).ap()
    return d


def phase1(nc, G, I, SC, ntg=8):
    S = Sched(G)
    with ExitStack() as es:
        def sb(name, shape, dt):
            return es.enter_context(nc.sbuf_tensor("p1_" + name, shape, dt))

        def ps(name, shape, dt):
            return es.enter_context(nc.psum_tensor("p1_" + name, shape, dt))

        w_sb = sb("w_sb", [128, 8, NSA_COLS], BF16)
        gb = sb("gb", [128, 1024], F32)
        gcol = sb("gcol", [64, 4], F32)
        ident = sb("ident", [128, 128], BF16)
        mhalf = sb("mhalf", [128, 8], F32)
        xt = [sb(f"xt{i}", [128, 1024], F32) for i in range(2)]
        junk = sb("junk", [128, 1024], BF16)
        ssx = [sb(f"ssx{i}", [128, 2], F32) for i in range(2)]
        hb = [sb(f"hb{i}", [128, 1024], BF16) for i in range(2)]
        hT = [sb(f"hT{i}", [128, 8, 128], BF16) for i in range(2)]
        sq = [sb(f"sq{i}", [128, 512], F32) for i in range(2)]
        qf = [sb(f"qf{i}", [128, 512], F32) for i in range(2)]
        ss = [sb(f"ss{i}", [128, 8], F32) for i in range(2)]
        rs = [sb(f"rs{i}", [128, 8], F32) for i in range(2)]
        qn = [sb(f"qn{i}", [128, 512], BF16) for i in range(4)]
        zt = [sb(f"zt{i}", [128, 512], F32) for i in range(2)]
        zh = [sb(f"zh{i}", [128, 512], F32) for i in range(2)]
        gtmp = sb("gtmp", [128, 48], F32)
        qTt = [sb(f"qTt{i}", [64, 16, 512], BF16) for i in range(2)]
        kvTt = [sb(f"kvTt{i}", [64, 16, 512], BF16) for i in range(2)]
        vsAt = [sb(f"vsAt{i}", [128, 4, 260], BF16) for i in range(2)]
        vwAt = [sb(f"vwAt{i}", [128, 4, 260], BF16) for i in range(2)]
        szt = [sb(f"szt{i}", [128, 4, 1024], BF16) for i in range(2)]
        gt = [sb(f"gt{i}", [128, 4, 48], F32) for i in range(2)]

        pj = [ps(f"pj{i}", [128, 512], F32) for i in range(4)]
        tpa = ps("tpa", [128, 1024], BF16)
        tpq = [ps(f"tpq{i}", [64, 1024], BF16) for i in range(2)]

        B = lambda n: Buf(n)
        b_w = B("w"); b_c = B("consts")
        b_xt = [B("xt") for _ in range(2)]; b_junk = B("junk"); b_ssx = [B("ssx") for _ in range(2)]
        b_hb = [B("hb") for _ in range(2)]; b_hT = [B("hT") for _ in range(2)]
        b_sq = [B("sq") for _ in range(2)]; b_qf = [B("qf") for _ in range(2)]; b_ss = [B("ss") for _ in range(2)]; b_rs = [B("rs") for _ in range(2)]
        b_qn = [B("qn") for _ in range(4)]; b_zt = [B("zt") for _ in range(2)]; b_zh = [B("zh") for _ in range(2)]
        b_gtmp = B("gtmp")
        b_qTt = [B("qTt") for _ in range(2)]; b_kvTt = [B("kvTt") for _ in range(2)]
        b_vsAt = [B("vsAt") for _ in range(2)]; b_vwAt = [B("vwAt") for _ in range(2)]
        b_szt = [B("szt") for _ in range(2)]; b_gt = [B("gt") for _ in range(2)]
        b_pj = [B("pj") for _ in range(4)]; b_tpa = B("tpa"); b_tpq = [B("tpq") for _ in range(2)]

        S.dma("sync", [(ident[:], I["ident"])], writes=[b_c])
        S.dma("sync", [(gb[:], bass.AP(I["norm_g"].tensor, 0, [[0, 128], [1, 1024]]))], writes=[b_c])
        for j, nm in enumerate(["nsa_q_g", "nsa_ks_g", "nsa_kw_g"]):
            S.dma("sync", [(gcol[:, j:j + 1], bass.AP(I[nm].tensor, 0, [[1, 64], [1, 1]]))], writes=[b_c])
        S.ts("vector", gcol[:, 0:1], gcol[:, 0:1], 0.125, None, ALU.mult, reads=[b_c], writes=[b_c])
        S.memset("vector", mhalf[:], -0.5, writes=[b_c])
        for i in range(2):
            S.memset("vector", vsAt[i][:], 1.0, writes=[b_vsAt[i]])
            S.memset("vector", vwAt[i][:], 1.0, writes=[b_vwAt[i]])
        half = NSA_COLS // 2
        b_wk = [Buf() for _ in range(8)]
        wt = []
        for kc in range(8):
            wt.append(S.dma("gpsimd", [(w_sb[:, kc, c0:c0 + half], I["nsa_w_in"][kc * 128:(kc + 1) * 128, c0:c0 + half])
                                       for c0 in (0, half)], writes=[b_wk[kc]], extra=wt[-2:-1] if len(wt) >= 2 else ()))

        nseg = 0
        tq = 0
        nqn = 0
        defer = []

        def flush(keep):
            while len(defer) > keep:
                defer.pop(0)()

        def norm_heads(pj_ap, b_pjn, nh, out_ap3, b_out):
            nonlocal nseg
            k = nseg % 2
            nseg += 1
            n = nh * 64
            S.act(sq[k][:, 0:n], pj_ap, AF.Square, reads=[b_pjn], writes=[b_sq[k]])
            S.act(qf[k][:, 0:n], pj_ap, AF.Copy, reads=[b_pjn], writes=[b_qf[k]])
            S.reduce(ss[k][:, 0:nh], sq[k][:, 0:n].rearrange("p (h d) -> p h d", d=64), ALU.add, AX.X,
                     reads=[b_sq[k]], writes=[b_ss[k]])
            S.ts("vector", ss[k][:, 0:nh], ss[k][:, 0:nh], 1.0 / 64, EPS, ALU.mult, ALU.add,
                 reads=[b_ss[k]], writes=[b_ss[k]])
            S.tt("gpsimd", rs[k][:, 0:nh], ss[k][:, 0:nh], mhalf[:, 0:nh], ALU.pow,
                 reads=[b_ss[k], b_c], writes=[b_rs[k]])
            S.tt("vector", out_ap3, qf[k][:, 0:n].rearrange("p (h d) -> p h d", d=64), bc(rs[k][:, 0:nh], 64), ALU.mult,
                 reads=[b_qf[k], b_rs[k]], writes=[b_out])

        def front(ti, part):
            xp = ti % 2
            if part == 1:
                for kc in range(8):
                    S.tr(tpa[:, kc * 128:(kc + 1) * 128], hb[xp][:, kc * 128:(kc + 1) * 128], ident[:],
                         reads=[b_hb[xp], b_c], writes=[b_tpa])
                S.copy("scalar", hT[xp][:].rearrange("p a b -> p (a b)"), tpa[:], reads=[b_tpa], writes=[b_hT[xp]])
                return
            S.dma("sync", [(xt[xp][:], I["x"][ti * 128:(ti + 1) * 128, :])], writes=[b_xt[xp]])
            S.act(junk[:], xt[xp][:], AF.Square, reads=[b_xt[xp]], writes=[b_junk, b_ssx[xp]],
                  accum_out=ssx[xp][:, 0:1])
            S.ts("vector", ssx[xp][:, 0:1], ssx[xp][:, 0:1], 1.0 / 1024, EPS, ALU.mult, ALU.add,
                 reads=[b_ssx[xp]], writes=[b_ssx[xp]])
            S.tt("gpsimd", ssx[xp][:, 1:2], ssx[xp][:, 0:1], mhalf[:, 0:1], ALU.pow,
                 reads=[b_ssx[xp], b_c], writes=[b_ssx[xp]])
            S.stt(hb[xp][:], xt[xp][:], ssx[xp][:, 1:2], gb[:], ALU.mult, ALU.mult,
                  reads=[b_xt[xp], b_ssx[xp], b_c], writes=[b_hb[xp]])


        for tg in range(ntg):
            sp = tg % 2
            for tl in range(4):
                ti = tg * 4 + tl
                xp = ti % 2
                if ti == 0:
                    front(0, 0); front(0, 1)
                if ti + 1 < ntg * 4:
                    front(ti + 1, 0)
                segs = [(0, 512, "q0"), (512, 512, "q1"), (1024, 512, "kcvc"), (1536, 512, "ksvs"),
                        (2048, 512, "kwvw"), (2608, 512, "z0"), (3120, 512, "z1"), (2560, 48, "gl")]
                for si, (c0, n, kind) in enumerate(segs):
                    pb = si % 4
                    if si == 5 and ti + 1 < ntg * 4:
                        front(ti + 1, 1)
                    flush(2)
                    for kc in range(8):
                        S.mm(pj[pb][:, 0:n], hT[xp][:, kc, :], w_sb[:, kc, c0:c0 + n], kc == 0, kc == 7,
                             reads=[b_hT[xp], b_wk[kc]], writes=[b_pj[pb]])
                    if kind in ("q0", "q1"):
                        qk = nqn % 4; nqn += 1
                        norm_heads(pj[pb][:, 0:512], b_pj[pb], 8, qn[qk][:].rearrange("p (h d) -> p h d", d=64), b_qn[qk])
                        h0 = 0 if kind == "q0" else 8

                        def C(qk=qk, h0=h0, sp=sp, tl=tl):
                            nonlocal tq
                            tk = tq % 2; tq += 1
                            for h in range(8):
                                S.tr(tpq[tk][:, h * 128:(h + 1) * 128], qn[qk][:, h * 64:(h + 1) * 64], ident[:],
                                     reads=[b_qn[qk], b_c], writes=[b_tpq[tk]])
                            S.act(qTt[sp][:, h0:h0 + 8, tl * 128:(tl + 1) * 128],
                                  tpq[tk][:].rearrange("p (h t) -> p h t", t=128), AF.Copy,
                                  reads=[b_tpq[tk], b_c], writes=[b_qTt[sp]], scale=gcol[:, 0:1])
                        defer.append(C)
                    elif kind == "kcvc":
                        qk = nqn % 4; nqn += 1
                        S.copy("scalar", qn[qk][:], pj[pb][:, 0:512], reads=[b_pj[pb]], writes=[b_qn[qk]])

                        def C(qk=qk, sp=sp, tl=tl):
                            nonlocal tq
                            tk = tq % 2; tq += 1
                            for h in range(8):
                                S.tr(tpq[tk][:, h * 128:(h + 1) * 128], qn[qk][:, h * 64:(h + 1) * 64], ident[:],
                                     reads=[b_qn[qk], b_c], writes=[b_tpq[tk]])
                            S.copy("vector", kvTt[sp][:, 0:8, tl * 128:(tl + 1) * 128],
                                   tpq[tk][:].rearrange("p (h t) -> p h t", t=128),
                                   reads=[b_tpq[tk]], writes=[b_kvTt[sp]])
                        defer.append(C)
                    elif kind in ("ksvs", "kwvw"):
                        qk = nqn % 4; nqn += 1
                        norm_heads(pj[pb][:, 0:256], b_pj[pb], 4,
                                   qn[qk][:, 0:256].rearrange("p (h d) -> p h d", d=64), b_qn[qk])
                        r0, gc = (8, 1) if kind == "ksvs" else (12, 2)

                        def C(qk=qk, sp=sp, tl=tl, r0=r0, gc=gc):
                            nonlocal tq
                            tk = tq % 2; tq += 1
                            for h in range(4):
                                S.tr(tpq[tk][:, h * 128:(h + 1) * 128], qn[qk][:, h * 64:(h + 1) * 64], ident[:],
                                     reads=[b_qn[qk], b_c], writes=[b_tpq[tk]])
                            S.act(kvTt[sp][:, r0:r0 + 4, tl * 128:(tl + 1) * 128],
                                  tpq[tk][:, 0:512].rearrange("p (h t) -> p h t", t=128), AF.Copy,
                                  reads=[b_tpq[tk], b_c], writes=[b_kvTt[sp]], scale=gcol[:, gc:gc + 1])
                        defer.append(C)
                        vt, bvt = (vsAt, b_vsAt) if kind == "ksvs" else (vwAt, b_vwAt)
                        S.copy("vector", vt[sp][:, tl, :].rearrange("p (g c) -> p g c", c=65)[:, :, 0:64],
                               pj[pb][:, 256:512].rearrange("p (g c) -> p g c", c=64),
                               reads=[b_pj[pb]], writes=[bvt[sp]])
                    elif kind in ("z0", "z1"):
                        zk = nseg % 2; nseg += 1
                        S.act(zt[zk][:], pj[pb][:, 0:512], AF.Tanh, reads=[b_pj[pb]], writes=[b_zt[zk]], scale=0.5)
                        S.act(zh[zk][:], pj[pb][:, 0:512], AF.Copy, reads=[b_pj[pb]], writes=[b_zh[zk]], scale=0.5)
                        z0 = 0 if kind == "z0" else 512
                        S.stt(szt[sp][:, tl, z0:z0 + 512], zt[zk][:], 1.0, zh[zk][:], ALU.add, ALU.mult,
                              reads=[b_zt[zk], b_zh[zk]], writes=[b_szt[sp]])
                    else:
                        S.act(gtmp[:], pj[pb][:, 0:48], AF.Tanh, reads=[b_pj[pb]], writes=[b_gtmp], scale=0.5)
                        S.ts("vector", gt[sp][:, tl, :], gtmp[:], 0.5, 0.5, ALU.mult, ALU.add,
                             reads=[b_gtmp], writes=[b_gt[sp]])
            flush(0)
            t0 = tg * 512
            S.dma("sync", [(SC["qT"].rearrange("h d t -> d h t")[:, :, t0:t0 + 512], qTt[sp][:])], reads=[b_qTt[sp]])
            S.dma("sync", [(SC["kvT"].rearrange("h d t -> d h t")[:, :, t0:t0 + 512], kvTt[sp][:])], reads=[b_kvTt[sp]])
            S.dma("sync", [(SC["vsA"][tg * 4:(tg + 1) * 4].rearrange("t p c -> p t c"), vsAt[sp][:])], reads=[b_vsAt[sp]])
            S.dma("sync", [(SC["vwA"][tg * 4:(tg + 1) * 4].rearrange("t p c -> p t c"), vwAt[sp][:])], reads=[b_vwAt[sp]])
            S.dma("sync", [(SC["sz"][t0:t0 + 512, :].rearrange("(t p) c -> p t c", p=128), szt[sp][:])], reads=[b_szt[sp]])
            S.dma("sync", [(SC["gates"][t0:t0 + 512, :].rearrange("(t p) c -> p t c", p=128), gt[sp][:])], reads=[b_gt[sp]])
        S.finish(nc)


EPS = 1e-6


def declare_scratch2(nc, debug):
    kind = dict(kind="ExternalOutput") if debug else {}
    dbg = debug if isinstance(debug, (set, list, tuple)) else None
    d = {}
    d["kccT"] = nc.dram_tensor("kccT_s", [64, 4, 256], BF16, **(kind if (dbg is None or "kccT" in dbg) else {})).ap()
    d["vcA"] = nc.dram_tensor("vcA_s", [128, 2, 4, 128], BF16, **(kind if (dbg is None or "vcA" in dbg) else {})).ap()
    return d


def phase2(nc, G, I, SC):
    S = Sched(G)
    with ExitStack() as es:
        def sb(name, shape, dt):
            return es.enter_context(nc.sbuf_tensor("p2_" + name, shape, dt))

        def ps(name, shape, dt):
            return es.enter_context(nc.psum_tensor("p2_" + name, shape, dt))

        kvT = sb("kvT", [64, 8, 4096], BF16)
        W1 = [sb(f"W1{i}", [64, 32, 256], BF16) for i in range(2)]
        W2 = [sb(f"W2{i}", [128, 2, 64], BF16) for i in range(2)]
        posf = sb("posf", [64, 2, 32], F32)
        posb = sb("posb", [64, 2, 32], BF16)
        c1h = sb("c1h", [128, 4], F32)
        ident = sb("ident", [128, 128], BF16)
        ovf = sb("ovf", [128, 2, 64], F32)
        gk = sb("gk", [64, 1], F32)
        mhalf = sb("mhalf", [128, 8], F32)
        th = [sb(f"th{i}", [128, 256], F32) for i in range(2)]
        uu = [sb(f"uu{i}", [128, 256], F32) for i in range(2)]
        hid = [[sb(f"hid{a}{b}", [128, 256], BF16) for b in range(2)] for a in range(2)]
        sq = sb("sq", [128, 256], F32)
        ss = sb("ss", [128, 4], F32)
        rs = sb("rs", [128, 4], F32)
        kn = sb("kn", [128, 256], BF16)
        kccT = sb("kccT", [64, 4, 256], BF16)
        vcA = sb("vcA", [128, 2, 4, 128], BF16)

        ph = [ps(f"ph{i}", [128, 512], F32) for i in range(2)]
        pc1 = ps("pc1", [128, 512], F32)
        po = ps("po", [128, 512], F32)
        tp = ps("tp", [64, 1024], BF16)

        b_kvT = Buf(); b_W1 = [Buf(), Buf()]; b_W2 = [Buf(), Buf()]; b_c = Buf(); b_pos = Buf(); b_c1h = Buf()
        b_th = [Buf(), Buf()]; b_uu = [Buf(), Buf()]; b_hid = [[Buf(), Buf()], [Buf(), Buf()]]
        b_sq = Buf(); b_ss = Buf(); b_rs = Buf(); b_kn = Buf(); b_kccT = Buf(); b_vcA = Buf()
        b_ph = [Buf(), Buf()]; b_pc1 = Buf(); b_po = Buf(); b_tp = Buf()

        S.dma("sync", [(kvT[:, 0:4, :], SC["kvT"][0:4].rearrange("h d t -> d h t")),
                       (kvT[:, 4:8, :], SC["kvT"][4:8].rearrange("h d t -> d h t"))], writes=[b_kvT])
        for kv, nm in enumerate(["nsa_cmp_k_w1", "nsa_cmp_v_w1"]):
            src = I[nm].rearrange("(l d) m -> d l m", d=64)
            S.dma("gpsimd", [(W1[kv][:, l0:l0 + 8, :], src[:, l0:l0 + 8, :]) for l0 in range(0, 32, 8)], writes=[b_W1[kv]])
        for kv, nm in enumerate(["nsa_cmp_k_w2", "nsa_cmp_v_w2"]):
            S.dma("gpsimd", [(W2[kv][:], I[nm].rearrange("(c p) m -> p c m", p=128))], writes=[b_W2[kv]])
        for kv, nm in enumerate(["nsa_cmp_pos_k", "nsa_cmp_pos_v"]):
            S.dma("sync", [(posf[:, kv, :], bass.AP(I[nm].tensor, 0, [[1, 64], [64, 32]]))], writes=[b_pos],
                  allow_slow_non_contiguous=True)
        S.copy("vector", posb[:], posf[:], reads=[b_pos], writes=[b_pos])
        S.dma("sync", [(ident[:], I["ident"])], writes=[b_c])
        S.dma("sync", [(ovf[:], I["overlap"].rearrange("(c p) j -> p c j", p=128))], writes=[b_c])
        S.dma("sync", [(gk[:], bass.AP(I["nsa_kc_g"].tensor, 0, [[1, 64], [1, 1]]))], writes=[b_c])
        S.memset("vector", mhalf[:], -0.5, writes=[b_c])
        for a in range(2):
            for b in range(2):
                S.memset("vector", hid[a][b][:], 0.0, writes=[b_hid[a][b]])
        S.memset("vector", vcA[:], 1.0, writes=[b_vcA])
        S.memset("vector", kccT[:], 0.0, writes=[b_kccT])

        for kv in range(2):
            for hh in range(2):
                col = kv * 2 + hh
                for l in range(32):
                    S.mm(pc1[:, col:col + 1], W1[kv][:, l, hh * 128:(hh + 1) * 128], posb[:, kv, l:l + 1],
                         l == 0, l == 31, reads=[b_W1[kv], b_pos], writes=[b_pc1])
                S.act(c1h[:, col:col + 1], pc1[:, col:col + 1], AF.Copy, reads=[b_pc1], writes=[b_c1h], scale=0.5)

        it = 0
        for kv in range(2):
            for g in range(4):
                par = it % 2
                for hh in range(2):
                    pb = hh
                    col = kv * 2 + hh
                    for l in range(32):
                        rhs = mkap(kvT[:, kv * 4 + g, l:l + 1], [[16, 255]])
                        S.mm(ph[pb][:, 0:255], W1[kv][:, l, hh * 128:(hh + 1) * 128], rhs, l == 0, l == 31,
                             reads=[b_W1[kv], b_kvT], writes=[b_ph[pb]])
                    S.act(th[hh][:, 0:255], ph[pb][:, 0:255], AF.Tanh, reads=[b_ph[pb], b_c1h], writes=[b_th[hh]],
                          scale=0.5, bias=c1h[:, col:col + 1])
                    S.act(uu[hh][:, 0:255], ph[pb][:, 0:255], AF.Identity, reads=[b_ph[pb], b_c1h], writes=[b_uu[hh]],
                          scale=0.5, bias=c1h[:, col:col + 1])
                    S.stt(hid[par][hh][:, 0:255], th[hh][:, 0:255], 1.0, uu[hh][:, 0:255], ALU.add, ALU.mult,
                          reads=[b_th[hh], b_uu[hh]], writes=[b_hid[par][hh]])
                for c in range(2):
                    o_ap = po[:, (c * 4 + g) * 64:(c * 4 + g + 1) * 64]
                    for hh in range(2):
                        S.mm(o_ap, hid[par][hh][:, c * 128:(c + 1) * 128], W2[kv][:, hh, :], hh == 0, hh == 1,
                             reads=[b_hid[par][hh], b_W2[kv]], writes=[b_po], skip_group_check=True)
                it += 1
            for c in range(2):
                src = po[:, c * 256:(c + 1) * 256]
                if kv == 0:
                    S.act(sq[:], src, AF.Square, reads=[b_po], writes=[b_sq])
                    S.reduce(ss[:], sq[:].rearrange("p (h d) -> p h d", d=64), ALU.add, AX.X, reads=[b_sq], writes=[b_ss])
                    S.ts("vector", ss[:], ss[:], 1.0 / 64, EPS, ALU.mult, ALU.add, reads=[b_ss], writes=[b_ss])
                    S.tt("gpsimd", rs[:], ss[:], mhalf[:, 0:4], ALU.pow, reads=[b_ss, b_c], writes=[b_rs])
                    S.tt("vector", kn[:].rearrange("p (h d) -> p h d", d=64), src.rearrange("p (h d) -> p h d", d=64),
                         bc(rs[:], 64), ALU.mult, reads=[b_po, b_rs], writes=[b_kn])
                    for g in range(4):
                        S.tr(tp[:, g * 128:(g + 1) * 128], kn[:, g * 64:(g + 1) * 64], ident[:],
                             reads=[b_kn, b_c], writes=[b_tp])
                    S.act(kccT[:, :, c * 128:(c + 1) * 128], tp[:, 0:512].rearrange("p (g t) -> p g t", t=128), AF.Copy,
                          reads=[b_tp, b_c], writes=[b_kccT], scale=gk[:, 0:1])
                else:
                    S.copy("vector", vcA[:, c, :, 0:64], src.rearrange("p (g d) -> p g d", d=64), reads=[b_po], writes=[b_vcA])
                    S.copy("vector", vcA[:, c, :, 65:128], mkap(ovf[:, c, 0:1], [[0, 4], [1, 63]]), reads=[b_c], writes=[b_vcA])
        S.dma("sync", [(SC["kccT"], kccT[:])], reads=[b_kccT])
        S.dma("sync", [(SC["vcA"], vcA[:])], reads=[b_vcA])
        S.finish(nc)


NEG_BIG = -30000.0


def declare_scratch3(nc, debug):
    kind = dict(kind="ExternalOutput") if debug else {}
    dbg = debug if isinstance(debug, (set, list, tuple)) else None
    d = {}
    d["outg"] = nc.dram_tensor("outg_s", [4096, 1024], BF16, **(kind if (dbg is None or "outg" in dbg) else {})).ap()
    return d


def phase3(nc, G, I, SC, jlist=None, stage=9):
    S = Sched(G)
    jlist = list(range(32)) if jlist is None else jlist
    with ExitStack() as es:
        def sb(name, shape, dt):
            return es.enter_context(nc.sbuf_tensor("p3_" + name, shape, dt))

        def ps(name, shape, dt):
            return es.enter_context(nc.psum_tensor("p3_" + name, shape, dt))

        KE = sb("KE", [128, 4, 4096], BF16)
        kwT = sb("kwT", [64, 4, 4096], BF16)
        vsA = sb("vsA", [128, 32, 260], BF16)
        vwA = sb("vwA", [128, 32, 260], BF16)
        kccT = sb("kccT", [64, 4, 256], BF16)
        vcA = sb("vcA", [128, 2, 4, 128], BF16)
        Ftab = sb("Ftab", [128, 32, 64], F32)
        ident = sb("ident", [128, 128], BF16)
        wbias = sb("wbias", [128, 2, 128], BF16)
        cmpb = [sb(f"cmpb{i}", [128, 2, 128], BF16) for i in range(2)]
        QS = [sb(f"QS{i}", [128, 512], BF16) for i in range(2)]
        gt = [sb(f"gt{i}", [128, 48], F32) for i in range(2)]
        szt = [sb(f"szt{i}", [128, 1024], BF16) for i in range(2)]
        Pt = [sb(f"Pt{i}", [128, 2, 512], BF16) for i in range(2)]
        acc2 = [sb(f"acc{i}", [128, 16, 64], F32) for i in range(2)]
        tmp = sb("tmp", [128, 4, 64], F32)
        og = [sb(f"og{i}", [128, 1024], BF16) for i in range(2)]
        zc = sb("zc", [128, 4], F32)
        rz = sb("rz", [128, 4], F32)
        coef = sb("coef", [128, 4], F32)
        score = sb("score", [128, 64], F32)
        work = sb("work", [128, 64], F32)
        m8 = sb("m8", [128, 16], F32)
        selb = sb("selb", [128, 128], BF16)

        pS = [ps(f"pS{i}", [128, 2, 512], F32) for i in range(2)]
        Oc = ps("Oc", [128, 4, 128], F32)
        Ow = ps("Ow", [128, 4, 128], F32)
        Os = ps("Os", [128, 4, 128], F32)
        tps = ps("tps", [128, 1024], BF16)

        b_c = Buf(); bK = {n: Buf() for n in ["KE", "kwT", "vsA", "vwA", "kccT", "vcA"]}
        b_cmpb = [Buf(), Buf()]; b_QSq = [Buf(), Buf()]; b_QSs = [Buf(), Buf()]
        b_gt = [Buf(), Buf()]; b_szt = [Buf(), Buf()]
        b_Pt = [Buf() for _ in range(2)]; b_acc2 = [Buf(), Buf()]; b_tmp = Buf(); b_og = [Buf(), Buf()]
        b_zc = Buf(); b_rz = Buf(); b_coef = Buf(); b_score = Buf(); b_work = Buf(); b_m8 = Buf(); b_selb = Buf()
        b_pS = [Buf() for _ in range(2)]; b_Oc = Buf(); b_Ow = Buf(); b_Os = Buf(); b_tps = Buf()

        S.dma("sync", [(ident[:], I["ident"])], writes=[b_c])
        S.dma("sync", [(Ftab[:], I["Ftab"])], writes=[b_c])
        S.dma("sync", [(wbias[:], I["wbias"].rearrange("w i u -> i w u"))], writes=[b_c])
        S.dma("sync", [(kccT[:], SC["kccT"])], writes=[bK["kccT"]])
        S.dma("sync", [(vcA[:], SC["vcA"])], writes=[bK["vcA"]])
        S.dma("sync", [(KE[0:64, :, :], SC["kvT"][8:12].rearrange("h d t -> d h t"))] + [(KE[64:128, g, :], I["Eexp"]) for g in range(4)], writes=[bK["KE"]])
        S.dma("sync", [(kwT[:], SC["kvT"][12:16].rearrange("h d t -> d h t"))], writes=[bK["kwT"]])
        S.dma("sync", [(vsA[:, a:a + 4, :], SC["vsA"][a:a + 4].rearrange("t p c -> p t c")) for a in range(0, 32, 4)], writes=[bK["vsA"]])
        S.dma("sync", [(vwA[:, a:a + 4, :], SC["vwA"][a:a + 4].rearrange("t p c -> p t c")) for a in range(0, 32, 4)], writes=[bK["vwA"]])
        S.memset("vector", selb[:], 0.0, writes=[b_selb])

        cnt = {"u": 0, "it": 0}

        units = []
        iters = []

        def add_units(tile_list, pre_first=None, pre_last=None, post_last=None):
            us = []
            for a in range(0, len(tile_list), 2):
                us.append(dict(tiles=tile_list[a:a + 2], pre=[], post=[]))
            if pre_first is not None:
                us[0]["pre"].append(pre_first)
            if pre_last is not None:
                us[-1]["pre"].append(pre_last)
            if post_last is not None:
                us[-1]["post"].append(post_last)
            return us

        def emit_S(u):
            for f in u["pre"]:
                f()
            ui = cnt["u"] % 2; cnt["u"] += 1
            u["ui"] = ui
            for ti, t in enumerate(u["tiles"]):
                mms = t["mms"]
                for k, (l, r) in enumerate(mms):
                    S.mm(pS[ui][:, ti, :], l, r, k == 0, k == len(mms) - 1, reads=t["reads"], writes=[b_pS[ui]])

        def emit_EP(u):
            ui = u["ui"]
            n = len(u["tiles"])
            S.act(Pt[ui][:, 0:n, :], pS[ui][:, 0:n, :], AF.Exp, reads=[b_pS[ui]], writes=[b_Pt[ui]])
            for ti, t in enumerate(u["tiles"]):
                for h in range(4):
                    S.mm(t["O"](h), Pt[ui][:, ti, h * 128:(h + 1) * 128], t["V"], t["first"] and h == 0, t["last"],
                         reads=[b_Pt[ui], t["bV"]], writes=[t["bO"]], skip_group_check=True)
            for f in u["post"]:
                f()

        for jn, j in enumerate(jlist):
            jp = jn % 2
            t0 = j * 128
            cs = []
            if j <= 16:
                cs.append((0, True))
            else:
                cs.append((0, False))
            if j >= 16:
                cs.append((1, True))
            acc = acc2[jp]; b_acc = b_acc2[jp]
            for g in range(4):
                it = cnt["it"]; cnt["it"] += 1
                qp = it % 2
                qtop = QS[qp][0:64, :]

                def pre_iter(jn=jn, j=j, jp=jp, t0=t0, g=g, qp=qp, cs=cs):
                    if g == 0:
                        S.dma("sync", [(gt[jp][:], SC["gates"][t0:t0 + 128, :])], writes=[b_gt[jp]])
                        S.dma("sync", [(szt[jp][:], SC["sz"][t0:t0 + 128, :])], writes=[b_szt[jp]])
                        S.dma("sync", [(cmpb[jp][:, c, :], I["cmpb"][j, c]) for (c, hb) in cs if hb], writes=[b_cmpb[jp]])
                    S.dma("sync", [(QS[qp][0:64, :].rearrange("p (h t) -> p h t", t=128),
                                    SC["qT"][4 * g:4 * g + 4, :, t0:t0 + 128].rearrange("h d t -> d h t"))],
                          writes=[b_QSq[qp]])

                ctiles = []
                for ci, (c, hasb) in enumerate(cs):
                    mms = [(kccT[:, g, c * 128:(c + 1) * 128], qtop)]
                    rd = [bK["kccT"], b_QSq[qp]]
                    if hasb:
                        mms.append((ident[:], mkap(cmpb[jp][:, c, 0:1], [[0, 4], [1, 128]])))
                        rd = rd + [b_c, b_cmpb[jp]]
                    ctiles.append(dict(mms=mms, reads=rd, O=(lambda h: Oc[:, h, :]), V=vcA[:, c, g, :], bV=bK["vcA"],
                                       bO=b_Oc, first=(ci == 0), last=(ci == len(cs) - 1)))

                def post_cmp(j=j, jp=jp, g=g, qp=qp, acc=acc, b_acc=b_acc):
                    S.ts("vector", zc[:], Oc[:, :, 64], 1e-30, None, ALU.max, reads=[b_Oc], writes=[b_zc])
                    S.op("vector", lambda e: e.reciprocal(out=rz[:], in_=zc[:]), reads=[b_zc], writes=[b_rz])
                    S.copy("vector", score[:], Ftab[:, j, :], reads=[b_c], writes=[b_score])
                    for h in range(4):
                        S.stt(score[:, 0:63], Oc[:, h, 65:128], rz[:, h:h + 1], score[:, 0:63], ALU.mult, ALU.add,
                              reads=[b_Oc, b_rz, b_score], writes=[b_score])
                    S.op("vector", lambda e: e.max(out=m8[:, 0:8], in_=score[:]), reads=[b_score], writes=[b_m8])
                    S.op("vector", lambda e: e.match_replace(out=work[:], in_to_replace=m8[:, 0:8], in_values=score[:],
                                                             imm_value=-3.0e38),
                         reads=[b_score, b_m8], writes=[b_work])
                    S.op("vector", lambda e: e.max(out=m8[:, 8:16], in_=work[:]), reads=[b_work], writes=[b_m8])
                    S.ts("vector", selb[:, 64:128], score[:], m8[:, 15:16], NEG_BIG, ALU.is_lt, ALU.mult,
                         reads=[b_score, b_m8], writes=[b_selb])
                    S.tt("vector", coef[:], rz[:], mkap(gt[jp][:, 12 * g:12 * g + 1], [[3, 4]]), ALU.mult,
                         reads=[b_rz, b_gt[jp]], writes=[b_coef])
                    S.tt("vector", acc[:, 4 * g:4 * g + 4, :], Oc[:, :, 0:64], bc(coef[:], 64), ALU.mult,
                         reads=[b_Oc, b_coef], writes=[b_acc])

                cu = add_units(ctiles, pre_first=pre_iter, post_last=post_cmp)

                kts = [kt for kt in range(j - 4, j + 1) if kt >= 0]
                wtiles = []
                for ki, kt in enumerate(kts):
                    mms = [(kwT[:, g, kt * 128:(kt + 1) * 128], qtop)]
                    rd = [bK["kwT"], b_QSq[qp]]
                    if kt == j:
                        mms.append((ident[:], mkap(wbias[:, 0, 0:1], [[0, 4], [1, 128]])))
                        rd = rd + [b_c]
                    elif kt == j - 4:
                        mms.append((ident[:], mkap(wbias[:, 1, 0:1], [[0, 4], [1, 128]])))
                        rd = rd + [b_c]
                    wtiles.append(dict(mms=mms, reads=rd, O=(lambda h: Ow[:, h, 0:65]), V=vwA[:, kt, g * 65:(g + 1) * 65],
                                       bV=bK["vwA"], bO=b_Ow, first=(ki == 0), last=(ki == len(kts) - 1)))

                def pre_selT(qp=qp):
                    S.tr(tps[:, 0:128], selb[:], ident[:], reads=[b_selb, b_c], writes=[b_tps])
                    S.copy("vector", QS[qp][64:128, :].rearrange("p (h t) -> p h t", t=128),
                           mkap(tps[64:128, 0:1], [[0, 4], [1, 128]]), reads=[b_tps], writes=[b_QSs[qp]])

                def post_win(jp=jp, g=g, acc=acc, b_acc=b_acc):
                    S.op("vector", lambda e: e.reciprocal(out=rz[:], in_=Ow[:, :, 64]), reads=[b_Ow], writes=[b_rz])
                    S.tt("vector", coef[:], rz[:], mkap(gt[jp][:, 12 * g + 2:12 * g + 3], [[3, 4]]), ALU.mult,
                         reads=[b_rz, b_gt[jp]], writes=[b_coef])
                    S.tt("vector", tmp[:], Ow[:, :, 0:64], bc(coef[:], 64), ALU.mult,
                         reads=[b_Ow, b_coef], writes=[b_tmp])
                    S.tt("gpsimd", acc[:, 4 * g:4 * g + 4, :], acc[:, 4 * g:4 * g + 4, :], tmp[:], ALU.add,
                         reads=[b_tmp], writes=[b_acc])

                wu = add_units(wtiles, post_last=post_win)

                stiles = []
                for kt in range(j + 1):
                    mms = [(KE[:, g, kt * 128:(kt + 1) * 128], QS[qp][:, :])]
                    rd = [bK["KE"], b_QSq[qp], b_QSs[qp]]
                    if kt == j:
                        mms.append((ident[:], mkap(wbias[:, 0, 0:1], [[0, 4], [1, 128]])))
                        rd = rd + [b_c]
                    stiles.append(dict(mms=mms, reads=rd, O=(lambda h: Os[:, h, 0:65]), V=vsA[:, kt, g * 65:(g + 1) * 65],
                                       bV=bK["vsA"], bO=b_Os, first=(kt == 0), last=(kt == j)))

                def post_sel(jp=jp, g=g, t0=t0, acc=acc, b_acc=b_acc):
                    S.op("vector", lambda e: e.reciprocal(out=rz[:], in_=Os[:, :, 64]), reads=[b_Os], writes=[b_rz])
                    S.tt("vector", coef[:], rz[:], mkap(gt[jp][:, 12 * g + 1:12 * g + 2], [[3, 4]]), ALU.mult,
                         reads=[b_rz, b_gt[jp]], writes=[b_coef])
                    S.tt("vector", tmp[:], Os[:, :, 0:64], bc(coef[:], 64), ALU.mult,
                         reads=[b_Os, b_coef], writes=[b_tmp])
                    S.tt("gpsimd", acc[:, 4 * g:4 * g + 4, :], acc[:, 4 * g:4 * g + 4, :], tmp[:], ALU.add,
                         reads=[b_tmp], writes=[b_acc])
                    if g == 3:
                        S.tt("gpsimd", og[jp][:], acc[:].rearrange("p h d -> p (h d)"), szt[jp][:], ALU.mult,
                             reads=[b_acc, b_szt[jp]], writes=[b_og[jp]])
                        S.dma("gpsimd", [(SC["outg"][t0:t0 + 128, :], og[jp][:])], reads=[b_og[jp]])

                su = add_units(stiles, pre_first=pre_selT, post_last=post_sel)
                iters.append((cu, wu, su))

        for ii, (cu, wu, su) in enumerate(iters):
            units.extend(cu)
            if ii > 0:
                units.extend(iters[ii - 1][2])
            units.extend(wu)
        if iters:
            units.extend(iters[-1][2])
        if units:
            emit_S(units[0])
            for i in range(len(units)):
                if i + 1 < len(units):
                    emit_S(units[i + 1])
                emit_EP(units[i])
        S.finish(nc)


EPS = 1e-6


def declare_scratch4(nc, debug):
    kind = dict(kind="ExternalOutput") if debug else {}
    dbg = debug if isinstance(debug, (set, list, tuple)) else None
    d = {}
    d["x2"] = nc.dram_tensor("x2_s", [4096, 1024], F32, **(kind if (dbg is None or "x2" in dbg) else {})).ap()
    d["h2T"] = nc.dram_tensor("h2T_s", [8, 128, 4096], BF16, **(kind if (dbg is None or "h2T" in dbg) else {})).ap()
    return d


def epilogue(nc, G, pfx, src, KW, w_out, x_in, p_in, gw_in, pw_in, x_out, ident_in, norm_g_row=None, hT_out=None, ntiles=32):
    S = Sched(G)
    KC = KW // 128
    with ExitStack() as es:
        def sb(name, shape, dt):
            return es.enter_context(nc.sbuf_tensor(pfx + name, shape, dt))

        def ps(name, shape, dt):
            return es.enter_context(nc.psum_tensor(pfx + name, shape, dt))

        wo = sb("wo", [128, KC, 1024], BF16)
        gw = sb("gw", [128, 8, 1024], BF16)
        pw = sb("pw", [128, 2, 1024], BF16)
        ident = sb("ident", [128, 128], BF16)
        gb = sb("gb", [128, 1024], F32)
        mhalf = sb("mhalf", [128, 2], F32)
        ogt = [sb(f"ogt{i}", [128, KW], BF16) for i in range(2)]
        ogT = [sb(f"ogT{i}", [128, KC, 128], BF16) for i in range(2)]
        xt = [sb(f"xt{i}", [128, 1024], F32) for i in range(2)]
        pt = [sb(f"pt{i}", [128, 256], F32) for i in range(2)]
        ptb = sb("ptb", [128, 256], BF16)
        pT = [sb(f"pT{i}", [128, 2, 128], BF16) for i in range(2)]
        x1 = [sb(f"x1{i}", [128, 1024], F32) for i in range(2)]
        x1b = [sb(f"x1b{i}", [128, 1024], BF16) for i in range(2)]
        x1T = [sb(f"x1T{i}", [128, 8, 128], BF16) for i in range(2)]
        th = sb("th", [128, 1024], F32)
        t2 = sb("t2", [128, 1024], F32)
        x2 = [sb(f"x2{i}", [128, 1024], F32) for i in range(2)]
        junk = sb("junk", [128, 1024], BF16)
        ssx = sb("ssx", [128, 2], F32)
        hb2 = [sb(f"hb{i}", [128, 1024], BF16) for i in range(2)]
        hTt = [sb(f"hTt{i}", [128, 8, 512], BF16) for i in range(2)]

        tp = [ps(f"tp{i}", [128, 1024], BF16) for i in range(2)]
        py = [ps(f"py{i}", [128, 512], F32) for i in range(2)]
        pg = [ps(f"pg{i}", [128, 512], F32) for i in range(2)]
        pp = [ps(f"pp{i}", [128, 512], F32) for i in range(2)]

        b_w = Buf(); b_gw = Buf(); b_pw = Buf(); b_c = Buf()
        b_ogt = [Buf(), Buf()]; b_ogT = [Buf(), Buf()]; b_xt = [Buf(), Buf()]; b_pt = [Buf(), Buf()]; b_ptb = Buf()
        b_pT = [Buf(), Buf()]; b_x1 = [Buf(), Buf()]; b_x1b = [Buf(), Buf()]; b_x1T = [Buf(), Buf()]; b_th = Buf(); b_t2 = Buf()
        b_x2 = [Buf(), Buf()]; b_junk = Buf(); b_ssx = Buf(); b_hb2 = [Buf(), Buf()]; b_hTt = [Buf(), Buf()]
        b_tp = [Buf(), Buf()]; b_py = [Buf(), Buf()]; b_pg = [Buf(), Buf()]; b_pp = [Buf(), Buf()]

        S.dma("sync", [(ident[:], ident_in)], writes=[b_c])
        if norm_g_row is not None:
            S.dma("sync", [(gb[:], norm_g_row)], writes=[b_c])
        S.memset("vector", mhalf[:], -0.5, writes=[b_c])
        wt = []
        b_wk = [Buf() for _ in range(KC)]
        b_gwk = [Buf() for _ in range(8)]
        for kc in range(0, KC, 2):
            wt.append(S.dma("gpsimd", [(wo[:, kc + a, :], w_out[(kc + a) * 128:(kc + a + 1) * 128, :]) for a in range(2)],
                            writes=[b_wk[kc], b_wk[kc + 1]], extra=wt[-2:-1] if len(wt) >= 2 else ()))
        for kc in range(0, 8, 2):
            wt.append(S.dma("gpsimd", [(gw[:, kc + a, :], gw_in[(kc + a) * 128:(kc + a + 1) * 128, :]) for a in range(2)],
                            writes=[b_gwk[kc], b_gwk[kc + 1]], extra=wt[-2:-1]))
        wt.append(S.dma("gpsimd", [(pw[:, kc, :], pw_in[kc * 128:(kc + 1) * 128, :]) for kc in range(2)], writes=[b_pw], extra=wt[-2:-1]))

        ntp = 0

        def stageA(ti):
            nonlocal ntp
            k = ti % 2
            r0 = ti * 128
            S.dma("sync", [(ogt[k][:], src[r0:r0 + 128, :])], writes=[b_ogt[k]])
            S.dma("sync", [(xt[k][:], x_in[r0:r0 + 128, :])], writes=[b_xt[k]])
            S.dma("sync", [(pt[k][:], p_in[r0:r0 + 128, :])], writes=[b_pt[k]])
            for c0 in range(0, KC, 8):
                q = ntp % 2; ntp += 1
                for kc in range(8):
                    S.tr(tp[q][:, kc * 128:(kc + 1) * 128], ogt[k][:, (c0 + kc) * 128:(c0 + kc + 1) * 128], ident[:],
                         reads=[b_ogt[k], b_c], writes=[b_tp[q]])
                S.copy("vector" if c0 == 0 else "scalar", ogT[k][:, c0:c0 + 8, :].rearrange("p a b -> p (a b)"), tp[q][:],
                       reads=[b_tp[q]], writes=[b_ogT[k]])
            S.copy("gpsimd", ptb[:], pt[k][:], reads=[b_pt[k]], writes=[b_ptb])
            q = ntp % 2; ntp += 1
            for c in range(2):
                S.tr(tp[q][:, c * 128:(c + 1) * 128], ptb[:, c * 128:(c + 1) * 128], ident[:],
                     reads=[b_ptb, b_c], writes=[b_tp[q]])
            S.copy("vector", pT[k][:].rearrange("p a b -> p (a b)"), tp[q][:, 0:256], reads=[b_tp[q]], writes=[b_pT[k]])
            for nb in range(2):
                for kc in range(KC):
                    S.mm(py[nb][:], ogT[k][:, kc, :], wo[:, kc, nb * 512:(nb + 1) * 512], kc == 0, kc == KC - 1,
                         reads=[b_ogT[k], b_wk[kc]], writes=[b_py[nb]])
                S.tt("vector", x1[k][:, nb * 512:(nb + 1) * 512], py[nb][:], xt[k][:, nb * 512:(nb + 1) * 512], ALU.add,
                     reads=[b_py[nb], b_xt[k]], writes=[b_x1[k]])
            S.copy("scalar", x1b[k][:], x1[k][:], reads=[b_x1[k]], writes=[b_x1b[k]])

        def stageB(ti):
            nonlocal ntp
            k = ti % 2
            r0 = ti * 128
            q = ntp % 2; ntp += 1
            for kc in range(8):
                S.tr(tp[q][:, kc * 128:(kc + 1) * 128], x1b[k][:, kc * 128:(kc + 1) * 128], ident[:],
                     reads=[b_x1b[k], b_c], writes=[b_tp[q]])
            S.copy("scalar", x1T[k][:].rearrange("p a b -> p (a b)"), tp[q][:], reads=[b_tp[q]], writes=[b_x1T[k]])
            for nb in range(2):
                for kc in range(8):
                    S.mm(pg[nb][:], x1T[k][:, kc, :], gw[:, kc, nb * 512:(nb + 1) * 512], kc == 0, kc == 7,
                         reads=[b_x1T[k], b_gwk[kc]], writes=[b_pg[nb]])
                for c in range(2):
                    S.mm(pp[nb][:], pT[k][:, c, :], pw[:, c, nb * 512:(nb + 1) * 512], c == 0, c == 1,
                         reads=[b_pT[k], b_pw], writes=[b_pp[nb]])
                sl = slice(nb * 512, (nb + 1) * 512)
                S.act(th[:, sl], pg[nb][:], AF.Tanh, reads=[b_pg[nb]], writes=[b_th], scale=0.5)
                S.stt(t2[:, sl], th[:, sl], 1.0, pp[nb][:], ALU.add, ALU.mult, reads=[b_th, b_pp[nb]], writes=[b_t2])
            S.stt(x2[k][:], t2[:], 0.5, x1[k][:], ALU.mult, ALU.add, reads=[b_t2, b_x1[k]], writes=[b_x2[k]])
            S.dma("gpsimd", [(x_out[r0:r0 + 128, :], x2[k][:])], reads=[b_x2[k]])
            if hT_out is not None:
                tg, tl = divmod(ti, 4)
                sp = tg % 2
                S.act(junk[:], x2[k][:], AF.Square, reads=[b_x2[k]], writes=[b_junk, b_ssx], accum_out=ssx[:, 0:1])
                S.ts("vector", ssx[:, 0:1], ssx[:, 0:1], 1.0 / 1024, EPS, ALU.mult, ALU.add, reads=[b_ssx], writes=[b_ssx])
                S.tt("gpsimd", ssx[:, 1:2], ssx[:, 0:1], mhalf[:, 0:1], ALU.pow, reads=[b_ssx, b_c], writes=[b_ssx])
                S.stt(hb2[k][:], x2[k][:], ssx[:, 1:2], gb[:], ALU.mult, ALU.mult, reads=[b_x2[k], b_ssx, b_c], writes=[b_hb2[k]])

        def stageC(ti):
            nonlocal ntp
            if hT_out is None:
                return
            k = ti % 2
            tg, tl = divmod(ti, 4)
            sp = tg % 2
            q = ntp % 2; ntp += 1
            for kc in range(8):
                S.tr(tp[q][:, kc * 128:(kc + 1) * 128], hb2[k][:, kc * 128:(kc + 1) * 128], ident[:],
                     reads=[b_hb2[k], b_c], writes=[b_tp[q]])
            S.copy("scalar", hTt[sp][:, :, tl * 128:(tl + 1) * 128], tp[q][:].rearrange("p (a b) -> p a b", b=128),
                   reads=[b_tp[q]], writes=[b_hTt[sp]])
            if tl == 3:
                S.dma("gpsimd", [(hT_out.rearrange("k p t -> p k t")[:, :, tg * 512:(tg + 1) * 512], hTt[sp][:])],
                      reads=[b_hTt[sp]])

        stageA(0)
        for ti in range(ntiles):
            if ti + 1 < ntiles:
                stageA(ti + 1)
            stageB(ti)
            if ti >= 1:
                stageC(ti - 1)
        stageC(ntiles - 1)
        S.finish(nc)


GN_EPS = 1e-5
RET_COLS = 6144


def declare_scratch5(nc, debug):
    kind = dict(kind="ExternalOutput") if debug else {}
    dbg = debug if isinstance(debug, (set, list, tuple)) else None
    d = {}
    d["og"] = nc.dram_tensor("og_s", [4096, 2048], BF16, **(kind if (dbg is None or "og" in dbg) else {})).ap()
    return d


def phase5(nc, G, I, SC, chunk_decay, ntiles=32):
    S = Sched(G)
    with ExitStack() as es:
        def sb(name, shape, dt):
            return es.enter_context(nc.sbuf_tensor("p5_" + name, shape, dt))

        def ps(name, shape, dt):
            return es.enter_context(nc.psum_tensor("p5_" + name, shape, dt))

        wr = sb("wr", [128, 8, RET_COLS], BF16)
        ident = sb("ident", [128, 128], BF16)
        decT = sb("decT", [128, 4, 128], F32)
        qdT = sb("qdT", [128, 4, 128], F32)
        kdec = sb("kdec", [128, 4], F32)
        mhalf = sb("mhalf", [128, 4], F32)
        st32 = sb("st32", [128, 4, 2, 512], F32)
        stb = sb("stb", [128, 4, 2, 512], BF16)
        hT = [sb(f"hT{i}", [128, 8, 128], BF16) for i in range(2)]
        rope = [sb(f"rope{i}", [128, 4, 128], F32) for i in range(2)]
        ra = sb("ra", [128, 2, 128], F32)
        rb = sb("rb", [128, 2, 128], F32)
        qr2 = [sb(f"qr{i}", [128, 4, 256], BF16) for i in range(2)]
        kr2 = [sb(f"kr{i}", [128, 4, 256], BF16) for i in range(2)]
        kd2 = [sb(f"kd{i}", [128, 4, 256], BF16) for i in range(2)]
        qT = sb("qT", [128, 4, 2, 128], BF16)
        qsT = sb("qsT", [128, 4, 2, 128], BF16)
        kT = sb("kT", [128, 4, 2, 128], BF16)
        vt = sb("vt", [128, 4, 512], BF16)
        zt = sb("zt", [128, 512], F32)
        zh = sb("zh", [128, 512], F32)
        szt = sb("szt", [128, 2048], BF16)
        attd = sb("attd", [128, 4, 128], BF16)
        o32 = sb("o32", [128, 4, 512], F32)
        junk = sb("junk", [128, 512], BF16)
        st = sb("st", [128, 8], F32)
        mv = sb("mv", [128, 16], F32)
        nbias = sb("nbias", [128, 4], F32)
        ogt = [sb(f"ogt{i}", [128, 2048], BF16) for i in range(2)]

        pj = [ps(f"pj{i}", [128, 512], F32) for i in range(2)]
        tp = ps("tp", [128, 1024], BF16)
        pa = ps("pa", [128, 4, 128], F32)
        po = [ps(f"po{i}", [128, 512], F32) for i in range(2)]
        pu = [ps(f"pu{i}", [128, 512], F32) for i in range(2)]

        b_w = Buf(); b_c = Buf(); b_st32h = [Buf() for _ in range(4)]; b_stbh = [Buf() for _ in range(4)]
        b_hT = [Buf(), Buf()]; b_rope = [Buf(), Buf()]; b_ra = Buf(); b_rb = Buf()
        b_qr2 = [Buf(), Buf()]; b_kr2 = [Buf(), Buf()]; b_kd2 = [Buf(), Buf()]; b_qT = Buf(); b_qsT = Buf(); b_kT = Buf(); b_vt = Buf()
        b_zt = Buf(); b_zh = Buf(); b_szt = Buf(); b_attd = Buf(); b_o32 = Buf(); b_junk = Buf()
        b_st = Buf(); b_mv = Buf(); b_nb = Buf(); b_ogt = [Buf(), Buf()]
        b_pj = [Buf(), Buf()]; b_tp = Buf(); b_pa = Buf(); b_po = [Buf(), Buf()]; b_pu = [Buf(), Buf()]

        S.dma("sync", [(ident[:], I["ident"])], writes=[b_c])
        S.dma("sync", [(decT[:], I["decT"])], writes=[b_c])
        S.dma("sync", [(qdT[:], I["qdT"])], writes=[b_c])
        S.dma("sync", [(kdec[:], I["kdec"])], writes=[b_c])
        S.memset("vector", mhalf[:], -0.5, writes=[b_c])
        S.memset("vector", st32[:], 0.0, writes=b_st32h)
        S.memset("vector", stb[:], 0.0, writes=b_stbh)
        b_wk = [Buf() for _ in range(8)]
        wt = []
        for kc in range(8):
            wt.append(S.dma("gpsimd", [(wr[:, kc, c0:c0 + 1536], I["ret_w_in"][kc * 128:(kc + 1) * 128, c0:c0 + 1536])
                                       for c0 in range(0, RET_COLS, 1536)], writes=[b_wk[kc]], extra=wt[-2:-1] if len(wt) >= 2 else ()))

        nseg = 0
        npo = 0

        def proj(k, c0):
            nonlocal nseg
            pb = nseg % 2; nseg += 1
            for kc in range(8):
                S.mm(pj[pb][:], hT[k][:, kc, :], wr[:, kc, c0:c0 + 512], kc == 0, kc == 7,
                     reads=[b_hT[k], b_wk[kc]], writes=[b_pj[pb]])
            return pj[pb], b_pj[pb]

        def stageA(ti):
            k = ti % 2
            r0 = ti * 128
            qr, kr, kd = qr2[k], kr2[k], kd2[k]
            b_qr, b_kr, b_kd = b_qr2[k], b_kr2[k], b_kd2[k]
            S.dma("sync", [(hT[k][:], SC["h2T"][:, :, r0:r0 + 128].rearrange("k p t -> p k t"))], writes=[b_hT[k]])
            S.dma("sync", [(rope[k][:], I["rope"][r0:r0 + 128])], writes=[b_rope[k]])
            for which in range(2):
                dst, b_dst = (qr, b_qr) if which == 0 else (kr, b_kr)
                cs, sn = (rope[k][:, 0, :], rope[k][:, 1, :]) if which == 0 else (rope[k][:, 2, :], rope[k][:, 3, :])
                csb = mkap(cs, [[0, 2], [1, 128]]); snb = mkap(sn, [[0, 2], [1, 128]])
                for half in range(2):
                    P, bP = proj(k, which * 1024 + half * 512)
                    x1 = mkap(P[:, 0:1], [[256, 2], [1, 128]])
                    x2 = mkap(P[:, 128:129], [[256, 2], [1, 128]])
                    o1 = dst[:, half * 2:half * 2 + 2, 0:128]
                    o2 = dst[:, half * 2:half * 2 + 2, 128:256]
                    S.tt("vector", ra[:], x1, csb, ALU.mult, reads=[bP, b_rope[k]], writes=[b_ra])
                    S.tt("vector", rb[:], x2, snb, ALU.mult, reads=[bP, b_rope[k]], writes=[b_rb])
                    S.tt("vector", o1, ra[:], rb[:], ALU.subtract, reads=[b_ra, b_rb], writes=[b_dst])
                    S.tt("vector", ra[:], x1, snb, ALU.mult, reads=[bP, b_rope[k]], writes=[b_ra])
                    S.tt("vector", rb[:], x2, csb, ALU.mult, reads=[bP, b_rope[k]], writes=[b_rb])
                    S.tt("vector", o2, ra[:], rb[:], ALU.add, reads=[b_ra, b_rb], writes=[b_dst])
            for h in range(4):
                S.act(kd[:, h, :], kr[:, h, :], AF.Copy, reads=[b_kr, b_c], writes=[b_kd], scale=kdec[:, h:h + 1])

        def stageB(ti):
            nonlocal npo
            k = ti % 2
            r0 = ti * 128
            qr, kr, kd = qr2[k], kr2[k], kd2[k]
            b_qr, b_kr, b_kd = b_qr2[k], b_kr2[k], b_kd2[k]
            for h in range(4):
                P, bP = proj(k, 2048 + h * 512)
                S.copy("scalar", vt[:, h, :], P[:], reads=[bP], writes=[b_vt])
            for zc in range(4):
                P, bP = proj(k, 4096 + zc * 512)
                S.act(zt[:], P[:], AF.Tanh, reads=[bP], writes=[b_zt], scale=0.5)
                S.act(zh[:], P[:], AF.Copy, reads=[bP], writes=[b_zh], scale=0.5)
                S.stt(szt[:, zc * 512:(zc + 1) * 512], zt[:], 1.0, zh[:], ALU.add, ALU.mult,
                      reads=[b_zt, b_zh], writes=[b_szt])
            for which in range(2):
                src, b_src = (qr, b_qr) if which == 0 else (kr, b_kr)
                for h in range(4):
                    for dc in range(2):
                        S.tr(tp[:, (h * 2 + dc) * 128:(h * 2 + dc + 1) * 128], src[:, h, dc * 128:(dc + 1) * 128], ident[:],
                             reads=[b_src, b_c], writes=[b_tp])
                if which == 0:
                    S.copy("scalar", qT[:].rearrange("p h c t -> p (h c t)"), tp[:], reads=[b_tp], writes=[b_qT])
                    S.tt("vector", qsT[:], tp[:].rearrange("p (h c t) -> p h c t", h=4, c=2),
                         mkap(qdT[:, 0, 0:1], [[128, 4], [0, 2], [1, 128]]),
                         ALU.mult, reads=[b_tp, b_c], writes=[b_qsT])
                else:
                    S.copy("scalar", kT[:].rearrange("p h c t -> p (h c t)"), tp[:], reads=[b_tp], writes=[b_kT])
            for h in range(4):
                for dc in range(2):
                    S.mm(pa[:, h, :], kT[:, h, dc, :], qT[:, h, dc, :], h == 0 and dc == 0, dc == 1,
                         reads=[b_kT, b_qT], writes=[b_pa], skip_group_check=True)
            S.tt("vector", attd[:], pa[:], decT[:], ALU.mult, reads=[b_pa, b_c], writes=[b_attd])
            for h in range(4):
                ob = npo % 2; npo += 1
                S.mm(po[ob][:], attd[:, h, :], vt[:, h, :], True, False, reads=[b_attd, b_vt], writes=[b_po[ob]])
                for dc in range(2):
                    S.mm(po[ob][:], qsT[:, h, dc, :], stb[:, h, dc, :], False, dc == 1,
                         reads=[b_qsT, b_stbh[h]], writes=[b_po[ob]])
                for dc in range(2):
                    S.mm(pu[dc][:], kd[:, h, dc * 128:(dc + 1) * 128], vt[:, h, :], True, True,
                         reads=[b_kd, b_vt], writes=[b_pu[dc]])
                    S.stt(st32[:, h, dc, :], st32[:, h, dc, :], float(chunk_decay[h]), pu[dc][:], ALU.mult, ALU.add,
                          reads=[b_pu[dc]], writes=[b_st32h[h]])
                S.copy("scalar", stb[:, h, :, :].rearrange("p c e -> p (c e)"), st32[:, h, :, :].rearrange("p c e -> p (c e)"), reads=[b_st32h[h]], writes=[b_stbh[h]])
                S.act(o32[:, h, :], po[ob][:], AF.Copy, reads=[b_po[ob]], writes=[b_o32, b_st], accum_out=st[:, 2 * h:2 * h + 1])
                S.act(junk[:], po[ob][:], AF.Square, reads=[b_po[ob]], writes=[b_junk, b_st], accum_out=st[:, 2 * h + 1:2 * h + 2])
            sv = st[:].rearrange("p (h two) -> p h two", two=2)
            S.ts("vector", mv[:, 0:4], sv[:, :, 0], 1.0 / 512, None, ALU.mult, reads=[b_st], writes=[b_mv])
            S.ts("vector", mv[:, 4:8], sv[:, :, 1], 1.0 / 512, None, ALU.mult, reads=[b_st], writes=[b_mv])
            S.tt("vector", mv[:, 8:12], mv[:, 0:4], mv[:, 0:4], ALU.mult, reads=[b_mv], writes=[b_mv])
            S.tt("vector", mv[:, 8:12], mv[:, 4:8], mv[:, 8:12], ALU.subtract, reads=[b_mv], writes=[b_mv])
            S.ts("vector", mv[:, 8:12], mv[:, 8:12], GN_EPS, None, ALU.add, reads=[b_mv], writes=[b_mv])
            S.tt("gpsimd", mv[:, 12:16], mv[:, 8:12], mhalf[:], ALU.pow, reads=[b_mv, b_c], writes=[b_mv])
            S.stt(nbias[:], mv[:, 0:4], -1.0, mv[:, 12:16], ALU.mult, ALU.mult, reads=[b_mv], writes=[b_nb])
            for h in range(4):
                S.act(o32[:, h, :], o32[:, h, :], AF.Identity, reads=[b_mv, b_nb], writes=[b_o32],
                      scale=mv[:, 12 + h:13 + h], bias=nbias[:, h:h + 1])
                S.tt("vector", ogt[k][:, h * 512:(h + 1) * 512], o32[:, h, :], szt[:, h * 512:(h + 1) * 512],
                     ALU.mult, reads=[b_o32, b_szt], writes=[b_ogt[k]])
            S.dma("gpsimd", [(SC["og"][r0:r0 + 128, :], ogt[k][:])], reads=[b_ogt[k]])

        stageA(0)
        for ti in range(ntiles):
            stageB(ti)
            if ti + 1 < ntiles:
                stageA(ti + 1)
        S.finish(nc)


from concourse.bass_utils import run_bass_kernel_spmd

W_NAMES = ["norm_g", "nsa_w_in", "nsa_q_g", "nsa_kc_g", "nsa_ks_g", "nsa_kw_g", "nsa_cmp_pos_k", "nsa_cmp_pos_v",
           "nsa_cmp_k_w1", "nsa_cmp_k_w2", "nsa_cmp_v_w1", "nsa_cmp_v_w2", "nsa_w_out", "ret_w_in", "ret_w_out"]
_CACHE = {}


def build(debug=False, upto=6):
    nc = bass.Bass("TRN2", target_bir_lowering=False)
    I = {}

    def din(name, shape, dt=F32):
        I[name] = nc.dram_tensor(name, list(shape), dt, kind="ExternalInput").ap()

    din("x", [4096, 1024]); din("p0", [4096, 256]); din("p1", [4096, 256]); din("norm_g", [2, 1024])
    din("nsa_w_in", [1024, 3632])
    for nm in ["nsa_q_g", "nsa_ks_g", "nsa_kw_g", "nsa_kc_g"]:
        din(nm, [1, 64])
    din("nsa_cmp_k_w1", [2048, 256]); din("nsa_cmp_v_w1", [2048, 256]); din("nsa_cmp_k_w2", [256, 64]); din("nsa_cmp_v_w2", [256, 64])
    din("nsa_cmp_pos_k", [32, 64]); din("nsa_cmp_pos_v", [32, 64])
    din("nsa_w_out", [1024, 1024]); din("ret_w_in", [1024, 6144]); din("ret_w_out", [2048, 1024])
    din("ple_w0", [256, 1024]); din("ple_w1", [256, 1024]); din("ple_gate_w0", [1024, 1024]); din("ple_gate_w1", [1024, 1024])
    din("ident", [128, 128], BF16); din("overlap", [256, 64]); din("Eexp", [64, 4096], BF16); din("Ftab", [128, 32, 64])
    din("cmpb", [32, 2, 128, 128], BF16); din("wbias", [2, 128, 128], BF16)
    din("decT", [128, 4, 128]); din("qdT", [128, 4, 128]); din("kdec", [128, 4]); din("rope", [4096, 4, 128])
    out = nc.dram_tensor("out", [4096, 1024], F32, kind="ExternalOutput").ap()
    SC = declare_scratch(nc, debug); SC.update(declare_scratch2(nc, debug)); SC.update(declare_scratch3(nc, debug))
    SC.update(declare_scratch4(nc, debug)); SC.update(declare_scratch5(nc, debug))
    C = make_consts()
    with ExitStack() as es:
        G = Glob(nc, es)
        phase1(nc, G, I, SC)
        if upto >= 2:
            phase2(nc, G, I, SC)
        if upto >= 3:
            phase3(nc, G, I, SC)
        g1 = bass.AP(I["norm_g"].tensor, 1024, [[0, 128], [1, 1024]])
        if upto >= 4:
          epilogue(nc, G, "e0_", SC["outg"], 1024, I["nsa_w_out"], I["x"], I["p0"], I["ple_gate_w0"], I["ple_w0"],
                 SC["x2"], I["ident"], norm_g_row=g1, hT_out=SC["h2T"])
        if upto >= 5:
            phase5(nc, G, I, SC, C["chunk_decay"])
        if upto >= 6:
          epilogue(nc, G, "e1_", SC["og"], 2048, I["ret_w_out"], SC["x2"], I["p1"], I["ple_gate_w1"], I["ple_w1"],
                 out, I["ident"])
    return nc


def make_in_maps(inputs, cores):
    C = make_consts()
    shared = {}
    f32 = lambda a: np.ascontiguousarray(np.asarray(a, dtype=np.float32))
    shared["norm_g"] = f32(inputs["norm_g"])
    for nm in ["nsa_w_in", "nsa_q_g", "nsa_kc_g", "nsa_ks_g", "nsa_kw_g", "nsa_cmp_pos_k", "nsa_cmp_pos_v",
               "nsa_cmp_k_w1", "nsa_cmp_k_w2", "nsa_cmp_v_w1", "nsa_cmp_v_w2", "nsa_w_out", "ret_w_in", "ret_w_out"]:
        a = f32(inputs[nm])[0]
        if a.ndim == 1:
            a = a[None, :]
        shared[nm] = np.ascontiguousarray(a)
    for l in range(2):
        shared[f"ple_w{l}"] = f32(inputs["ple_w"])[l]
        shared[f"ple_gate_w{l}"] = f32(inputs["ple_gate_w"])[l]
    for k in ["ident", "overlap", "Eexp", "Ftab", "cmpb", "wbias", "decT", "qdT", "kdec", "rope"]:
        shared[k] = C[k]
    x = f32(inputs["x"]); p = f32(inputs["p"])
    maps = []
    for b in cores:
        m = dict(shared)
        m["x"] = x[b]; m["p0"] = p[0, b]; m["p1"] = p[1, b]
        maps.append(m)
    return maps


def kernel(**inputs):
    if "nc" not in _CACHE:
        _CACHE["nc"] = build()
    nc = _CACHE["nc"]
    maps = make_in_maps(inputs, list(range(8)))
    res = run_bass_kernel_spmd(nc, maps, core_ids=list(range(8)))
    return np.stack([np.asarray(r["out"], dtype=np.float32) for r in res.results], axis=0)
```
